# Optimizing a Trainium2 kernel written in Bass

```python
import math
import jax
import jax.numpy as jnp
from jax import lax
import numpy as np

D_MODEL = 1024
BATCH = 8
SEQ = 4096
DEPTH = 2

GRID_W = 64
CTX_LEN = 256
QBLOCK = 128
ROPE_THETA = 10000.0
NORM_EPS = 1e-6
N_MOD = 6

MIX_WIDTH = D_MODEL
HEAD_DIM = 64
GQA_HEADS = 8
GQA_KV_HEADS = 2
GQA_GROUP = GQA_HEADS // GQA_KV_HEADS
GQA_WIDTH = GQA_HEADS * HEAD_DIM
DIFF_HEADS = 4
DIFF_QK_DIM = HEAD_DIM // 2
DIFF_V_DIM = HEAD_DIM
DIFF_WIDTH = DIFF_HEADS * DIFF_V_DIM
MLA_HEADS = 4
MLA_Q_RANK = 192
MLA_KV_RANK = 128
MLA_NOPE_DIM = 64
MLA_ROPE_DIM = 32
MLA_V_DIM = 64
MLA_WIDTH = MLA_HEADS * MLA_V_DIM
IN_SPLITS = (GQA_HEADS * HEAD_DIM, GQA_KV_HEADS * HEAD_DIM, GQA_KV_HEADS * HEAD_DIM,
             DIFF_HEADS * 2 * DIFF_QK_DIM, DIFF_HEADS * 2 * DIFF_QK_DIM, DIFF_HEADS * DIFF_V_DIM,
             MLA_Q_RANK, MLA_KV_RANK, MLA_ROPE_DIM)
IN_WIDTH = sum(IN_SPLITS)
D_FF_DENSE = 2816
N_EXPERTS = 8
TOP_K = 2
D_FF_EXPERT = 1792

kernel_name = "hybrid_diffusion_prefix_block"


def _rmsnorm(x, g):
    xf = x.astype(jnp.float32)
    y = xf * lax.rsqrt(jnp.mean(xf * xf, axis=-1, keepdims=True) + NORM_EPS)
    return (y * g.astype(jnp.float32)).astype(x.dtype)


def _modulate(h, shift, scale):
    return h * (1 + scale) + shift


def _split_cols(y):
    out, start = [], 0
    for w in IN_SPLITS:
        out.append(y[..., start:start + w])
        start += w
    return out


def _axial_rope_table(rows, cols, dim):
    quarter = dim // 4
    inv_freq = ROPE_THETA ** (-jnp.arange(quarter, dtype=jnp.float32) / quarter)
    ang = jnp.concatenate([rows[:, None] * inv_freq, cols[:, None] * inv_freq], axis=-1)
    return jnp.cos(ang)[:, None, :], jnp.sin(ang)[:, None, :]


def _rope(x, table):
    cos, sin = table[0].astype(x.dtype), table[1].astype(x.dtype)
    xr = x.reshape(*x.shape[:-1], x.shape[-1] // 2, 2)
    x1, x2 = xr[..., 0], xr[..., 1]
    return jnp.stack([x1 * cos - x2 * sin, x1 * sin + x2 * cos], axis=-1).reshape(x.shape)


def _softmax32(s, scale):
    return jax.nn.softmax(s.astype(jnp.float32) * scale, axis=-1)


def _sweep(fn, *qs):
    B, S = qs[0].shape[:2]
    nb = S // QBLOCK
    blocks = tuple(jnp.moveaxis(q.reshape(B, nb, QBLOCK, *q.shape[2:]), 1, 0) for q in qs)
    out = lax.map(lambda blk: fn(*blk), blocks)
    return jnp.moveaxis(out, 0, 1).reshape(B, S, *out.shape[3:])


def _gqa_attend(k, v):
    scale = HEAD_DIM ** -0.5

    def fn(q):
        p = _softmax32(jnp.einsum('bqhgd,bkhd->bhgqk', q, k), scale).astype(v.dtype)
        return jnp.einsum('bhgqk,bkhd->bqhgd', p, v)
    return fn


def _diff_attend(k, v, lam):
    scale = DIFF_QK_DIM ** -0.5

    def fn(q):
        p = _softmax32(jnp.einsum('bqhjd,bkhjd->bhjqk', q, k), scale)
        a = (p[:, :, 0] - lam * p[:, :, 1]).astype(v.dtype)
        return jnp.einsum('bhqk,bkhd->bqhd', a, v)
    return fn


def _mla_attend(k_nope, k_rope, v):
    scale = (MLA_NOPE_DIM + MLA_ROPE_DIM) ** -0.5

    def fn(q_nope, q_rope):
        s = jnp.einsum('bqhd,bkhd->bhqk', q_nope, k_nope) + jnp.einsum('bqhd,bkd->bhqk', q_rope, k_rope)
        p = _softmax32(s, scale).astype(v.dtype)
        return jnp.einsum('bhqk,bkhd->bqhd', p, v)
    return fn


def _gqa_mixer(lat, ctxp, gq, gk, table, need_ctx):
    q, k, v = lat
    qc, kc, vc = ctxp
    B, S = q.shape[:2]
    C = qc.shape[1]
    q = _rope(_rmsnorm(q.reshape(B, S, GQA_HEADS, HEAD_DIM), gq), table)
    k = _rope(_rmsnorm(k.reshape(B, S, GQA_KV_HEADS, HEAD_DIM), gk), table)
    v = v.reshape(B, S, GQA_KV_HEADS, HEAD_DIM)
    kc = _rmsnorm(kc.reshape(B, C, GQA_KV_HEADS, HEAD_DIM), gk)
    vc = vc.reshape(B, C, GQA_KV_HEADS, HEAD_DIM)
    fn = _gqa_attend(jnp.concatenate([kc, k], axis=1), jnp.concatenate([vc, v], axis=1))
    o = _sweep(fn, q.reshape(B, S, GQA_KV_HEADS, GQA_GROUP, HEAD_DIM)).reshape(B, S, GQA_WIDTH)
    oc = None
    if need_ctx:
        qc = _rmsnorm(qc.reshape(B, C, GQA_HEADS, HEAD_DIM), gq).reshape(B, C, GQA_KV_HEADS, GQA_GROUP, HEAD_DIM)
        oc = _gqa_attend(kc, vc)(qc).reshape(B, C, GQA_WIDTH)
    return o, oc


def _diff_mixer(lat, ctxp, lq1, lk1, lq2, lk2, g_sub, lam_init, table, need_ctx):
    q, k, v = lat
    qc, kc, vc = ctxp
    B, S = q.shape[:2]
    C = qc.shape[1]
    f32 = jnp.float32
    lam = (jnp.exp(jnp.sum(lq1.astype(f32) * lk1.astype(f32)))
           - jnp.exp(jnp.sum(lq2.astype(f32) * lk2.astype(f32))) + lam_init)
    q = _rope(q.reshape(B, S, DIFF_HEADS * 2, DIFF_QK_DIM), table).reshape(B, S, DIFF_HEADS, 2, DIFF_QK_DIM)
    k = _rope(k.reshape(B, S, DIFF_HEADS * 2, DIFF_QK_DIM), table).reshape(B, S, DIFF_HEADS, 2, DIFF_QK_DIM)
    v = v.reshape(B, S, DIFF_HEADS, DIFF_V_DIM)
    kc = kc.reshape(B, C, DIFF_HEADS, 2, DIFF_QK_DIM)
    vc = vc.reshape(B, C, DIFF_HEADS, DIFF_V_DIM)
    fn = _diff_attend(jnp.concatenate([kc, k], axis=1), jnp.concatenate([vc, v], axis=1), lam)
    o = _sweep(fn, q)
    o = (_rmsnorm(o, g_sub) * (1 - lam_init)).reshape(B, S, DIFF_WIDTH)
    oc = None
    if need_ctx:
        oc = _diff_attend(kc, vc, lam)(qc.reshape(B, C, DIFF_HEADS, 2, DIFF_QK_DIM))
        oc = (_rmsnorm(oc, g_sub) * (1 - lam_init)).reshape(B, C, DIFF_WIDTH)
    return o, oc


def _mla_mixer(lat, ctxp, g_cq, g_ckv, w_uq, w_ukv, table, need_ctx):
    cq, ckv, kr = lat
    cqc, ckvc, krc = ctxp
    B, S = cq.shape[:2]
    C = cqc.shape[1]

    def up_q(t):
        y = (_rmsnorm(t, g_cq) @ w_uq).reshape(*t.shape[:2], MLA_HEADS, MLA_NOPE_DIM + MLA_ROPE_DIM)
        return y[..., :MLA_NOPE_DIM], y[..., MLA_NOPE_DIM:]

    def up_kv(t):
        y = (_rmsnorm(t, g_ckv) @ w_ukv).reshape(*t.shape[:2], MLA_HEADS, MLA_NOPE_DIM + MLA_V_DIM)
        return y[..., :MLA_NOPE_DIM], y[..., MLA_NOPE_DIM:]

    qn, qr = up_q(cq)
    qr = _rope(qr, table)
    kn, v = up_kv(ckv)
    kr = _rope(kr[:, :, None, :], table)[:, :, 0]
    knc, vc = up_kv(ckvc)
    fn = _mla_attend(jnp.concatenate([knc, kn], axis=1), jnp.concatenate([krc, kr], axis=1),
                     jnp.concatenate([vc, v], axis=1))
    o = _sweep(fn, qn, qr).reshape(B, S, MLA_WIDTH)
    oc = None
    if need_ctx:
        qnc, qrc = up_q(cqc)
        oc = _mla_attend(knc, krc, vc)(qnc, qrc).reshape(B, C, MLA_WIDTH)
    return o, oc


def _swiglu(h, w_in, w_out):
    gate, up = jnp.split(h @ w_in, 2, axis=-1)
    return (jax.nn.silu(gate) * up) @ w_out


def _moe_swiglu(h, w_router, w_in, w_out):
    logits = (h @ w_router).astype(jnp.float32)
    top_val, top_idx = lax.top_k(logits, TOP_K)
    top_w = jax.nn.softmax(top_val, axis=-1)
    gates = jnp.sum(jax.nn.one_hot(top_idx, N_EXPERTS, dtype=jnp.float32) * top_w[..., None], axis=-2)
    gates = gates.astype(h.dtype)
    out = jnp.zeros_like(h)
    for e in range(N_EXPERTS):
        out = out + gates[..., e:e + 1] * _swiglu(h, w_in[e], w_out[e])
    return out


def setup_inputs(seed: int = 0) -> dict:
    key = jax.random.key(seed)
    ks = jax.random.split(key, 32)
    n_dense = (DEPTH + 1) // 2
    n_moe = DEPTH // 2
    f32 = jnp.float32

    def nrm(k, shape, s):
        return jax.random.normal(k, shape, f32) * s

    def gain(k, shape):
        return 1.0 + 0.1 * jax.random.normal(k, shape, f32)

    return {
        "x": nrm(ks[0], (BATCH, SEQ, D_MODEL), 1.0),
        "c": nrm(ks[1], (BATCH, D_MODEL), 1.0),
        "ctx": nrm(ks[2], (BATCH, CTX_LEN, D_MODEL), 1.0),
        "c_ctx": nrm(ks[3], (D_MODEL,), 1.0),
        "w_mod": nrm(ks[4], (DEPTH, D_MODEL, N_MOD * D_MODEL), 0.5 * D_MODEL ** -0.5),
        "b_mod": nrm(ks[5], (DEPTH, N_MOD * D_MODEL), 0.02),
        "g_attn": gain(ks[6], (DEPTH, D_MODEL)),
        "g_ffn": gain(ks[7], (DEPTH, D_MODEL)),
        "w_in": nrm(ks[8], (DEPTH, D_MODEL, IN_WIDTH), D_MODEL ** -0.5),
        "w_out": nrm(ks[9], (DEPTH, MIX_WIDTH, D_MODEL), MIX_WIDTH ** -0.5),
        "gqa_gq": gain(ks[10], (DEPTH, HEAD_DIM)),
        "gqa_gk": gain(ks[11], (DEPTH, HEAD_DIM)),
        "diff_lq1": nrm(ks[12], (DEPTH, DIFF_QK_DIM), 0.1),
        "diff_lk1": nrm(ks[13], (DEPTH, DIFF_QK_DIM), 0.1),
        "diff_lq2": nrm(ks[14], (DEPTH, DIFF_QK_DIM), 0.1),
        "diff_lk2": nrm(ks[15], (DEPTH, DIFF_QK_DIM), 0.1),
        "diff_gsub": gain(ks[16], (DEPTH, DIFF_V_DIM)),
        "mla_gcq": gain(ks[17], (DEPTH, MLA_Q_RANK)),
        "mla_gckv": gain(ks[18], (DEPTH, MLA_KV_RANK)),
        "mla_wuq": nrm(ks[19], (DEPTH, MLA_Q_RANK, MLA_HEADS * (MLA_NOPE_DIM + MLA_ROPE_DIM)), MLA_Q_RANK ** -0.5),
        "mla_wukv": nrm(ks[20], (DEPTH, MLA_KV_RANK, MLA_HEADS * (MLA_NOPE_DIM + MLA_V_DIM)), MLA_KV_RANK ** -0.5),
        "ffn_w_in": nrm(ks[21], (n_dense, D_MODEL, 2 * D_FF_DENSE), D_MODEL ** -0.5),
        "ffn_w_out": nrm(ks[22], (n_dense, D_FF_DENSE, D_MODEL), D_FF_DENSE ** -0.5),
        "moe_router": nrm(ks[23], (n_moe, D_MODEL, N_EXPERTS), D_MODEL ** -0.5),
        "moe_w_in": nrm(ks[24], (n_moe, N_EXPERTS, D_MODEL, 2 * D_FF_EXPERT), D_MODEL ** -0.5),
        "moe_w_out": nrm(ks[25], (n_moe, N_EXPERTS, D_FF_EXPERT, D_MODEL), D_FF_EXPERT ** -0.5),
        "g_final": gain(ks[26], (D_MODEL,)),
    }


def reference(x, c, ctx, c_ctx, w_mod, b_mod, g_attn, g_ffn, w_in, w_out, gqa_gq, gqa_gk,
              diff_lq1, diff_lk1, diff_lq2, diff_lk2, diff_gsub, mla_gcq, mla_gckv, mla_wuq, mla_wukv,
              ffn_w_in, ffn_w_out, moe_router, moe_w_in, moe_w_out, g_final):
    B, S, D = x.shape
    ROWS = S // GRID_W
    t = jnp.arange(ROWS * GRID_W)
    rows = (t // GRID_W).astype(jnp.float32)
    cols = (t % GRID_W).astype(jnp.float32)
    table_a = _axial_rope_table(rows, cols, HEAD_DIM)
    table_b = _axial_rope_table(rows, cols, DIFF_QK_DIM)
    table_c = _axial_rope_table(rows, cols, MLA_ROPE_DIM)
    sc = jax.nn.silu(c)
    scc = jax.nn.silu(c_ctx)
    xc = ctx
    for l in range(DEPTH):
        need_ctx = l < DEPTH - 1
        mod = (sc @ w_mod[l] + b_mod[l]).reshape(B, N_MOD, 1, D)
        modc = (scc @ w_mod[l] + b_mod[l]).reshape(N_MOD, D)
        h = _modulate(_rmsnorm(x, g_attn[l]), mod[:, 0], mod[:, 1])
        hc = _modulate(_rmsnorm(xc, g_attn[l]), modc[0], modc[1])
        p = _split_cols(h @ w_in[l])
        pc = _split_cols(hc @ w_in[l])
        oa, oac = _gqa_mixer(p[0:3], pc[0:3], gqa_gq[l], gqa_gk[l], table_a, need_ctx)
        ob, obc = _diff_mixer(p[3:6], pc[3:6], diff_lq1[l], diff_lk1[l], diff_lq2[l], diff_lk2[l],
                              diff_gsub[l], 0.8 - 0.6 * math.exp(-0.3 * l), table_b, need_ctx)
        om, omc = _mla_mixer(p[6:9], pc[6:9], mla_gcq[l], mla_gckv[l], mla_wuq[l], mla_wukv[l],
                             table_c, need_ctx)
        x = x + mod[:, 2] * (jnp.concatenate([oa, ob, om], axis=-1) @ w_out[l])
        if need_ctx:
            xc = xc + modc[2] * (jnp.concatenate([oac, obc, omc], axis=-1) @ w_out[l])
        if l % 2 == 0:
            wi, wo = ffn_w_in[l // 2], ffn_w_out[l // 2]
            ffn = lambda u: _swiglu(u, wi, wo)
        else:
            wr, wi, wo = moe_router[l // 2], moe_w_in[l // 2], moe_w_out[l // 2]
            ffn = lambda u: _moe_swiglu(u, wr, wi, wo)
        h2 = _modulate(_rmsnorm(x, g_ffn[l]), mod[:, 3], mod[:, 4])
        x = x + mod[:, 5] * ffn(h2)
        if need_ctx:
            h2c = _modulate(_rmsnorm(xc, g_ffn[l]), modc[3], modc[4])
            xc = xc + modc[5] * ffn(h2c)
    return _rmsnorm(x, g_final)
```

```python
import math
from contextlib import ExitStack

import numpy as np
import concourse.bass as bass
import concourse.mybir as mybir
from concourse.bass_utils import run_bass_kernel_spmd

F32 = mybir.dt.float32
BF16 = mybir.dt.bfloat16
AF = mybir.ActivationFunctionType
ALU = mybir.AluOpType
AX = mybir.AxisListType

D = 1024
S_LAT = 4096
CTX = 256
T = S_LAT + CTX
NT128 = T // 128
EPS = 1e-6
ENGINES = ("pe", "act", "dve", "pool", "sp")

FM_CHUNKS = 24
NX = FM_CHUNKS * 128 + 384
VG_OFF = FM_CHUNKS * 128
VD_OFF = VG_OFF + 128


def _winx_cols():
    def sw(a):
        return [i ^ 1 for i in a]
    cols = []
    qg = list(range(0, 512))
    for c in range(4):
        cols += qg[c * 128:(c + 1) * 128]
    for c in range(4):
        cols += sw(qg[c * 128:(c + 1) * 128])
    for kv in range(2):
        k = list(range(512 + kv * 64, 512 + kv * 64 + 64))
        cols += k + k
    for kv in range(2):
        k = sw(list(range(512 + kv * 64, 512 + kv * 64 + 64)))
        cols += k + k
    qd = list(range(768, 1024))
    for c in range(2):
        cols += qd[c * 128:(c + 1) * 128]
    for c in range(2):
        cols += sw(qd[c * 128:(c + 1) * 128])
    kd = list(range(1024, 1280))
    for c in range(2):
        cols += kd[c * 128:(c + 1) * 128]
    for c in range(2):
        cols += sw(kd[c * 128:(c + 1) * 128])
    cols += list(range(1536, 1664))
    b = list(range(1664, 1728)) + list(range(1856, 1888))
    cols += b + [1664] * 32
    bs = list(range(1664, 1728)) + sw(list(range(1856, 1888)))
    cols += bs + [1664] * 32
    cols += list(range(1728, 1856))
    assert len(cols) == FM_CHUNKS * 128
    cols += list(range(640, 768))
    cols += list(range(1280, 1536))
    assert len(cols) == NX
    return np.array(cols, dtype=np.int64)


def _rope_tables():
    t = np.arange(S_LAT)
    rows = (t // 64).astype(np.float32)
    colsf = (t % 64).astype(np.float32)

    def tab(dim):
        quarter = dim // 4
        inv = (np.float32(10000.0) ** (-np.arange(quarter, dtype=np.float32) / np.float32(quarter))).astype(np.float32)
        ang = np.concatenate([rows[:, None] * inv, colsf[:, None] * inv], axis=-1).astype(np.float32)
        cos = np.cos(ang).astype(np.float32)
        sin = np.sin(ang).astype(np.float32)
        cd = np.repeat(cos, 2, axis=1)
        sd = np.repeat(sin, 2, axis=1)
        sd[:, 0::2] *= -1.0
        reps = 128 // dim
        cfull = np.ones((128, T), np.float32)
        sfull = np.zeros((128, T), np.float32)
        cfull[:, :S_LAT] = np.tile(cd.T, (reps, 1))
        sfull[:, :S_LAT] = np.tile(sd.T, (reps, 1))
        return cfull, sfull
    return tab(64) + tab(32)


class Op:
    __slots__ = ("eng", "emit", "deps", "needed", "tok", "dma", "gidx", "grp")

    def __init__(self, eng, emit, dma=None):
        self.eng = eng
        self.emit = emit
        self.deps = []
        self.needed = False
        self.tok = None
        self.dma = dma
        self.gidx = 0
        self.grp = None


class Sched:
    def __init__(self, nc, stack):
        self.nc = nc
        self.stack = stack
        self.ops = {e: [] for e in ENGINES}
        self.last_w = {}
        self.readers = {}
        self.esem = {e: stack.enter_context(nc.semaphore("es_" + e)) for e in ENGINES}
        self.dsem = {}
        self.dcum = {}
        self.dlast = {}
        self.final_waits = []
        self.bar_deps = []
        self.bar_gen = 0
        self.bar_seen = {e: 0 for e in ENGINES}
        self.nrec = 0
        self.cur_grp = None
        self.ngrp = 0

    def begin_group(self):
        self.ngrp += 1
        self.cur_grp = (self.ngrp, self.nrec)

    def end_group(self):
        self.cur_grp = None

    def _rec(self, op, reads, writes):
        deps = op.deps
        e = op.eng
        self.nrec += 1
        op.gidx = self.nrec
        op.grp = self.cur_grp
        if self.bar_seen[e] != self.bar_gen:
            self.bar_seen[e] = self.bar_gen
            deps.extend(self.bar_deps)
        lw = self.last_w
        rd = self.readers
        for k in reads:
            w = lw.get(k)
            if w is not None:
                deps.append(w)
            rd.setdefault(k, []).append(op)
        for k in writes:
            w = lw.get(k)
            if w is not None:
                deps.append(w)
            r = rd.get(k)
            if r:
                deps.extend(x for x in r if x is not op)
            rd[k] = []
            lw[k] = op
        self.ops[e].append(op)
        return op

    def op(self, eng, emit, reads=(), writes=()):
        return self._rec(Op(eng, emit), reads, writes)

    def dma(self, eng, chan, emit, reads=(), writes=()):
        if chan not in self.dsem:
            self.dsem[chan] = self.stack.enter_context(self.nc.semaphore("ds%d" % len(self.dsem)))
            self.dcum[chan] = 0
        self.dcum[chan] += 16
        op = Op(eng, emit, dma=(chan, self.dcum[chan]))
        self.dlast[chan] = op
        return self._rec(op, reads, writes)

    def barrier(self):
        deps = []
        for e in ENGINES:
            for op in reversed(self.ops[e]):
                if op.dma is None:
                    deps.append(op)
                    break
        deps.extend(self.dlast.values())
        self.bar_deps = deps
        self.bar_gen += 1
        self.last_w = {}
        self.readers = {}

    def finalize(self):
        for e in ENGINES:
            for op in self.ops[e]:
                best = {}
                for d in op.deps:
                    if d.dma is not None or (d.eng == "pe" and e == "pe"):
                        continue
                    cur = best.get(d.eng)
                    if cur is None or d.gidx > cur.gidx:
                        best[d.eng] = d
                for d in best.values():
                    d.needed = True
        for e in ENGINES:
            n = 0
            for op in self.ops[e]:
                if op.dma is not None:
                    op.tok = (("dma", op.dma[0]), op.dma[1])
                elif op.needed:
                    n += 1
                    op.tok = (e, n)
        self.plan = {}
        for e in ENGINES:
            seen = {}
            plan = []
            hoist = {}
            for op in self.ops[e]:
                if op.grp is not None:
                    h = hoist.setdefault(op.grp[0], {})
                    for d in op.deps:
                        if d.tok is None or d.gidx > op.grp[1]:
                            continue
                        sk, v = d.tok
                        if h.get(sk, 0) < v:
                            h[sk] = v
            done_grp = set()
            for op in self.ops[e]:
                need = {}
                if op.grp is not None and op.grp[0] not in done_grp:
                    done_grp.add(op.grp[0])
                    need.update(hoist[op.grp[0]])
                for d in op.deps:
                    if d.tok is None:
                        continue
                    sk, v = d.tok
                    if need.get(sk, 0) < v:
                        need[sk] = v
                waits = []
                for sk, v in need.items():
                    if seen.get(sk, 0) < v:
                        seen[sk] = v
                        waits.append((sk, v))
                plan.append((waits, op))
            self.plan[e] = plan

    def _sem(self, sk):
        if isinstance(sk, tuple):
            return self.dsem[sk[1]]
        return self.esem[sk]

    def emit_engine(self, e, eng):
        for waits, op in self.plan[e]:
            for sk, v in waits:
                eng.wait_ge(self._sem(sk), v)
            ins = op.emit(eng)
            if op.dma is not None:
                ins.then_inc(self.dsem[op.dma[0]], 16)
            elif op.needed:
                ins.then_inc(self.esem[e], 1)
        if e == "sp":
            for chan in self.final_waits:
                eng.wait_ge(self.dsem[chan], self.dcum[chan])

    def run(self):
        self.finalize()
        with self.nc.Block() as block:
            @block.tensor
            def _(eng):
                self.emit_engine("pe", eng)

            @block.scalar
            def _(eng):
                self.emit_engine("act", eng)

            @block.vector
            def _(eng):
                self.emit_engine("dve", eng)

            @block.gpsimd
            def _(eng):
                self.emit_engine("pool", eng)

            @block.sync
            def _(eng):
                self.emit_engine("sp", eng)


class Arena:
    def __init__(self, nc, stack, nbytes):
        self.n = nbytes
        self.t = stack.enter_context(nc.sbuf_tensor("arena", [128, nbytes // 4], F32))
        self.off = 0

    def reset(self, off=0):
        self.off = off

    def f32(self, cols):
        a = self.off
        self.off += cols * 4
        assert self.off <= self.n, ("arena overflow", self.off, self.n)
        return self.t[:, a // 4:a // 4 + cols]

    def bf16(self, cols):
        c2 = (cols + 1) // 2 * 2
        a = self.off
        self.off += c2 * 2
        assert self.off <= self.n, ("arena overflow", self.off, self.n)
        return self.t[:, a // 4:a // 4 + c2 // 2].bitcast(BF16)[:, 0:cols]


ARENA_BYTES = 209200
H_BYTES = 8 * T * 2


def build(n_layers=2, debug=False):
    nc = bass.Bass("TRN2", target_bir_lowering=False)

    def din(name, shape):
        return nc.dram_tensor(name, list(shape), F32, kind="ExternalInput").ap()

    x_d = din("x", [S_LAT, D])
    ctx_d = din("ctx", [CTX, D])
    cc_d = din("cc", [2, D])
    w_mod_d = din("w_mod", [2, D, 6 * D])
    b_mod_d = din("b_mod", [2, 6 * D])
    g_attn_d = din("g_attn", [2, D])
    g_ffn_d = din("g_ffn", [2, D])
    g_final_d = din("g_final", [1, D])
    w_inx_d = din("w_inx", [2, D, NX])
    w_out_d = din("w_out", [2, D, D])
    smallp_d = din("smallp", [128, 16])
    lamv_d = din("lamv", [2, 128])
    w_uq_d = din("w_uq", [2, 192, 384])
    w_uqs_d = din("w_uqs", [2, 192, 384])
    w_ukvk_d = din("w_ukvk", [2, 128, 256])
    w_ukvv_d = din("w_ukvv", [2, 128, 256])
    ffn_w_in_d = din("ffn_w_in", [1, D, 5632])
    ffn_w_out_d = din("ffn_w_out", [1, 2816, D])
    moe_router_d = din("moe_router", [1, D, 8])
    moe_w_in_d = din("moe_w_in", [1, 8, D, 3584])
    moe_w_out_d = din("moe_w_out", [1, 8, 1792, D])
    ropeA_c_d = din("ropeA_c", [128, T])
    ropeA_s_d = din("ropeA_s", [128, T])
    ropeB_c_d = din("ropeB_c", [128, T])
    ropeB_s_d = din("ropeB_s", [128, T])
    cst_d = din("cst", [128, 384])
    out_d = nc.dram_tensor("out", [S_LAT, D], F32, kind="ExternalOutput").ap()
    skind = "ExternalOutput" if debug else "Internal"
    xs_d = nc.dram_tensor("xs", [T, D], F32, kind=skind).ap()
    ao_d = nc.dram_tensor("ao", [8, 128, T], BF16, kind=skind).ap()
    modv_d = nc.dram_tensor("modv", [2, 2, 6 * D], F32, kind=skind).ap()
    if debug:
        dbg_d = nc.dram_tensor("dbg", [128, 8 * T], BF16, kind="ExternalOutput").ap()

    with ExitStack() as st:
        S = Sched(nc, st)
        A = Arena(nc, st, ARENA_BYTES)
        psall = st.enter_context(nc.psum_tensor("psall", [128, 4096], F32))
        ps = [psall[:, i * 512:(i + 1) * 512] for i in range(8)]
        cstb = st.enter_context(nc.sbuf_tensor("cstb", [128, 384], BF16))
        smallp = st.enter_context(nc.sbuf_tensor("smallp_sb", [128, 16], F32))
        scc = st.enter_context(nc.sbuf_tensor("scc", [128, 16], F32))
        ssq = st.enter_context(nc.sbuf_tensor("ssq", [128, 40], F32))
        rstd = st.enter_context(nc.sbuf_tensor("rstd", [128, 40], F32))
        gates = st.enter_context(nc.sbuf_tensor("gates", [128, 32 * 8], F32))
        tiny = st.enter_context(nc.sbuf_tensor("tiny", [128, 64], F32))
        cvec = st.enter_context(nc.sbuf_tensor("cvec", [128, 8], F32))
        lamt = st.enter_context(nc.sbuf_tensor("lamt", [128, 160], F32))
        ident = cstb[:, 0:128]
        ones = cstb[:, 128:256]
        bd64 = cstb[:, 256:384]
        PK = lambda i: ("ps", i)

        def hT_view():
            return A.t[:, 0:H_BYTES // 4].bitcast(BF16).rearrange("p (k n) -> p k n", k=8)

        for ci_, cv_ in enumerate((EPS, 64 * EPS, 192 * EPS, 128 * EPS)):
            S.op("dve", lambda e, ci_=ci_, cv_=cv_: e.memset(cvec[:, ci_:ci_ + 1], cv_), writes=["cvec"])
        S.dma("pool", "cst", lambda e: e.dma_start(out=cstb[:], in_=cst_d), writes=["cst"])
        S.dma("sp", "smallp", lambda e: e.dma_start(out=smallp[:], in_=smallp_d), writes=["smallp"])
        scc3 = scc[:].rearrange("p (k r) -> p k r", r=2)
        S.dma("sp", "cc0", lambda e: e.dma_start(out=scc3[:, :, 0], in_=cc_d[0].rearrange("(k p) -> p k", p=128),
                                                  allow_slow_non_contiguous=True), writes=["scc0"])
        S.dma("sp", "cc1", lambda e: e.dma_start(out=scc3[:, :, 1], in_=cc_d[1].rearrange("(k p) -> p k", p=128),
                                                  allow_slow_non_contiguous=True), writes=["scc1"])
        S.op("act", lambda e: e.activation(scc[:], scc[:], AF.Silu), reads=["scc0", "scc1"], writes=["scc"])

        def phase_mod(l):
            A.reset(0)
            wm = [A.f32(8 * 512).rearrange("p (k n) -> p k n", k=8) for _ in range(2)]
            modsb = A.f32(6 * D)
            bmod = A.f32(6 * D)
            gt = A.f32(2 * D)
            S.dma("sp", "bmod", lambda e: e.dma_start(out=bmod[0:2, :], in_=b_mod_d[l].partition_broadcast(2)), writes=["bmod"])
            S.dma("sp", "gt0", lambda e: e.dma_start(out=gt[0:2, 0:D], in_=g_attn_d[l].partition_broadcast(2)), writes=["gt0"])
            S.dma("sp", "gt1", lambda e: e.dma_start(out=gt[0:2, D:2 * D], in_=g_ffn_d[l].partition_broadcast(2)), writes=["gt1"])
            for nt in range(12):
                sl = nt % 2
                S.dma("sp", ("wm", sl), lambda e, nt=nt, sl=sl: e.dma_start(
                    out=wm[sl], in_=w_mod_d[l][:, nt * 512:(nt + 1) * 512].rearrange("(k p) n -> p k n", p=128)),
                    writes=[("wm", sl)])
                for k in range(8):
                    S.op("pe", lambda e, sl=sl, k=k: e.matmul(ps[sl][0:2, :], scc3[:, k, :], wm[sl][:, k, :],
                                                             start=(k == 0), stop=(k == 7)),
                         reads=[("wm", sl), "scc"], writes=[PK(sl)])
                S.op("dve", lambda e, sl=sl, nt=nt: e.tensor_tensor(modsb[0:2, nt * 512:(nt + 1) * 512], ps[sl][0:2, :],
                                                                   bmod[0:2, nt * 512:(nt + 1) * 512], ALU.add),
                     reads=[PK(sl), "bmod"], writes=["modsb"])
            for j, g0 in ((1, 0), (4, D)):
                S.op("dve", lambda e, j=j, g0=g0: e.scalar_tensor_tensor(
                    modsb[0:2, j * D:(j + 1) * D], modsb[0:2, j * D:(j + 1) * D], 1.0, gt[0:2, g0:g0 + D], ALU.add, ALU.mult),
                    reads=["modsb", "gt0", "gt1"], writes=["modsb"])
            S.dma("sp", "modv", lambda e: e.dma_start(out=modv_d[l], in_=modsb[0:2, :]), reads=["modsb"], writes=[("modv", l)])

        for l in range(n_layers):
            phase_mod(l)
            S.barrier()

        def load_bc(dst, l, r, j, key):
            S.dma("sp", key, lambda e: e.dma_start(out=dst, in_=modv_d[l][r, j * D:(j + 1) * D].partition_broadcast(128)),
                  reads=[("modv", l)], writes=[key])

        def src_tile(l, t):
            if l == 0:
                if t < 32:
                    return x_d[t * 128:(t + 1) * 128, :]
                return ctx_d[(t - 32) * 128:(t - 31) * 128, :]
            return xs_d[t * 128:(t + 1) * 128, :]

        def norm_mod_T(tag, t, xt_ap, xt_key, mul_bc, mul_key, add_bc, add_key, bufs, hT, deferred=None):
            sl = t % 2
            sqj, t1, hb = bufs["sqj"][sl], bufs["t1"][sl], bufs["hb"][sl]
            S.op("act", lambda e: e.activation(sqj, xt_ap, AF.Square, accum_out=ssq[:, t:t + 1]),
                 reads=[xt_key], writes=[(tag, "sqj", sl), (tag, "ssq", t)])
            S.op("act", lambda e: e.activation(rstd[:, t:t + 1], ssq[:, t:t + 1], AF.Ln, bias=cvec[:, 0:1], scale=1.0 / D),
                 reads=[(tag, "ssq", t)], writes=[(tag, "rstd", t)])
            S.op("act", lambda e: e.activation(rstd[:, t:t + 1], rstd[:, t:t + 1], AF.Exp, scale=-0.5),
                 reads=[(tag, "rstd", t)], writes=[(tag, "rstd", t)])
            S.op("dve", lambda e: e.scalar_tensor_tensor(t1, xt_ap, rstd[:, t:t + 1], mul_bc, ALU.mult, ALU.mult),
                 reads=[xt_key, (tag, "rstd", t), mul_key], writes=[(tag, "t1", sl)])
            S.op("pool", lambda e: e.tensor_tensor(hb, t1, add_bc, ALU.add),
                 reads=[(tag, "t1", sl), add_key], writes=[(tag, "hb", sl)])
            pb = 6 + sl
            psT = ps[pb][:].bitcast(BF16)
            for k in range(8):
                S.op("pe", lambda e, k=k: e.transpose(psT[:, k * 128:(k + 1) * 128], hb[:, k * 128:(k + 1) * 128], ident),
                     reads=[(tag, "hb", sl), "cst"], writes=[PK(pb)])
            def evac():
                S.op("act", lambda e: e.activation(hT[:, :, t * 128:(t + 1) * 128],
                                                   psT[:, 0:1024].rearrange("p (k n) -> p k n", k=8), AF.Copy),
                     reads=[PK(pb)], writes=[("hT", t)])
            if deferred is None:
                evac()
            else:
                deferred.append(evac)

        def alloc_norm_bufs():
            return {"sqj": [A.bf16(D) for _ in range(2)], "t1": [A.f32(D) for _ in range(2)],
                    "hb": [A.bf16(D) for _ in range(2)]}

        def phase_A(l):
            A.reset(H_BYTES)
            hT = hT_view()
            bufs = alloc_norm_bufs()
            xt = [A.f32(D) for _ in range(3)]
            mul = [A.f32(D) for _ in range(2)]
            add = [A.f32(D) for _ in range(2)]
            for r in range(2):
                load_bc(mul[r], l, r, 1, ("Amul", r))
                load_bc(add[r], l, r, 0, ("Aadd", r))
            S.op("dve", lambda e: e.memset(ssq[:], 0.0), writes=[("A", "ssq", t) for t in range(NT128)])
            dfr = []
            for t in range(NT128):
                r = 0 if t < 32 else 1
                s3 = t % 3
                S.dma("sp", ("xt", s3), lambda e, t=t, s3=s3: e.dma_start(out=xt[s3], in_=src_tile(l, t)),
                      reads=[("xs", t)], writes=[("xt", s3)])
                norm_mod_T("A", t, xt[s3], ("xt", s3), mul[r], ("Amul", r), add[r], ("Aadd", r), bufs, hT, deferred=dfr)
                while len(dfr) > 1:
                    dfr.pop(0)()
            while dfr:
                dfr.pop(0)()

        def attn_unit(KT, QT, VXh, kcs, n, scale, obank, PT, sbufs, rk, wk, tp=None):
            tpk = {"tile_position": tp} if tp is not None else {}
            groups = [kcs[i:i + 2] for i in range(0, len(kcs), 2)]
            ng = len(groups)

            def qk(g):
                sb = sbufs[g % len(sbufs)]
                w = len(groups[g])
                for i, kc in enumerate(groups[g]):
                    S.op("pe", lambda e, i=i, kc=kc: e.matmul(ps[sb + i][:, 0:n], KT[:, kc * 128:(kc + 1) * 128], QT, start=True, stop=True, **tpk),
                         reads=rk, writes=[PK(sb + i)])
                sl = g % len(PT)
                if n == 512:
                    srcap = psall[:, sb * 512:(sb + w) * 512]
                    dstap = PT[sl][:, 0:w * 512]
                else:
                    srcap = psall[:, sb * 512:(sb + w) * 512].rearrange("p (b c) -> p b c", c=512)[:, :, 0:n]
                    dstap = PT[sl][:, 0:w * 512].rearrange("p (b c) -> p b c", c=512)[:, :, 0:n]
                S.op("act", lambda e: e.activation(dstap, srcap, AF.Exp, scale=scale),
                     reads=[PK(sb + i) for i in range(w)], writes=[("PT", sl)])

            def pv(g):
                sl = g % len(PT)
                for i, kc in enumerate(groups[g]):
                    first = (g == 0 and i == 0)
                    last = (g == ng - 1 and i == len(groups[g]) - 1)
                    S.op("pe", lambda e, i=i, kc=kc, first=first, last=last: e.matmul(
                        ps[obank][:, 0:n], VXh[:, kc, :], PT[sl][:, i * 512:i * 512 + n], start=first, stop=last),
                        reads=[("PT", sl)] + rk, writes=[PK(obank)] + wk)

            qk(0)
            if ng > 1:
                qk(1)
            for g in range(ng):
                if g + 2 < ng:
                    qk(g + 2)
                pv(g)

        def attn_multi(lanes, kcs, n, scale, PT, sbufs, rk, hook=None):
            nk = len(kcs)
            L = len(lanes)

            def qk(g):
                sb = sbufs[g % len(sbufs)]
                kc = kcs[g]
                for i, (KT, QT, VXh, ob, tp) in enumerate(lanes):
                    S.op("pe", lambda e, i=i, KT=KT, QT=QT, tp=tp: e.matmul(ps[sb + i][:, 0:n], KT[:, kc * 128:(kc + 1) * 128], QT,
                                                                          start=True, stop=True, tile_position=tp),
                         reads=rk, writes=[PK(sb + i)])
                sl = g % len(PT)
                if n == 512:
                    srcap = psall[:, sb * 512:(sb + L) * 512]
                    dstap = PT[sl][:, 0:L * 512]
                else:
                    srcap = psall[:, sb * 512:(sb + L) * 512].rearrange("p (b c) -> p b c", c=512)[:, :, 0:n]
                    dstap = PT[sl][:, 0:L * 512].rearrange("p (b c) -> p b c", c=512)[:, :, 0:n]
                S.op("act", lambda e: e.activation(dstap, srcap, AF.Exp, scale=scale),
                     reads=[PK(sb + i) for i in range(L)], writes=[("PT", sl)])

            def pv(g):
                kc = kcs[g]
                sl = g % len(PT)
                for i, (KT, QT, VXh, ob, tp) in enumerate(lanes):
                    S.op("pe", lambda e, i=i, VXh=VXh, ob=ob: e.matmul(ps[ob][:, 0:n], VXh[:, kc, :], PT[sl][:, i * 512:i * 512 + n],
                                                                      start=(g == 0), stop=(g == nk - 1)),
                         reads=[("PT", sl)] + rk, writes=[PK(ob)])

            qk(0)
            if nk > 1:
                qk(1)
            hk = min(4, nk - 1)
            for g in range(nk):
                if g + 2 < nk:
                    qk(g + 2)
                pv(g)
                if hook is not None and g == hk:
                    hook()

        def q_tiles(need_ctx):
            tl = [(tt * 512, 512, list(range(NT128))) for tt in range(8)]
            if need_ctx:
                tl.append((S_LAT, 256, [32, 33]))
            return tl

        def tok_tiles():
            return [(tt * 512, 512) for tt in range(8)] + [(S_LAT, 256)]

        def load_win(c0, c1, dst, key):
            wd = w_inx_d_l[0]
            S.dma("pool", key, lambda e: e.dma_start(out=dst, in_=wd[:, c0:c1].rearrange("(k p) n -> p k n", p=128)),
                  writes=[key])

        w_inx_d_l = [None]

        def proj_fm(pbank, win, c, hT, tok0, n, wkey):
            for k in range(8):
                S.op("pe", lambda e, k=k: e.matmul(ps[pbank][:, 0:n], win[:, k, c * 128:(c + 1) * 128], hT[:, k, tok0:tok0 + n],
                                                  start=(k == 0), stop=(k == 7)),
                     reads=[wkey, "hTall"], writes=[PK(pbank)])

        def load_rope(cdst, sdst, cd, sd, tok0, n, sl):
            S.dma("sp", ("rc", sl), lambda e: e.dma_start(out=cdst[:, 0:n], in_=cd[:, tok0:tok0 + n]), writes=[("rc", sl)])
            S.dma("sp", ("rs", sl), lambda e: e.dma_start(out=sdst[:, 0:n], in_=sd[:, tok0:tok0 + n]), writes=[("rs", sl)])

        def phase_gqa(l, need_ctx):
            A.reset(H_BYTES)
            hT = hT_view()
            QG = A.bf16(4 * T).rearrange("p (c n) -> p c n", c=4)
            KG = A.bf16(2 * T).rearrange("p (c n) -> p c n", c=2)
            VXf = A.bf16(2 * NT128 * 128)
            VX = VXf.rearrange("p (h t c) -> p h t c", h=2, t=NT128)
            win = A.bf16(8 * 1664).rearrange("p (k n) -> p k n", k=8)
            rc = [A.f32(512) for _ in range(2)]
            rs = [A.f32(512) for _ in range(2)]
            sq = [A.bf16(512) for _ in range(2)]
            rr = [A.f32(512) for _ in range(2)]
            t1 = [A.f32(512) for _ in range(2)]
            t2 = [A.f32(512) for _ in range(2)]
            PT = [A.bf16(1024) for _ in range(3)]
            AO = [A.bf16(512) for _ in range(2)]
            rec = [A.f32(512) for _ in range(2)]
            osb = [A.f32(512) for _ in range(2)]
            load_win(0, 1536, win[:, :, 0:1536], "winA")
            load_win(VG_OFF, VG_OFF + 128, win[:, :, 1536:1664], "winB")
            S.op("pool", lambda e: e.memset(VXf.rearrange("p (g c) -> p g c", c=128)[:, :, 64:128], 1.0), writes=["VXones"])
            S.op("pool", lambda e: e.memset(tiny[:, 0:1], 0.0), reads=[("hT", t) for t in range(NT128)], writes=["hTall"])

            def chunk(tok0, n, sl, ci, kind, idx, cn, cs, gcol, gscol):
                b0 = (ci % 2) * 3
                u = ci % 2
                proj_fm(b0, win, cn, hT, tok0, n, "winA")
                proj_fm(b0 + 1, win, cs, hT, tok0, n, "winA")
                S.op("act", lambda e: e.activation(sq[u][:, 0:n], ps[b0][:, 0:n], AF.Square),
                     reads=[PK(b0)], writes=[("sq", u)])
                S.op("pe", lambda e: e.matmul(ps[b0 + 2][:, 0:n], bd64, sq[u][:, 0:n], start=True, stop=True),
                     reads=[("sq", u), "cst"], writes=[PK(b0 + 2)])
                S.op("act", lambda e: e.activation(rr[u][:, 0:n], ps[b0 + 2][:, 0:n], AF.Ln, bias=cvec[:, 1:1 + 1], scale=1.0), reads=[PK(b0 + 2), "cvec"], writes=[("rr", u)])
                S.op("act", lambda e: e.activation(rr[u][:, 0:n], rr[u][:, 0:n], AF.Exp, scale=-0.5), reads=[("rr", u)], writes=[("rr", u)])
                S.op("dve", lambda e: e.tensor_tensor(t1[u][:, 0:n], ps[b0][:, 0:n], rr[u][:, 0:n], ALU.mult),
                     reads=[PK(b0), ("rr", u)], writes=[("t1", u)])
                S.op("dve", lambda e: e.tensor_tensor(t2[u][:, 0:n], ps[b0 + 1][:, 0:n], rr[u][:, 0:n], ALU.mult),
                     reads=[PK(b0 + 1), ("rr", u)], writes=[("t2", u)])
                S.op("dve", lambda e: e.scalar_tensor_tensor(t1[u][:, 0:n], t1[u][:, 0:n], smallp[:, l * 8 + gcol:l * 8 + gcol + 1],
                                                             rc[sl][:, 0:n], ALU.mult, ALU.mult),
                     reads=[("t1", u), ("rc", sl), "smallp"], writes=[("t1", u)])
                S.op("dve", lambda e: e.scalar_tensor_tensor(t2[u][:, 0:n], t2[u][:, 0:n], smallp[:, l * 8 + gscol:l * 8 + gscol + 1],
                                                              rs[sl][:, 0:n], ALU.mult, ALU.mult),
                     reads=[("t2", u), ("rs", sl), "smallp"], writes=[("t2", u)])
                dst = QG[:, idx, tok0:tok0 + n] if kind == "q" else KG[:, idx, tok0:tok0 + n]
                S.op("pool", lambda e: e.tensor_tensor(dst, t1[u][:, 0:n], t2[u][:, 0:n], ALU.add),
                     reads=[("t1", u), ("t2", u)], writes=[("QK", kind, idx, tok0)])

            for it, (tok0, n) in enumerate(tok_tiles()):
                is_ctx = tok0 >= S_LAT
                sl = it % 2
                load_rope(rc[sl], rs[sl], ropeA_c_d, ropeA_s_d, tok0, n, sl)
                chunks = []
                if (not is_ctx) or need_ctx:
                    chunks += [("q", c, c, 4 + c, 0, 1) for c in range(4)]
                chunks += [("k", kv, 8 + kv, 10 + kv, 2, 3) for kv in range(2)]
                for ci, (kind, idx, cn, cs, gcol, gscol) in enumerate(chunks):
                    chunk(tok0, n, sl, ci, kind, idx, cn, cs, gcol, gscol)

            def vtile(t):
                b = 6 + t % 2
                for k in range(8):
                    S.op("pe", lambda e, k=k: e.matmul(ps[b][:, 0:128], hT[:, k, t * 128:(t + 1) * 128], win[:, k, 1536:1664],
                                                      start=(k == 0), stop=(k == 7)),
                         reads=["winB", "hTall"], writes=[PK(b)])
                S.op("dve", lambda e: e.tensor_copy(VX[:, :, t, 0:64], ps[b][:, 0:128].rearrange("p (h c) -> p h c", h=2)),
                     reads=[PK(b), "VXones"], writes=[("VX", t)])
            for t in range(NT128):
                vtile(t)
            S.op("pool", lambda e: e.memset(tiny[:, 1:2], 0.0),
                 reads=[("VX", t) for t in range(NT128)] + [("QK", "k", kv, tk) for kv in range(2) for (tk, _n) in tok_tiles()],
                 writes=["KVall"])

            def unit2(c, kv, tok0, n, kcs, a):
                lanes = []
                for hh in range(2):
                    rsl = slice(hh * 64, hh * 64 + 64)
                    lanes.append((KG[rsl, kv, :], QG[rsl, c, tok0:tok0 + n], VX[:, kv], 6 + hh, (hh * 64, 0)))
                attn_multi(lanes, kcs, n, 8.0, PT, [0, 2, 4], ["KVall", ("QK", "q", c, tok0)])
                for hh in range(2):
                    ob = 6 + hh
                    rsl = slice(hh * 64, hh * 64 + 64)
                    S.op("dve", lambda e, ob=ob, hh=hh: e.tensor_copy(rec[hh][0:64, 0:n], ps[ob][64:128, 0:n]),
                         reads=[PK(ob)], writes=[("rec", hh)])
                    S.op("dve", lambda e, ob=ob, hh=hh: e.tensor_copy(osb[hh][0:64, 0:n], ps[ob][0:64, 0:n]),
                         reads=[PK(ob)], writes=[("osb", hh)])
                for hh in range(2):
                    rsl = slice(hh * 64, hh * 64 + 64)
                    S.op("dve", lambda e, hh=hh: e.reciprocal(rec[hh][0:64, 0:n], rec[hh][0:64, 0:n]),
                         reads=[("rec", hh)], writes=[("rec", hh)])
                    S.op("dve", lambda e, hh=hh, rsl=rsl: e.tensor_tensor(AO[a][rsl, 0:n], osb[hh][0:64, 0:n], rec[hh][0:64, 0:n], ALU.mult),
                         reads=[("osb", hh), ("rec", hh)], writes=[("AO", a, hh)])

            def store(a, cidx, tok0, n):
                S.dma("sp", ("AOst", a), lambda e: e.dma_start(out=ao_d[cidx, :, tok0:tok0 + n], in_=AO[a][:, 0:n]),
                      reads=[("AO", a, 0), ("AO", a, 1)], writes=[("ao", cidx, tok0)])
            un = 0
            for c in range(4):
                for qi, (tok0, n, kcs) in enumerate(q_tiles(need_ctx)):
                    a = (c + qi) % 2
                    unit2(c, c // 2, tok0, n, kcs, a)
                    store(a, c, tok0, n)

        def phase_diff(l, need_ctx):
            lam_init = 0.8 - 0.6 * math.exp(-0.3 * l)
            A.reset(H_BYTES)
            hT = hT_view()
            QD = A.bf16(2 * T).rearrange("p (c n) -> p c n", c=2)
            KD = A.bf16(2 * T).rearrange("p (c n) -> p c n", c=2)
            VXf = A.bf16(4 * NT128 * 128)
            VX = VXf.rearrange("p (h t c) -> p h t c", h=4, t=NT128)
            win = A.bf16(8 * 1280).rearrange("p (k n) -> p k n", k=8)
            rc = [A.f32(512) for _ in range(2)]
            rs = [A.f32(512) for _ in range(2)]
            t1 = [A.f32(512) for _ in range(2)]
            t2 = [A.f32(512) for _ in range(2)]
            PT = [A.bf16(1024) for _ in range(3)]
            AO = [A.bf16(512) for _ in range(2)]
            rec = [A.f32(512) for _ in range(2)]
            dd = [A.f32(512) for _ in range(2)]
            sq = [A.bf16(512) for _ in range(2)]
            full = [rec[0], rec[1], dd[0], dd[1]]
            PT4 = [A.bf16(2048) for _ in range(3)]
            load_win(12 * 128, 20 * 128, win[:, :, 0:1024], "winA")
            load_win(VD_OFF, VD_OFF + 256, win[:, :, 1024:1280], "winB")
            S.op("pool", lambda e: e.memset(VXf.rearrange("p (g c) -> p g c", c=128)[:, :, 64:128], 1.0), writes=["VXones"])
            S.op("pool", lambda e: e.memset(tiny[:, 0:1], 0.0), reads=[("hT", t) for t in range(NT128)], writes=["hTall"])
            S.dma("sp", "lamv", lambda e: e.dma_start(out=lamt[:, 0:128], in_=lamv_d[l].partition_broadcast(128)), writes=["lamt"])
            S.op("dve", lambda e: e.tensor_tensor(lamt[:, 128:160], lamt[:, 0:32], lamt[:, 32:64], ALU.mult), reads=["lamt"], writes=["lam1"])
            S.op("dve", lambda e: e.tensor_reduce(tiny[:, 8:9], lamt[:, 128:160], AX.X, ALU.add), reads=["lam1"], writes=["lams1"])
            S.op("dve", lambda e: e.tensor_tensor(lamt[:, 128:160], lamt[:, 64:96], lamt[:, 96:128], ALU.mult), reads=["lamt", "lams1"], writes=["lam1"])
            S.op("dve", lambda e: e.tensor_reduce(tiny[:, 9:10], lamt[:, 128:160], AX.X, ALU.add), reads=["lam1"], writes=["lams2"])
            S.op("act", lambda e: e.activation(tiny[:, 10:12], tiny[:, 8:10], AF.Exp), reads=["lams1", "lams2"], writes=["lame"])
            S.op("dve", lambda e: e.tensor_tensor(tiny[:, 12:13], tiny[:, 11:12], tiny[:, 10:11], ALU.subtract), reads=["lame"], writes=["lamd"])
            S.op("dve", lambda e: e.tensor_scalar(tiny[:, 13:14], tiny[:, 12:13], -lam_init, 0.0, ALU.add, ALU.add), reads=["lamd"], writes=["neglam"])
            S.op("dve", lambda e: e.tensor_scalar(tiny[:, 14:15], smallp[:, l * 8 + 4:l * 8 + 5], 8.0 * (1.0 - lam_init), 0.0, ALU.mult, ALU.add),
                 reads=["smallp"], writes=["gs8"])

            def chunk(tok0, n, sl, ci, kind, idx, cn, cs):
                b0 = (ci % 2) * 2
                u = ci % 2
                proj_fm(b0, win, cn, hT, tok0, n, "winA")
                proj_fm(b0 + 1, win, cs, hT, tok0, n, "winA")
                S.op("dve", lambda e: e.tensor_tensor(t1[u][:, 0:n], ps[b0][:, 0:n], rc[sl][:, 0:n], ALU.mult),
                     reads=[PK(b0), ("rc", sl)], writes=[("t1", u)])
                S.op("dve", lambda e: e.tensor_tensor(t2[u][:, 0:n], ps[b0 + 1][:, 0:n], rs[sl][:, 0:n], ALU.mult),
                     reads=[PK(b0 + 1), ("rs", sl)], writes=[("t2", u)])
                dst = QD[:, idx, tok0:tok0 + n] if kind == "q" else KD[:, idx, tok0:tok0 + n]
                S.op("pool", lambda e: e.tensor_tensor(dst, t1[u][:, 0:n], t2[u][:, 0:n], ALU.add),
                     reads=[("t1", u), ("t2", u)], writes=[("QK", kind, idx, tok0)])

            for it, (tok0, n) in enumerate(tok_tiles()):
                is_ctx = tok0 >= S_LAT
                sl = it % 2
                load_rope(rc[sl], rs[sl], ropeB_c_d, ropeB_s_d, tok0, n, sl)
                chunks = []
                if (not is_ctx) or need_ctx:
                    chunks += [("q", c, c, 2 + c) for c in range(2)]
                chunks += [("k", c, 4 + c, 6 + c) for c in range(2)]
                for ci, (kind, idx, cn, cs) in enumerate(chunks):
                    chunk(tok0, n, sl, ci, kind, idx, cn, cs)

            def vtile(t):
                b = 6 + t % 2
                for k in range(8):
                    S.op("pe", lambda e, k=k: e.matmul(ps[b][:, 0:256], hT[:, k, t * 128:(t + 1) * 128], win[:, k, 1024:1280],
                                                      start=(k == 0), stop=(k == 7)),
                         reads=["winB", "hTall"], writes=[PK(b)])
                S.op("dve", lambda e: e.tensor_copy(VX[:, :, t, 0:64], ps[b][:, 0:256].rearrange("p (h c) -> p h c", h=4)),
                     reads=[PK(b), "VXones"], writes=[("VX", t)])
            for t in range(NT128):
                vtile(t)
            S.op("pool", lambda e: e.memset(tiny[:, 1:2], 0.0),
                 reads=[("VX", t) for t in range(NT128)] + [("QK", "k", c, tk) for c in range(2) for (tk, _n) in tok_tiles()],
                 writes=["KVall"])

            def post_a(n):
                for i in range(4):
                    S.op("dve", lambda e, i=i: e.tensor_copy(full[i][:, 0:n], ps[4 + i][:, 0:n]), reads=[PK(4 + i)], writes=[("full", i)])
                for hh in range(2):
                    f0, f1 = full[2 * hh], full[2 * hh + 1]
                    k0, k1 = ("full", 2 * hh), ("full", 2 * hh + 1)
                    S.op("dve", lambda e, hh=hh, f0=f0: e.reciprocal(t1[hh][0:64, 0:n], f0[64:128, 0:n]), reads=[k0], writes=[("t1", hh)])
                    S.op("dve", lambda e, hh=hh, f1=f1: e.reciprocal(t2[hh][0:64, 0:n], f1[64:128, 0:n]), reads=[k1], writes=[("t2", hh)])
                    S.op("pool", lambda e, hh=hh, f0=f0: e.tensor_tensor(f0[0:64, 0:n], f0[0:64, 0:n], t1[hh][0:64, 0:n], ALU.mult),
                         reads=[k0, ("t1", hh)], writes=[k0])
                    S.op("pool", lambda e, hh=hh, f1=f1: e.tensor_tensor(f1[0:64, 0:n], f1[0:64, 0:n], t2[hh][0:64, 0:n], ALU.mult),
                         reads=[k1, ("t2", hh)], writes=[k1])
                    S.op("dve", lambda e, hh=hh, f0=f0, f1=f1: e.scalar_tensor_tensor(t1[hh][0:64, 0:n], f1[0:64, 0:n], tiny[0:64, 13:14], f0[0:64, 0:n],
                                                                                   ALU.mult, ALU.add),
                         reads=[k0, k1, "neglam"], writes=[("t1", hh)])

            def post_b(n, a, cidx, tok0):
                for hh in range(2):
                    S.op("act", lambda e, hh=hh: e.activation(sq[hh][0:64, 0:n], t1[hh][0:64, 0:n], AF.Square), reads=[("t1", hh)], writes=[("sq", hh)])
                for hh in range(2):
                    S.op("pe", lambda e, hh=hh: e.matmul(ps[hh][0:64, 0:n], ones[0:64, 0:64], sq[hh][0:64, 0:n], start=True, stop=True),
                         reads=[("sq", hh), "cst"], writes=[PK(hh)])
                    S.op("act", lambda e, hh=hh: e.activation(t2[hh][0:64, 0:n], ps[hh][0:64, 0:n], AF.Ln, bias=cvec[0:64, 1:2], scale=1.0),
                         reads=[PK(hh), "cvec"], writes=[("t2", hh)])
                    S.op("act", lambda e, hh=hh: e.activation(t2[hh][0:64, 0:n], t2[hh][0:64, 0:n], AF.Exp, scale=-0.5), reads=[("t2", hh)], writes=[("t2", hh)])
                    S.op("dve", lambda e, hh=hh: e.scalar_tensor_tensor(AO[a][hh * 64:hh * 64 + 64, 0:n], t1[hh][0:64, 0:n], tiny[0:64, 14:15],
                                                                        t2[hh][0:64, 0:n], ALU.mult, ALU.mult),
                         reads=[("t1", hh), ("t2", hh), "gs8"], writes=[("AO", a, hh)])
                S.dma("sp", ("AOst", a), lambda e: e.dma_start(out=ao_d[cidx, :, tok0:tok0 + n], in_=AO[a][:, 0:n]),
                      reads=[("AO", a, 0), ("AO", a, 1)], writes=[("ao", cidx, tok0)])

            pending = []

            def flush():
                while pending:
                    pending.pop(0)()

            def unit4(c, tok0, n, kcs, a):
                lanes = []
                for hh in range(2):
                    for j in range(2):
                        rb = hh * 64 + j * 32
                        lanes.append((KD[rb:rb + 32, c, :], QD[rb:rb + 32, c, tok0:tok0 + n], VX[:, 2 * c + hh], 4 + 2 * hh + j, (rb, 0)))
                attn_multi(lanes, kcs, n, 32.0 ** -0.5, PT4, [0], ["KVall", ("QK", "q", c, tok0)], hook=flush)
                flush()
                post_a(n)
                pending.append(lambda: post_b(n, a, 4 + c, tok0))

            ui = 0
            for c in range(2):
                for (tok0, n, kcs) in q_tiles(need_ctx):
                    unit4(c, tok0, n, kcs, ui % 2)
                    ui += 1
            flush()

        def phase_mla(l, need_ctx):
            A.reset(H_BYTES)
            hT = hT_view()
            cqnA = A.bf16(T)
            cqnB = A.bf16(T)
            ckvn = A.bf16(T)
            krr = A.bf16(T)
            VXf = A.bf16(4 * NT128 * 128)
            VX = VXf.rearrange("p (h t c) -> p h t c", h=4, t=NT128)
            win = A.bf16(8 * 512).rearrange("p (k n) -> p k n", k=8)
            wuq = A.bf16(2 * 384).rearrange("p (k n) -> p k n", k=2)
            wuqs = A.bf16(2 * 384).rearrange("p (k n) -> p k n", k=2)
            wkk = A.bf16(256)
            wkv = A.bf16(256)
            rc = [A.f32(512) for _ in range(2)]
            rs = [A.f32(512) for _ in range(2)]
            sq = [A.bf16(512) for _ in range(2)]
            rr = [A.f32(512) for _ in range(2)]
            t1 = [A.f32(512) for _ in range(2)]
            t2 = [A.f32(512) for _ in range(2)]
            PT = [A.bf16(1024) for _ in range(3)]
            AO = [A.bf16(512) for _ in range(2)]
            rec = [A.f32(512) for _ in range(2)]
            osb = [A.f32(512) for _ in range(2)]
            load_win(20 * 128, 24 * 128, win, "winA")
            S.dma("pool", "wuq0", lambda e: e.dma_start(out=wuq[:, 0, :], in_=w_uq_d[l][0:128, :]), writes=["wuq0"])
            S.dma("pool", "wuq1", lambda e: e.dma_start(out=wuq[0:64, 1, :], in_=w_uq_d[l][128:192, :]), writes=["wuq1"])
            S.dma("pool", "wuqs0", lambda e: e.dma_start(out=wuqs[:, 0, :], in_=w_uqs_d[l][0:128, :]), writes=["wuqs0"])
            S.dma("pool", "wuqs1", lambda e: e.dma_start(out=wuqs[0:64, 1, :], in_=w_uqs_d[l][128:192, :]), writes=["wuqs1"])
            S.dma("pool", "wkk", lambda e: e.dma_start(out=wkk, in_=w_ukvk_d[l]), writes=["wkk"])
            S.dma("pool", "wkv", lambda e: e.dma_start(out=wkv, in_=w_ukvv_d[l]), writes=["wkv"])
            WUQ = ["wuq0", "wuq1", "wuqs0", "wuqs1"]
            S.op("pool", lambda e: e.memset(VXf.rearrange("p (g c) -> p g c", c=128)[:, :, 64:128], 1.0), writes=["VXones"])
            S.op("pool", lambda e: e.memset(tiny[:, 0:1], 0.0), reads=[("hT", t) for t in range(NT128)], writes=["hTall"])
            S.op("dve", lambda e: e.tensor_scalar(tiny[:, 16:18], smallp[:, l * 8 + 5:l * 8 + 7], math.sqrt(192.0), 0.0, ALU.mult, ALU.add),
                 reads=["smallp"], writes=["gcq"])
            S.op("dve", lambda e: e.tensor_scalar(tiny[:, 18:19], smallp[:, l * 8 + 7:l * 8 + 8], math.sqrt(128.0), 0.0, ALU.mult, ALU.add),
                 reads=["smallp"], writes=["gckv"])

            def ptile(tok0, n, sl):
                load_rope(rc[sl], rs[sl], ropeB_c_d, ropeB_s_d, tok0, n, sl)
                proj_fm(0, win, 0, hT, tok0, n, "winA")
                proj_fm(1, win, 1, hT, tok0, n, "winA")
                proj_fm(2, win, 2, hT, tok0, n, "winA")
                proj_fm(3, win, 3, hT, tok0, n, "winA")
                S.op("act", lambda e: e.activation(sq[0][:, 0:n], ps[0][:, 0:n], AF.Square), reads=[PK(0)], writes=[("sq", 0)])
                S.op("act", lambda e: e.activation(sq[1][0:64, 0:n], ps[1][0:64, 0:n], AF.Square), reads=[PK(1)], writes=[("sq", 1)])
                S.op("pe", lambda e: e.matmul(ps[4][:, 0:n], ones, sq[0][:, 0:n], start=True, stop=False), reads=[("sq", 0), "cst"], writes=[PK(4)])
                S.op("pe", lambda e: e.matmul(ps[4][:, 0:n], ones[0:64, :], sq[1][0:64, 0:n], start=False, stop=True), reads=[("sq", 1), "cst"], writes=[PK(4)])
                S.op("act", lambda e: e.activation(rr[0][:, 0:n], ps[4][:, 0:n], AF.Ln, bias=cvec[:, 2:2 + 1], scale=1.0), reads=[PK(4), "cvec"], writes=[("rr", 0)])
                S.op("act", lambda e: e.activation(rr[0][:, 0:n], rr[0][:, 0:n], AF.Exp, scale=-0.5), reads=[("rr", 0)], writes=[("rr", 0)])
                S.op("dve", lambda e: e.scalar_tensor_tensor(cqnA[:, tok0:tok0 + n], ps[0][:, 0:n], tiny[:, 16:17], rr[0][:, 0:n], ALU.mult, ALU.mult),
                     reads=[PK(0), ("rr", 0), "gcq"], writes=[("cqnA", tok0)])
                S.op("dve", lambda e: e.scalar_tensor_tensor(cqnB[0:64, tok0:tok0 + n], ps[1][0:64, 0:n], tiny[0:64, 17:18], rr[0][0:64, 0:n], ALU.mult, ALU.mult),
                     reads=[PK(1), ("rr", 0), "gcq"], writes=[("cqnB", tok0)])
                S.op("act", lambda e: e.activation(sq[0][:, 0:n], ps[3][:, 0:n], AF.Square), reads=[PK(3)], writes=[("sq", 0)])
                S.op("pe", lambda e: e.matmul(ps[5][:, 0:n], ones, sq[0][:, 0:n], start=True, stop=True), reads=[("sq", 0), "cst"], writes=[PK(5)])
                S.op("act", lambda e: e.activation(rr[1][:, 0:n], ps[5][:, 0:n], AF.Ln, bias=cvec[:, 3:3 + 1], scale=1.0), reads=[PK(5), "cvec"], writes=[("rr", 1)])
                S.op("act", lambda e: e.activation(rr[1][:, 0:n], rr[1][:, 0:n], AF.Exp, scale=-0.5), reads=[("rr", 1)], writes=[("rr", 1)])
                S.op("dve", lambda e: e.scalar_tensor_tensor(ckvn[:, tok0:tok0 + n], ps[3][:, 0:n], tiny[:, 18:19], rr[1][:, 0:n], ALU.mult, ALU.mult),
                     reads=[PK(3), ("rr", 1), "gckv"], writes=[("ckvn", tok0)])
                S.op("dve", lambda e: e.tensor_tensor(t1[0][64:96, 0:n], ps[1][64:96, 0:n], rc[sl][64:96, 0:n], ALU.mult),
                     reads=[PK(1), ("rc", sl)], writes=[("t1", 0)])
                S.op("dve", lambda e: e.tensor_tensor(t2[0][64:96, 0:n], ps[2][64:96, 0:n], rs[sl][64:96, 0:n], ALU.mult),
                     reads=[PK(2), ("rs", sl)], writes=[("t2", 0)])
                S.op("pool", lambda e: e.tensor_tensor(krr[64:96, tok0:tok0 + n], t1[0][64:96, 0:n], t2[0][64:96, 0:n], ALU.add),
                     reads=[("t1", 0), ("t2", 0)], writes=[("krr", tok0)])
            for it, (tok0, n) in enumerate(tok_tiles()):
                ptile(tok0, n, it % 2)

            def vtile(t):
                b = 6 + t % 2
                S.op("pe", lambda e: e.matmul(ps[b][:, 0:256], ckvn[:, t * 128:(t + 1) * 128], wkv, start=True, stop=True),
                     reads=["wkv", ("ckvn", (t // 4) * 512)], writes=[PK(b)])
                S.op("dve", lambda e: e.tensor_copy(VX[:, :, t, 0:64], ps[b][:, 0:256].rearrange("p (h c) -> p h c", h=4)),
                     reads=[PK(b), "VXones"], writes=[("VX", t)])
            for t in range(NT128):
                vtile(t)
            S.barrier()
            QM = A.t[:, 0:H_BYTES // 8].bitcast(BF16).rearrange("p (h n) -> p h n", h=4)
            KM = A.t[:, H_BYTES // 8:H_BYTES // 4].bitcast(BF16).rearrange("p (h n) -> p h n", h=4)

            def uhead(tok0, n, sl, h, doq):
                u = h % 2
                if doq:
                    for (bank, w) in ((2 * u, wuq), (2 * u + 1, wuqs)):
                        S.op("pe", lambda e, bank=bank, w=w: e.matmul(ps[bank][0:96, 0:n], w[:, 0, h * 96:(h + 1) * 96], cqnA[:, tok0:tok0 + n],
                                                                     start=True, stop=False),
                             reads=WUQ, writes=[PK(bank)])
                        S.op("pe", lambda e, bank=bank, w=w: e.matmul(ps[bank][0:96, 0:n], w[0:64, 1, h * 96:(h + 1) * 96], cqnB[0:64, tok0:tok0 + n],
                                                                     start=False, stop=True),
                             reads=WUQ, writes=[PK(bank)])
                    S.op("act", lambda e: e.activation(QM[0:64, h, tok0:tok0 + n], ps[2 * u][0:64, 0:n], AF.Copy),
                         reads=[PK(2 * u)], writes=[("QMn", h, tok0)])
                    S.op("dve", lambda e: e.tensor_tensor(t1[u][64:96, 0:n], ps[2 * u][64:96, 0:n], rc[sl][64:96, 0:n], ALU.mult),
                         reads=[PK(2 * u), ("rc", sl)], writes=[("t1", u)])
                    S.op("dve", lambda e: e.tensor_tensor(t2[u][64:96, 0:n], ps[2 * u + 1][64:96, 0:n], rs[sl][64:96, 0:n], ALU.mult),
                         reads=[PK(2 * u + 1), ("rs", sl)], writes=[("t2", u)])
                    S.op("pool", lambda e: e.tensor_tensor(QM[64:96, h, tok0:tok0 + n], t1[u][64:96, 0:n], t2[u][64:96, 0:n], ALU.add),
                         reads=[("t1", u), ("t2", u)], writes=[("QMr", h, tok0)])
                kb = 4 + u
                S.op("pe", lambda e: e.matmul(ps[kb][0:64, 0:n], wkk[:, h * 64:(h + 1) * 64], ckvn[:, tok0:tok0 + n], start=True, stop=True),
                     reads=["wkk"], writes=[PK(kb)])
                S.op("dve", lambda e: e.tensor_copy(KM[0:64, h, tok0:tok0 + n], ps[kb][0:64, 0:n]),
                     reads=[PK(kb)], writes=[("KMn", h, tok0)])
                S.op("pool", lambda e: e.tensor_copy(KM[64:96, h, tok0:tok0 + n], krr[64:96, tok0:tok0 + n]),
                     writes=[("KMr", h, tok0)])
            for it, (tok0, n) in enumerate(tok_tiles()):
                is_ctx = tok0 >= S_LAT
                sl = it % 2
                load_rope(rc[sl], rs[sl], ropeB_c_d, ropeB_s_d, tok0, n, sl)
                for h in range(4):
                    uhead(tok0, n, sl, h, (not is_ctx) or need_ctx)
            S.op("pool", lambda e: e.memset(tiny[:, 1:2], 0.0),
                 reads=[(kk, h, tk) for kk in ("KMn", "KMr") for h in range(4) for (tk, _n) in tok_tiles()],
                 writes=["KVall"])

            def unit(c, hh, tok0, n, kcs, ob, u, a):
                h = 2 * c + hh
                attn_unit(KM[0:96, h, :], QM[0:96, h, tok0:tok0 + n], VX[:, h], kcs, n, 96.0 ** -0.5, ob, PT, [0, 2, 4],
                          ["KVall", ("QMn", h, tok0), ("QMr", h, tok0)], [])
                S.op("dve", lambda e: e.tensor_copy(rec[u][0:64, 0:n], ps[ob][64:128, 0:n]),
                     reads=[PK(ob)], writes=[("rec", u)])
                S.op("dve", lambda e: e.tensor_copy(osb[u][0:64, 0:n], ps[ob][0:64, 0:n]),
                     reads=[PK(ob)], writes=[("osb", u)])
                S.op("dve", lambda e: e.reciprocal(rec[u][0:64, 0:n], rec[u][0:64, 0:n]),
                         reads=[("rec", u)], writes=[("rec", u)])
                S.op("dve", lambda e: e.tensor_tensor(AO[a][hh * 64:hh * 64 + 64, 0:n], osb[u][0:64, 0:n], rec[u][0:64, 0:n], ALU.mult),
                     reads=[("osb", u), ("rec", u)], writes=[("AO", a, hh)])

            def store(a, cidx, tok0, n):
                S.dma("sp", ("AOst", a), lambda e: e.dma_start(out=ao_d[cidx, :, tok0:tok0 + n], in_=AO[a][:, 0:n]),
                      reads=[("AO", a, 0), ("AO", a, 1)], writes=[("ao", cidx, tok0)])
            un = 0
            for c in range(2):
                for qi, (tok0, n, kcs) in enumerate(q_tiles(need_ctx)):
                    a = (c + qi) % 2
                    for hh in range(2):
                        unit(c, hh, tok0, n, kcs, 6 + un % 2, un % 2, a)
                        un += 1
                    store(a, 6 + c, tok0, n)

        def phase_C(l, need_ctx, moe):
            A.reset(H_BYTES)
            hT = hT_view()
            wout = A.bf16(8 * D).rearrange("p (k n) -> p k n", k=8)
            bufs = alloc_norm_bufs()
            xt = [A.f32(D) for _ in range(2)]
            xn = [A.f32(D) for _ in range(2)]
            aot = [A.bf16(8 * 128).rearrange("p (k n) -> p k n", k=8) for _ in range(2)]
            nr = 2 if need_ctx else 1
            G2 = [A.f32(D) for _ in range(nr)]
            Fm = [A.f32(D) for _ in range(nr)]
            Fa = [A.f32(D) for _ in range(nr)]
            S.dma("pool", "wout", lambda e: e.dma_start(out=wout, in_=w_out_d[l].rearrange("(k p) n -> p k n", p=128)), writes=["wout"])
            for r in range(nr):
                load_bc(G2[r], l, r, 2, ("G2", r))
                load_bc(Fm[r], l, r, 4, ("Fm", r))
                load_bc(Fa[r], l, r, 3, ("Fa", r))
            wr = lg = None
            if moe:
                wr = A.bf16(64).rearrange("p (k n) -> p k n", k=8)
                lg = A.f32(8)
                S.dma("pool", "wr", lambda e: e.dma_start(out=wr, in_=moe_router_d[0].rearrange("(k p) n -> p k n", p=128)), writes=["wr"])
            S.op("dve", lambda e: e.memset(ssq[:], 0.0), writes=[("C", "ssq", t) for t in range(NT128)])
            ntl = NT128 if need_ctx else 32
            g3 = gates[:].rearrange("p (t e) -> p t e", e=8)
            tn = tiny
            dfr = []

            def cload(t):
                sl = t % 2
                S.dma("sp", ("aot", sl), lambda e: e.dma_start(out=aot[sl], in_=ao_d[:, :, t * 128:(t + 1) * 128].rearrange("c p n -> p c n")),
                      reads=[("ao", c, (t // 4) * 512) for c in range(8)], writes=[("aot", sl)])
                S.dma("sp", ("xt", sl), lambda e: e.dma_start(out=xt[sl], in_=src_tile(l, t)), reads=[("xs", t)], writes=[("xt", sl)])

            def ctile(t):
                r = 0 if t < 32 else 1
                sl = t % 2
                for half in range(2):
                    b = 2 * sl + half
                    for k in range(8):
                        S.op("pe", lambda e, b=b, k=k, half=half: e.matmul(ps[b][:, :], aot[sl][:, k, :], wout[:, k, half * 512:(half + 1) * 512],
                                                                         start=(k == 0), stop=(k == 7)),
                             reads=[("aot", sl), "wout"], writes=[PK(b)])
                    S.op("dve", lambda e, b=b, half=half: e.tensor_tensor(xn[sl][:, half * 512:(half + 1) * 512], ps[b][:, :],
                                                                        G2[r][:, half * 512:(half + 1) * 512], ALU.mult),
                         reads=[PK(b), ("G2", r)], writes=[("xn", sl)])
                S.op("pool", lambda e: e.tensor_tensor(xn[sl], xn[sl], xt[sl], ALU.add),
                     reads=[("xn", sl), ("xt", sl)], writes=[("xn", sl)])
                S.dma("sp", ("xst", sl), lambda e: e.dma_start(out=xs_d[t * 128:(t + 1) * 128, :], in_=xn[sl]),
                      reads=[("xn", sl)], writes=[("xs", t)])
                norm_mod_T("C", t, xn[sl], ("xn", sl), Fm[r], ("Fm", r), Fa[r], ("Fa", r), bufs, hT, deferred=dfr)
                if moe:
                    dfr.append(lambda: router(t))

            def router(t):
                if True:
                    for k in range(8):
                        S.op("pe", lambda e, k=k: e.matmul(ps[4][:, 0:8], hT[:, k, t * 128:(t + 1) * 128], wr[:, k, :], start=(k == 0), stop=(k == 7)),
                             reads=[("hT", t), "wr"], writes=[PK(4)])
                    S.op("dve", lambda e: e.tensor_copy(lg, ps[4][:, 0:8]), reads=[PK(4)], writes=["lg"])
                    S.op("dve", lambda e: e.tensor_reduce(tn[:, 20:21], lg, AX.X, ALU.max), reads=["lg"], writes=["m1"])
                    S.op("dve", lambda e: e.tensor_scalar(tn[:, 24:32], lg, tn[:, 20:21], 0.0, ALU.is_equal, ALU.add), reads=["lg", "m1"], writes=["eq1"])
                    S.op("dve", lambda e: e.scalar_tensor_tensor(tn[:, 32:40], tn[:, 24:32], -1e30, lg, ALU.mult, ALU.add), reads=["eq1", "lg"], writes=["lg2"])
                    S.op("dve", lambda e: e.tensor_reduce(tn[:, 21:22], tn[:, 32:40], AX.X, ALU.max), reads=["lg2"], writes=["m2"])
                    S.op("dve", lambda e: e.tensor_scalar(tn[:, 40:48], tn[:, 32:40], tn[:, 21:22], 0.0, ALU.is_equal, ALU.add), reads=["lg2", "m2"], writes=["eq2"])
                    S.op("dve", lambda e: e.tensor_tensor(tn[:, 22:23], tn[:, 21:22], tn[:, 20:21], ALU.subtract), reads=["m1", "m2"], writes=["dm"])
                    S.op("act", lambda e: e.activation(tn[:, 23:24], tn[:, 22:23], AF.Exp), reads=["dm"], writes=["edm"])
                    S.op("dve", lambda e: e.tensor_scalar(tn[:, 48:49], tn[:, 23:24], 1.0, 0.0, ALU.add, ALU.add), reads=["edm"], writes=["den"])
                    S.op("dve", lambda e: e.reciprocal(tn[:, 49:50], tn[:, 48:49]), reads=["den"], writes=["w1"])
                    S.op("dve", lambda e: e.tensor_tensor(tn[:, 50:51], tn[:, 23:24], tn[:, 49:50], ALU.mult), reads=["edm", "w1"], writes=["w2"])
                    S.op("dve", lambda e: e.tensor_scalar(tn[:, 52:60], tn[:, 24:32], tn[:, 49:50], 0.0, ALU.mult, ALU.add), reads=["eq1", "w1"], writes=["g1"])
                    S.op("dve", lambda e: e.scalar_tensor_tensor(g3[:, t, :], tn[:, 40:48], tn[:, 50:51], tn[:, 52:60], ALU.mult, ALU.add),
                         reads=["eq2", "w2", "g1"], writes=[("gates", t)])
            nkeep = 2 if moe else 1
            cload(0)
            for t in range(ntl):
                if t + 1 < ntl:
                    cload(t + 1)
                ctile(t)
                while len(dfr) > nkeep:
                    dfr.pop(0)()
            while dfr:
                dfr.pop(0)()

        def phase_D(l, need_ctx, moe):
            ntok = T if need_ctx else S_LAT
            A.reset(H_BYTES)
            hT = hT_view()
            if moe:
                units = [(moe_w_in_d[0, e][:, 0:1792], moe_w_in_d[0, e][:, 1792:3584], moe_w_out_d[0, e], e) for e in range(8)]
                nf = 14
            else:
                units = [(ffn_w_in_d[0][:, f0:f0 + 1408], ffn_w_in_d[0][:, 2816 + f0:2816 + f0 + 1408], ffn_w_out_d[0][f0:f0 + 1408, :], None)
                         for f0 in (0, 1408)]
                nf = 11
            wg = A.bf16(8 * nf * 128).rearrange("p (k n) -> p k n", k=8)
            wu = A.bf16(8 * nf * 128).rearrange("p (k n) -> p k n", k=8)
            wo = A.bf16(nf * D).rearrange("p (j n) -> p j n", j=nf)
            aT = [A.bf16(nf * 512).rearrange("p (j n) -> p j n", j=nf) for _ in range(2)]
            xt = [A.f32(D) for _ in range(2)]
            t1 = [A.f32(D) for _ in range(2)]
            sg = [A.f32(512) for _ in range(2)]
            nr = 2 if need_ctx else 1
            G5 = [A.f32(D) for _ in range(nr)]
            for r in range(nr):
                load_bc(G5[r], l, r, 5, ("G5", r))
            g3 = gates[:].rearrange("p (t e) -> p t e", e=8)
            tiles = [(tt * 512, 512) for tt in range(8)] + ([(S_LAT, 256)] if need_ctx else [])
            S.op("pool", lambda e: e.memset(tiny[:, 0:1], 0.0), reads=[("hT", t) for t in range(ntok // 128)], writes=["hTall"])

            def fchunk(tok0, n, asl, j):
                u = j % 2
                S.begin_group()
                for k in range(8):
                    S.op("pe", lambda e, k=k: e.matmul(ps[u][:, 0:n], wg[:, k, j * 128:(j + 1) * 128], hT[:, k, tok0:tok0 + n],
                                                      start=(k == 0), stop=(k == 7)),
                         reads=["wg", "hTall"], writes=[PK(u)])
                for k in range(8):
                    S.op("pe", lambda e, k=k: e.matmul(ps[2 + u][:, 0:n], wu[:, k, j * 128:(j + 1) * 128], hT[:, k, tok0:tok0 + n],
                                                      start=(k == 0), stop=(k == 7)),
                         reads=["wu", "hTall"], writes=[PK(2 + u)])
                S.end_group()
                S.op("act", lambda e: e.activation(sg[u][:, 0:n], ps[u][:, 0:n], AF.Silu), reads=[PK(u)], writes=[("sg", u)])
                S.op("dve", lambda e: e.tensor_tensor(aT[asl][:, j, 0:n], sg[u][:, 0:n], ps[2 + u][:, 0:n], ALU.mult),
                     reads=[("sg", u), PK(2 + u)], writes=[("aT", asl, j)])

            def ytile(tok0, asl, sub, eidx):
                t = tok0 // 128 + sub
                r = 0 if t < 32 else 1
                xsl = t % 2
                S.dma("sp", ("xt", xsl), lambda e: e.dma_start(out=xt[xsl], in_=xs_d[t * 128:(t + 1) * 128, :]),
                      reads=[("xs", t)], writes=[("xt", xsl)])
                S.begin_group()
                for half in range(2):
                    b = 4 + 2 * xsl + half
                    for j in range(nf):
                        S.op("pe", lambda e, b=b, j=j, half=half: e.matmul(
                            ps[b][:, :], aT[asl][:, j, sub * 128:(sub + 1) * 128], wo[:, j, half * 512:(half + 1) * 512],
                            start=(j == 0), stop=(j == nf - 1)),
                            reads=[("aT", asl, j), "wo"], writes=[PK(b)])
                    if half == 1:
                        S.end_group()
                    sc = g3[:, t, eidx:eidx + 1] if eidx is not None else 1.0
                    rd = [PK(b), ("G5", r)] + ([("gates", t)] if eidx is not None else [])
                    S.op("dve", lambda e, b=b, half=half, sc=sc: e.scalar_tensor_tensor(
                        t1[xsl][:, half * 512:(half + 1) * 512], ps[b][:, :], sc, G5[r][:, half * 512:(half + 1) * 512], ALU.mult, ALU.mult),
                        reads=rd, writes=[("t1", xsl)])
                S.op("dve", lambda e: e.tensor_tensor(t1[xsl], t1[xsl], xt[xsl], ALU.add),
                     reads=[("t1", xsl), ("xt", xsl)], writes=[("t1", xsl)])
                S.dma("sp", ("xst", xsl), lambda e: e.dma_start(out=xs_d[t * 128:(t + 1) * 128, :], in_=t1[xsl]),
                      reads=[("t1", xsl)], writes=[("xs", t)])

            def load_unit(gd, ud, od):
                S.dma("pool", "wg", lambda e: e.dma_start(out=wg, in_=gd.rearrange("(k p) n -> p k n", p=128)), writes=["wg"])
                S.dma("pool", "wu", lambda e: e.dma_start(out=wu, in_=ud.rearrange("(k p) n -> p k n", p=128)), writes=["wu"])
                S.dma("pool", "wo", lambda e: e.dma_start(out=wo, in_=od.rearrange("(j p) n -> p j n", p=128)), writes=["wo"])
            ti = 0
            for (gd, ud, od, eidx) in units:
                load_unit(gd, ud, od)
                for (tok0, n) in tiles:
                    asl = ti % 2
                    ti += 1
                    for j in range(nf):
                        fchunk(tok0, n, asl, j)
                    for sub in range(n // 128):
                        ytile(tok0, asl, sub, eidx)

        def phase_final():
            A.reset(0)
            gf = A.f32(D)
            xt = [A.f32(D) for _ in range(3)]
            sqj = [A.bf16(D) for _ in range(2)]
            ot = [A.f32(D) for _ in range(2)]
            S.dma("sp", "gf", lambda e: e.dma_start(out=gf, in_=g_final_d[0].partition_broadcast(128)), writes=["gf"])
            S.op("dve", lambda e: e.memset(ssq[:], 0.0), writes=[("F", "ssq", t) for t in range(32)])

            def fload(t):
                s3 = t % 3
                S.dma("sp", ("xt", s3), lambda e: e.dma_start(out=xt[s3], in_=xs_d[t * 128:(t + 1) * 128, :]),
                      reads=[("xs", t)], writes=[("xt", s3)])

            def ftile(t):
                s3 = t % 3
                sl = t % 2
                S.op("act", lambda e: e.activation(sqj[sl], xt[s3], AF.Square, accum_out=ssq[:, t:t + 1]),
                     reads=[("xt", s3)], writes=[("sqj", sl), ("F", "ssq", t)])
                S.op("act", lambda e: e.activation(rstd[:, t:t + 1], ssq[:, t:t + 1], AF.Ln, bias=cvec[:, 0:1], scale=1.0 / D),
                     reads=[("F", "ssq", t)], writes=[("F", "rstd", t)])
                S.op("act", lambda e: e.activation(rstd[:, t:t + 1], rstd[:, t:t + 1], AF.Exp, scale=-0.5),
                     reads=[("F", "rstd", t)], writes=[("F", "rstd", t)])
                S.op("dve", lambda e: e.scalar_tensor_tensor(ot[sl], xt[s3], rstd[:, t:t + 1], gf, ALU.mult, ALU.mult),
                     reads=[("xt", s3), ("F", "rstd", t), "gf"], writes=[("ot", sl)])
                S.dma("sp", ("ost", sl), lambda e: e.dma_start(out=out_d[t * 128:(t + 1) * 128, :], in_=ot[sl]),
                      reads=[("ot", sl)], writes=[("out", t)])
            fload(0)
            fload(1)
            for t in range(32):
                if t + 2 < 32:
                    fload(t + 2)
                ftile(t)
            S.final_waits += [("ost", 0), ("ost", 1)]

        for l in range(n_layers):
            need_ctx = l < 1
            moe = (l % 2 == 1)
            w_inx_d_l[0] = w_inx_d[l]
            phase_A(l)
            S.barrier()
            if debug == "hT" and l == 0:
                S.dma("sp", "dbg", lambda e: e.dma_start(out=dbg_d, in_=A.t[:, 0:H_BYTES // 4].bitcast(BF16)), writes=["dbg"])
                S.final_waits += ["dbg"]
                break
            phase_gqa(l, need_ctx)
            S.barrier()
            phase_diff(l, need_ctx)
            S.barrier()
            phase_mla(l, need_ctx)
            S.barrier()
            phase_C(l, need_ctx, moe)
            S.barrier()
            phase_D(l, need_ctx, moe)
            S.barrier()
        phase_final()
        S.run()
    return nc


_CACHE = {}


def _host_inputs(inputs):
    f = lambda a: np.ascontiguousarray(np.asarray(a, dtype=np.float32))
    cols = _winx_cols()
    w_in = f(inputs["w_in"])
    shared = {
        "w_mod": f(inputs["w_mod"]), "b_mod": f(inputs["b_mod"]),
        "g_attn": f(inputs["g_attn"]), "g_ffn": f(inputs["g_ffn"]),
        "g_final": f(inputs["g_final"]).reshape(1, D),
        "w_inx": np.ascontiguousarray(w_in[:, :, cols]),
        "w_out": f(inputs["w_out"]),
        "ffn_w_in": f(inputs["ffn_w_in"]), "ffn_w_out": f(inputs["ffn_w_out"]),
        "moe_router": f(inputs["moe_router"]), "moe_w_in": f(inputs["moe_w_in"]), "moe_w_out": f(inputs["moe_w_out"]),
    }
    p = np.arange(128)
    sp = np.zeros((128, 16), np.float32)
    gq, gk = f(inputs["gqa_gq"]), f(inputs["gqa_gk"])
    gs, gcq, gckv = f(inputs["diff_gsub"]), f(inputs["mla_gcq"]), f(inputs["mla_gckv"])
    for l in range(2):
        sp[:, l * 8 + 0] = gq[l][p % 64]
        sp[:, l * 8 + 1] = gq[l][(p % 64) ^ 1]
        sp[:, l * 8 + 2] = gk[l][p % 64]
        sp[:, l * 8 + 3] = gk[l][(p % 64) ^ 1]
        sp[:, l * 8 + 4] = gs[l][p % 64]
        sp[:, l * 8 + 5] = gcq[l][0:128]
        sp[:, l * 8 + 6] = gcq[l][128 + (p % 64)]
        sp[:, l * 8 + 7] = gckv[l]
    shared["smallp"] = sp
    shared["lamv"] = np.ascontiguousarray(np.concatenate(
        [f(inputs["diff_lq1"]), f(inputs["diff_lk1"]), f(inputs["diff_lq2"]), f(inputs["diff_lk2"])], axis=1))
    wuq = f(inputs["mla_wuq"])
    swc = np.arange(384)
    hh, rr = swc // 96, swc % 96
    swc = np.where(rr >= 64, hh * 96 + 64 + ((rr - 64) ^ 1), swc)
    shared["w_uq"] = wuq
    shared["w_uqs"] = np.ascontiguousarray(wuq[:, :, swc])
    wukv = f(inputs["mla_wukv"])
    kc = np.concatenate([np.arange(h * 128, h * 128 + 64) for h in range(4)])
    vc = np.concatenate([np.arange(h * 128 + 64, h * 128 + 128) for h in range(4)])
    shared["w_ukvk"] = np.ascontiguousarray(wukv[:, :, kc])
    shared["w_ukvv"] = np.ascontiguousarray(wukv[:, :, vc])
    ac, as_, bc, bs = _rope_tables()
    shared.update({"ropeA_c": ac, "ropeA_s": as_, "ropeB_c": bc, "ropeB_s": bs})
    cst = np.zeros((128, 384), np.float32)
    cst[:, 0:128] = np.eye(128, dtype=np.float32)
    cst[:, 128:256] = 1.0
    cst[0:64, 256:320] = 1.0
    cst[64:128, 320:384] = 1.0
    shared["cst"] = cst
    x = f(inputs["x"])
    ctx = f(inputs["ctx"])
    c = f(inputs["c"])
    c_ctx = f(inputs["c_ctx"])
    maps = []
    for b in range(x.shape[0]):
        m = dict(shared)
        m["x"] = x[b]
        m["ctx"] = ctx[b]
        m["cc"] = np.ascontiguousarray(np.stack([c[b], c_ctx], axis=0))
        maps.append(m)
    return maps


def kernel(**inputs):
    maps = _host_inputs(inputs)
    if "nc" not in _CACHE:
        _CACHE["nc"] = build()
    nc = _CACHE["nc"]
    res = run_bass_kernel_spmd(nc, maps, core_ids=list(range(len(maps))))
    return np.stack([np.asarray(r["out"]).astype(np.float32) for r in res.results], axis=0)
```

```python
import math
from contextlib import ExitStack

import numpy as np
import concourse.bass as bass
import concourse.mybir as mybir
from concourse.bass_utils import run_bass_kernel_spmd

F32 = mybir.dt.float32
BF16 = mybir.dt.bfloat16
AF = mybir.ActivationFunctionType
ALU = mybir.AluOpType
AX = mybir.AxisListType

D = 1024
S_LAT = 4096
CTX = 256
T = S_LAT + CTX
NT128 = T // 128
EPS = 1e-6
ENGINES = ("pe", "act", "dve", "pool", "sp")

FM_CHUNKS = 24
NX = FM_CHUNKS * 128 + 384
VG_OFF = FM_CHUNKS * 128
VD_OFF = VG_OFF + 128


def _winx_cols():
    def sw(a):
        return [i ^ 1 for i in a]
    cols = []
    qg = list(range(0, 512))
    for c in range(4):
        cols += qg[c * 128:(c + 1) * 128]
    for c in range(4):
        cols += sw(qg[c * 128:(c + 1) * 128])
    for kv in range(2):
        k = list(range(512 + kv * 64, 512 + kv * 64 + 64))
        cols += k + k
    for kv in range(2):
        k = sw(list(range(512 + kv * 64, 512 + kv * 64 + 64)))
        cols += k + k
    qd = list(range(768, 1024))
    for c in range(2):
        cols += qd[c * 128:(c + 1) * 128]
    for c in range(2):
        cols += sw(qd[c * 128:(c + 1) * 128])
    kd = list(range(1024, 1280))
    for c in range(2):
        cols += kd[c * 128:(c + 1) * 128]
    for c in range(2):
        cols += sw(kd[c * 128:(c + 1) * 128])
    cols += list(range(1536, 1664))
    b = list(range(1664, 1728)) + list(range(1856, 1888))
    cols += b + [1664] * 32
    bs = list(range(1664, 1728)) + sw(list(range(1856, 1888)))
    cols += bs + [1664] * 32
    cols += list(range(1728, 1856))
    assert len(cols) == FM_CHUNKS * 128
    cols += list(range(640, 768))
    cols += list(range(1280, 1536))
    assert len(cols) == NX
    return np.array(cols, dtype=np.int64)


def _rope_tables():
    t = np.arange(S_LAT)
    rows = (t // 64).astype(np.float32)
    colsf = (t % 64).astype(np.float32)

    def tab(dim):
        quarter = dim // 4
        inv = (np.float32(10000.0) ** (-np.arange(quarter, dtype=np.float32) / np.float32(quarter))).astype(np.float32)
        ang = np.concatenate([rows[:, None] * inv, colsf[:, None] * inv], axis=-1).astype(np.float32)
        cos = np.cos(ang).astype(np.float32)
        sin = np.sin(ang).astype(np.float32)
        cd = np.repeat(cos, 2, axis=1)
        sd = np.repeat(sin, 2, axis=1)
        sd[:, 0::2] *= -1.0
        reps = 128 // dim
        cfull = np.ones((128, T), np.float32)
        sfull = np.zeros((128, T), np.float32)
        cfull[:, :S_LAT] = np.tile(cd.T, (reps, 1))
        sfull[:, :S_LAT] = np.tile(sd.T, (reps, 1))
        return cfull, sfull
    return tab(64) + tab(32)


class Op:
    __slots__ = ("eng", "emit", "deps", "needed", "tok", "dma", "gidx", "grp")

    def __init__(self, eng, emit, dma=None):
        self.eng = eng
        self.emit = emit
        self.deps = []
        self.needed = False
        self.tok = None
        self.dma = dma
        self.gidx = 0
        self.grp = None


class Sched:
    def __init__(self, nc, stack):
        self.nc = nc
        self.stack = stack
        self.ops = {e: [] for e in ENGINES}
        self.last_w = {}
        self.readers = {}
        self.esem = {e: stack.enter_context(nc.semaphore("es_" + e)) for e in ENGINES}
        self.dsem = {}
        self.dcum = {}
        self.dlast = {}
        self.final_waits = []
        self.bar_deps = []
        self.bar_gen = 0
        self.bar_seen = {e: 0 for e in ENGINES}
        self.nrec = 0
        self.cur_grp = None
        self.ngrp = 0

    def begin_group(self):
        self.ngrp += 1
        self.cur_grp = (self.ngrp, self.nrec)

    def end_group(self):
        self.cur_grp = None

    def _rec(self, op, reads, writes):
        deps = op.deps
        e = op.eng
        self.nrec += 1
        op.gidx = self.nrec
        op.grp = self.cur_grp
        if self.bar_seen[e] != self.bar_gen:
            self.bar_seen[e] = self.bar_gen
            deps.extend(self.bar_deps)
        lw = self.last_w
        rd = self.readers
        for k in reads:
            w = lw.get(k)
            if w is not None:
                deps.append(w)
            rd.setdefault(k, []).append(op)
        for k in writes:
            w = lw.get(k)
            if w is not None:
                deps.append(w)
            r = rd.get(k)
            if r:
                deps.extend(x for x in r if x is not op)
            rd[k] = []
            lw[k] = op
        self.ops[e].append(op)
        return op

    def op(self, eng, emit, reads=(), writes=()):
        return self._rec(Op(eng, emit), reads, writes)

    def dma(self, eng, chan, emit, reads=(), writes=()):
        if chan not in self.dsem:
            self.dsem[chan] = self.stack.enter_context(self.nc.semaphore("ds%d" % len(self.dsem)))
            self.dcum[chan] = 0
        self.dcum[chan] += 16
        op = Op(eng, emit, dma=(chan, self.dcum[chan]))
        self.dlast[chan] = op
        return self._rec(op, reads, writes)

    def barrier(self):
        deps = []
        for e in ENGINES:
            for op in reversed(self.ops[e]):
                if op.dma is None:
                    deps.append(op)
                    break
        deps.extend(self.dlast.values())
        self.bar_deps = deps
        self.bar_gen += 1
        self.last_w = {}
        self.readers = {}

    def finalize(self):
        for e in ENGINES:
            for op in self.ops[e]:
                best = {}
                for d in op.deps:
                    if d.dma is not None or (d.eng == "pe" and e == "pe"):
                        continue
                    cur = best.get(d.eng)
                    if cur is None or d.gidx > cur.gidx:
                        best[d.eng] = d
                for d in best.values():
                    d.needed = True
        for e in ENGINES:
            n = 0
            for op in self.ops[e]:
                if op.dma is not None:
                    op.tok = (("dma", op.dma[0]), op.dma[1])
                elif op.needed:
                    n += 1
                    op.tok = (e, n)
        self.plan = {}
        for e in ENGINES:
            seen = {}
            plan = []
            hoist = {}
            for op in self.ops[e]:
                if op.grp is not None:
                    h = hoist.setdefault(op.grp[0], {})
                    for d in op.deps:
                        if d.tok is None or d.gidx > op.grp[1]:
                            continue
                        sk, v = d.tok
                        if h.get(sk, 0) < v:
                            h[sk] = v
            done_grp = set()
            for op in self.ops[e]:
                need = {}
                if op.grp is not None and op.grp[0] not in done_grp:
                    done_grp.add(op.grp[0])
                    need.update(hoist[op.grp[0]])
                for d in op.deps:
                    if d.tok is None:
                        continue
                    sk, v = d.tok
                    if need.get(sk, 0) < v:
                        need[sk] = v
                waits = []
                for sk, v in need.items():
                    if seen.get(sk, 0) < v:
                        seen[sk] = v
                        waits.append((sk, v))
                plan.append((waits, op))
            self.plan[e] = plan

    def _sem(self, sk):
        if isinstance(sk, tuple):
            return self.dsem[sk[1]]
        return self.esem[sk]

    def emit_engine(self, e, eng):
        for waits, op in self.plan[e]:
            for sk, v in waits:
                eng.wait_ge(self._sem(sk), v)
            ins = op.emit(eng)
            if op.dma is not None:
                ins.then_inc(self.dsem[op.dma[0]], 16)
            elif op.needed:
                ins.then_inc(self.esem[e], 1)
        if e == "sp":
            for chan in self.final_waits:
                eng.wait_ge(self.dsem[chan], self.dcum[chan])

    def run(self):
        self.finalize()
        with self.nc.Block() as block:
            @block.tensor
            def _(eng):
                self.emit_engine("pe", eng)

            @block.scalar
            def _(eng):
                self.emit_engine("act", eng)

            @block.vector
            def _(eng):
                self.emit_engine("dve", eng)

            @block.gpsimd
            def _(eng):
                self.emit_engine("pool", eng)

            @block.sync
            def _(eng):
                self.emit_engine("sp", eng)


class Arena:
    def __init__(self, nc, stack, nbytes):
        self.n = nbytes
        self.t = stack.enter_context(nc.sbuf_tensor("arena", [128, nbytes // 4], F32))
        self.off = 0

    def reset(self, off=0):
        self.off = off

    def f32(self, cols):
        a = self.off
        self.off += cols * 4
        assert self.off <= self.n, ("arena overflow", self.off, self.n)
        return self.t[:, a // 4:a // 4 + cols]

    def bf16(self, cols):
        c2 = (cols + 1) // 2 * 2
        a = self.off
        self.off += c2 * 2
        assert self.off <= self.n, ("arena overflow", self.off, self.n)
        return self.t[:, a // 4:a // 4 + c2 // 2].bitcast(BF16)[:, 0:cols]


ARENA_BYTES = 209200
H_BYTES = 8 * T * 2


def build(n_layers=2, debug=False):
    nc = bass.Bass("TRN2", target_bir_lowering=False)

    def din(name, shape):
        return nc.dram_tensor(name, list(shape), F32, kind="ExternalInput").ap()

    x_d = din("x", [S_LAT, D])
    ctx_d = din("ctx", [CTX, D])
    cc_d = din("cc", [2, D])
    w_mod_d = din("w_mod", [2, D, 6 * D])
    b_mod_d = din("b_mod", [2, 6 * D])
    g_attn_d = din("g_attn", [2, D])
    g_ffn_d = din("g_ffn", [2, D])
    g_final_d = din("g_final", [1, D])
    w_inx_d = din("w_inx", [2, D, NX])
    w_out_d = din("w_out", [2, D, D])
    smallp_d = din("smallp", [128, 16])
    lamv_d = din("lamv", [2, 128])
    w_uq_d = din("w_uq", [2, 192, 384])
    w_uqs_d = din("w_uqs", [2, 192, 384])
    w_ukvk_d = din("w_ukvk", [2, 128, 256])
    w_ukvv_d = din("w_ukvv", [2, 128, 256])
    ffn_w_in_d = din("ffn_w_in", [1, D, 5632])
    ffn_w_out_d = din("ffn_w_out", [1, 2816, D])
    moe_router_d = din("moe_router", [1, D, 8])
    moe_w_in_d = din("moe_w_in", [1, 8, D, 3584])
    moe_w_out_d = din("moe_w_out", [1, 8, 1792, D])
    ropeA_c_d = din("ropeA_c", [128, T])
    ropeA_s_d = din("ropeA_s", [128, T])
    ropeB_c_d = din("ropeB_c", [128, T])
    ropeB_s_d = din("ropeB_s", [128, T])
    cst_d = din("cst", [128, 384])
    out_d = nc.dram_tensor("out", [S_LAT, D], F32, kind="ExternalOutput").ap()
    skind = "ExternalOutput" if debug else "Internal"
    xs_d = nc.dram_tensor("xs", [T, D], F32, kind=skind).ap()
    ao_d = nc.dram_tensor("ao", [8, 128, T], BF16, kind=skind).ap()
    modv_d = nc.dram_tensor("modv", [2, 2, 6 * D], F32, kind=skind).ap()
    if debug:
        dbg_d = nc.dram_tensor("dbg", [128, 8 * T], BF16, kind="ExternalOutput").ap()

    with ExitStack() as st:
        S = Sched(nc, st)
        A = Arena(nc, st, ARENA_BYTES)
        psall = st.enter_context(nc.psum_tensor("psall", [128, 4096], F32))
        ps = [psall[:, i * 512:(i + 1) * 512] for i in range(8)]
        cstb = st.enter_context(nc.sbuf_tensor("cstb", [128, 384], BF16))
        smallp = st.enter_context(nc.sbuf_tensor("smallp_sb", [128, 16], F32))
        scc = st.enter_context(nc.sbuf_tensor("scc", [128, 16], F32))
        ssq = st.enter_context(nc.sbuf_tensor("ssq", [128, 40], F32))
        rstd = st.enter_context(nc.sbuf_tensor("rstd", [128, 40], F32))
        gates = st.enter_context(nc.sbuf_tensor("gates", [128, 32 * 8], F32))
        tiny = st.enter_context(nc.sbuf_tensor("tiny", [128, 64], F32))
        cvec = st.enter_context(nc.sbuf_tensor("cvec", [128, 8], F32))
        lamt = st.enter_context(nc.sbuf_tensor("lamt", [128, 160], F32))
        ident = cstb[:, 0:128]
        ones = cstb[:, 128:256]
        bd64 = cstb[:, 256:384]
        PK = lambda i: ("ps", i)

        def hT_view():
            return A.t[:, 0:H_BYTES // 4].bitcast(BF16).rearrange("p (k n) -> p k n", k=8)

        for ci_, cv_ in enumerate((EPS, 64 * EPS, 192 * EPS, 128 * EPS)):
            S.op("dve", lambda e, ci_=ci_, cv_=cv_: e.memset(cvec[:, ci_:ci_ + 1], cv_), writes=["cvec"])
        S.dma("pool", "cst", lambda e: e.dma_start(out=cstb[:], in_=cst_d), writes=["cst"])
        S.dma("sp", "smallp", lambda e: e.dma_start(out=smallp[:], in_=smallp_d), writes=["smallp"])
        scc3 = scc[:].rearrange("p (k r) -> p k r", r=2)
        S.dma("sp", "cc0", lambda e: e.dma_start(out=scc3[:, :, 0], in_=cc_d[0].rearrange("(k p) -> p k", p=128),
                                                  allow_slow_non_contiguous=True), writes=["scc0"])
        S.dma("sp", "cc1", lambda e: e.dma_start(out=scc3[:, :, 1], in_=cc_d[1].rearrange("(k p) -> p k", p=128),
                                                  allow_slow_non_contiguous=True), writes=["scc1"])
        S.op("act", lambda e: e.activation(scc[:], scc[:], AF.Silu), reads=["scc0", "scc1"], writes=["scc"])

        def phase_mod(l):
            A.reset(0)
            wm = [A.f32(8 * 512).rearrange("p (k n) -> p k n", k=8) for _ in range(2)]
            modsb = A.f32(6 * D)
            bmod = A.f32(6 * D)
            gt = A.f32(2 * D)
            S.dma("sp", "bmod", lambda e: e.dma_start(out=bmod[0:2, :], in_=b_mod_d[l].partition_broadcast(2)), writes=["bmod"])
            S.dma("sp", "gt0", lambda e: e.dma_start(out=gt[0:2, 0:D], in_=g_attn_d[l].partition_broadcast(2)), writes=["gt0"])
            S.dma("sp", "gt1", lambda e: e.dma_start(out=gt[0:2, D:2 * D], in_=g_ffn_d[l].partition_broadcast(2)), writes=["gt1"])
            for nt in range(12):
                sl = nt % 2
                S.dma("sp", ("wm", sl), lambda e, nt=nt, sl=sl: e.dma_start(
                    out=wm[sl], in_=w_mod_d[l][:, nt * 512:(nt + 1) * 512].rearrange("(k p) n -> p k n", p=128)),
                    writes=[("wm", sl)])
                for k in range(8):
                    S.op("pe", lambda e, sl=sl, k=k: e.matmul(ps[sl][0:2, :], scc3[:, k, :], wm[sl][:, k, :],
                                                             start=(k == 0), stop=(k == 7)),
                         reads=[("wm", sl), "scc"], writes=[PK(sl)])
                S.op("dve", lambda e, sl=sl, nt=nt: e.tensor_tensor(modsb[0:2, nt * 512:(nt + 1) * 512], ps[sl][0:2, :],
                                                                   bmod[0:2, nt * 512:(nt + 1) * 512], ALU.add),
                     reads=[PK(sl), "bmod"], writes=["modsb"])
            for j, g0 in ((1, 0), (4, D)):
                S.op("dve", lambda e, j=j, g0=g0: e.scalar_tensor_tensor(
                    modsb[0:2, j * D:(j + 1) * D], modsb[0:2, j * D:(j + 1) * D], 1.0, gt[0:2, g0:g0 + D], ALU.add, ALU.mult),
                    reads=["modsb", "gt0", "gt1"], writes=["modsb"])
            S.dma("sp", "modv", lambda e: e.dma_start(out=modv_d[l], in_=modsb[0:2, :]), reads=["modsb"], writes=[("modv", l)])

        for l in range(n_layers):
            phase_mod(l)
            S.barrier()

        def load_bc(dst, l, r, j, key):
            S.dma("sp", key, lambda e: e.dma_start(out=dst, in_=modv_d[l][r, j * D:(j + 1) * D].partition_broadcast(128)),
                  reads=[("modv", l)], writes=[key])

        def src_tile(l, t):
            if l == 0:
                if t < 32:
                    return x_d[t * 128:(t + 1) * 128, :]
                return ctx_d[(t - 32) * 128:(t - 31) * 128, :]
            return xs_d[t * 128:(t + 1) * 128, :]

        def norm_mod_T(tag, t, xt_ap, xt_key, mul_bc, mul_key, add_bc, add_key, bufs, hT, deferred=None):
            sl = t % 2
            sqj, t1, hb = bufs["sqj"][sl], bufs["t1"][sl], bufs["hb"][sl]
            S.op("act", lambda e: e.activation(sqj, xt_ap, AF.Square, accum_out=ssq[:, t:t + 1]),
                 reads=[xt_key], writes=[(tag, "sqj", sl), (tag, "ssq", t)])
            S.op("act", lambda e: e.activation(rstd[:, t:t + 1], ssq[:, t:t + 1], AF.Ln, bias=cvec[:, 0:1], scale=1.0 / D),
                 reads=[(tag, "ssq", t)], writes=[(tag, "rstd", t)])
            S.op("act", lambda e: e.activation(rstd[:, t:t + 1], rstd[:, t:t + 1], AF.Exp, scale=-0.5),
                 reads=[(tag, "rstd", t)], writes=[(tag, "rstd", t)])
            S.op("dve", lambda e: e.scalar_tensor_tensor(t1, xt_ap, rstd[:, t:t + 1], mul_bc, ALU.mult, ALU.mult),
                 reads=[xt_key, (tag, "rstd", t), mul_key], writes=[(tag, "t1", sl)])
            S.op("pool", lambda e: e.tensor_tensor(hb, t1, add_bc, ALU.add),
                 reads=[(tag, "t1", sl), add_key], writes=[(tag, "hb", sl)])
            pb = 6 + sl
            psT = ps[pb][:].bitcast(BF16)
            for k in range(8):
                S.op("pe", lambda e, k=k: e.transpose(psT[:, k * 128:(k + 1) * 128], hb[:, k * 128:(k + 1) * 128], ident),
                     reads=[(tag, "hb", sl), "cst"], writes=[PK(pb)])
            def evac():
                S.op("act", lambda e: e.activation(hT[:, :, t * 128:(t + 1) * 128],
                                                   psT[:, 0:1024].rearrange("p (k n) -> p k n", k=8), AF.Copy),
                     reads=[PK(pb)], writes=[("hT", t)])
            if deferred is None:
                evac()
            else:
                deferred.append(evac)

        def alloc_norm_bufs():
            return {"sqj": [A.bf16(D) for _ in range(2)], "t1": [A.f32(D) for _ in range(2)],
                    "hb": [A.bf16(D) for _ in range(2)]}

        def phase_A(l):
            A.reset(H_BYTES)
            hT = hT_view()
            bufs = alloc_norm_bufs()
            xt = [A.f32(D) for _ in range(3)]
            mul = [A.f32(D) for _ in range(2)]
            add = [A.f32(D) for _ in range(2)]
            for r in range(2):
                load_bc(mul[r], l, r, 1, ("Amul", r))
                load_bc(add[r], l, r, 0, ("Aadd", r))
            S.op("dve", lambda e: e.memset(ssq[:], 0.0), writes=[("A", "ssq", t) for t in range(NT128)])
            dfr = []
            for t in range(NT128):
                r = 0 if t < 32 else 1
                s3 = t % 3
                S.dma("sp", ("xt", s3), lambda e, t=t, s3=s3: e.dma_start(out=xt[s3], in_=src_tile(l, t)),
                      reads=[("xs", t)], writes=[("xt", s3)])
                norm_mod_T("A", t, xt[s3], ("xt", s3), mul[r], ("Amul", r), add[r], ("Aadd", r), bufs, hT, deferred=dfr)
                while len(dfr) > 1:
                    dfr.pop(0)()
            while dfr:
                dfr.pop(0)()

        def attn_unit(KT, QT, VXh, kcs, n, scale, obank, PT, sbufs, rk, wk, tp=None):
            tpk = {"tile_position": tp} if tp is not None else {}
            groups = [kcs[i:i + 2] for i in range(0, len(kcs), 2)]
            ng = len(groups)

            def qk(g):
                sb = sbufs[g % len(sbufs)]
                w = len(groups[g])
                for i, kc in enumerate(groups[g]):
                    S.op("pe", lambda e, i=i, kc=kc: e.matmul(ps[sb + i][:, 0:n], KT[:, kc * 128:(kc + 1) * 128], QT, start=True, stop=True, **tpk),
                         reads=rk, writes=[PK(sb + i)])
                sl = g % len(PT)
                if n == 512:
                    srcap = psall[:, sb * 512:(sb + w) * 512]
                    dstap = PT[sl][:, 0:w * 512]
                else:
                    srcap = psall[:, sb * 512:(sb + w) * 512].rearrange("p (b c) -> p b c", c=512)[:, :, 0:n]
                    dstap = PT[sl][:, 0:w * 512].rearrange("p (b c) -> p b c", c=512)[:, :, 0:n]
                S.op("act", lambda e: e.activation(dstap, srcap, AF.Exp, scale=scale),
                     reads=[PK(sb + i) for i in range(w)], writes=[("PT", sl)])

            def pv(g):
                sl = g % len(PT)
                for i, kc in enumerate(groups[g]):
                    first = (g == 0 and i == 0)
                    last = (g == ng - 1 and i == len(groups[g]) - 1)
                    S.op("pe", lambda e, i=i, kc=kc, first=first, last=last: e.matmul(
                        ps[obank][:, 0:n], VXh[:, kc, :], PT[sl][:, i * 512:i * 512 + n], start=first, stop=last),
                        reads=[("PT", sl)] + rk, writes=[PK(obank)] + wk)

            qk(0)
            if ng > 1:
                qk(1)
            for g in range(ng):
                if g + 2 < ng:
                    qk(g + 2)
                pv(g)

        def attn_multi(lanes, kcs, n, scale, PT, sbufs, rk, hook=None):
            nk = len(kcs)
            L = len(lanes)

            def qk(g):
                sb = sbufs[g % len(sbufs)]
                kc = kcs[g]
                for i, (KT, QT, VXh, ob, tp) in enumerate(lanes):
                    S.op("pe", lambda e, i=i, KT=KT, QT=QT, tp=tp: e.matmul(ps[sb + i][:, 0:n], KT[:, kc * 128:(kc + 1) * 128], QT,
                                                                          start=True, stop=True, tile_position=tp),
                         reads=rk, writes=[PK(sb + i)])
                sl = g % len(PT)
                if n == 512:
                    srcap = psall[:, sb * 512:(sb + L) * 512]
                    dstap = PT[sl][:, 0:L * 512]
                else:
                    srcap = psall[:, sb * 512:(sb + L) * 512].rearrange("p (b c) -> p b c", c=512)[:, :, 0:n]
                    dstap = PT[sl][:, 0:L * 512].rearrange("p (b c) -> p b c", c=512)[:, :, 0:n]
                S.op("act", lambda e: e.activation(dstap, srcap, AF.Exp, scale=scale),
                     reads=[PK(sb + i) for i in range(L)], writes=[("PT", sl)])

            def pv(g):
                kc = kcs[g]
                sl = g % len(PT)
                for i, (KT, QT, VXh, ob, tp) in enumerate(lanes):
                    S.op("pe", lambda e, i=i, VXh=VXh, ob=ob: e.matmul(ps[ob][:, 0:n], VXh[:, kc, :], PT[sl][:, i * 512:i * 512 + n],
                                                                      start=(g == 0), stop=(g == nk - 1)),
                         reads=[("PT", sl)] + rk, writes=[PK(ob)])

            qk(0)
            if nk > 1:
                qk(1)
            hk = min(4, nk - 1)
            for g in range(nk):
                if g + 2 < nk:
                    qk(g + 2)
                pv(g)
                if hook is not None and g == hk:
                    hook()

        def q_tiles(need_ctx):
            tl = [(tt * 512, 512, list(range(NT128))) for tt in range(8)]
            if need_ctx:
                tl.append((S_LAT, 256, [32, 33]))
            return tl

        def tok_tiles():
            return [(tt * 512, 512) for tt in range(8)] + [(S_LAT, 256)]

        def load_win(c0, c1, dst, key):
            wd = w_inx_d_l[0]
            S.dma("pool", key, lambda e: e.dma_start(out=dst, in_=wd[:, c0:c1].rearrange("(k p) n -> p k n", p=128)),
                  writes=[key])

        w_inx_d_l = [None]

        def proj_fm(pbank, win, c, hT, tok0, n, wkey):
            for k in range(8):
                S.op("pe", lambda e, k=k: e.matmul(ps[pbank][:, 0:n], win[:, k, c * 128:(c + 1) * 128], hT[:, k, tok0:tok0 + n],
                                                  start=(k == 0), stop=(k == 7)),
                     reads=[wkey, "hTall"], writes=[PK(pbank)])

        def load_rope(cdst, sdst, cd, sd, tok0, n, sl):
            S.dma("sp", ("rc", sl), lambda e: e.dma_start(out=cdst[:, 0:n], in_=cd[:, tok0:tok0 + n]), writes=[("rc", sl)])
            S.dma("sp", ("rs", sl), lambda e: e.dma_start(out=sdst[:, 0:n], in_=sd[:, tok0:tok0 + n]), writes=[("rs", sl)])

        def phase_gqa(l, need_ctx):
            A.reset(H_BYTES)
            hT = hT_view()
            QG = A.bf16(4 * T).rearrange("p (c n) -> p c n", c=4)
            KG = A.bf16(2 * T).rearrange("p (c n) -> p c n", c=2)
            VXf = A.bf16(2 * NT128 * 128)
            VX = VXf.rearrange("p (h t c) -> p h t c", h=2, t=NT128)
            win = A.bf16(8 * 1664).rearrange("p (k n) -> p k n", k=8)
            rc = [A.f32(512) for _ in range(2)]
            rs = [A.f32(512) for _ in range(2)]
            sq = [A.bf16(512) for _ in range(2)]
            rr = [A.f32(512) for _ in range(2)]
            t1 = [A.f32(512) for _ in range(2)]
            t2 = [A.f32(512) for _ in range(2)]
            PT = [A.bf16(1024) for _ in range(3)]
            AO = [A.bf16(512) for _ in range(2)]
            rec = [A.f32(512) for _ in range(2)]
            osb = [A.f32(512) for _ in range(2)]
            load_win(0, 1536, win[:, :, 0:1536], "winA")
            load_win(VG_OFF, VG_OFF + 128, win[:, :, 1536:1664], "winB")
            S.op("pool", lambda e: e.memset(VXf.rearrange("p (g c) -> p g c", c=128)[:, :, 64:128], 1.0), writes=["VXones"])
            S.op("pool", lambda e: e.memset(tiny[:, 0:1], 0.0), reads=[("hT", t) for t in range(NT128)], writes=["hTall"])

            def chunk(tok0, n, sl, ci, kind, idx, cn, cs, gcol, gscol):
                b0 = (ci % 2) * 3
                u = ci % 2
                proj_fm(b0, win, cn, hT, tok0, n, "winA")
                proj_fm(b0 + 1, win, cs, hT, tok0, n, "winA")
                S.op("act", lambda e: e.activation(sq[u][:, 0:n], ps[b0][:, 0:n], AF.Square),
                     reads=[PK(b0)], writes=[("sq", u)])
                S.op("pe", lambda e: e.matmul(ps[b0 + 2][:, 0:n], bd64, sq[u][:, 0:n], start=True, stop=True),
                     reads=[("sq", u), "cst"], writes=[PK(b0 + 2)])
                S.op("act", lambda e: e.activation(rr[u][:, 0:n], ps[b0 + 2][:, 0:n], AF.Ln, bias=cvec[:, 1:1 + 1], scale=1.0), reads=[PK(b0 + 2), "cvec"], writes=[("rr", u)])
                S.op("act", lambda e: e.activation(rr[u][:, 0:n], rr[u][:, 0:n], AF.Exp, scale=-0.5), reads=[("rr", u)], writes=[("rr", u)])
                S.op("dve", lambda e: e.tensor_tensor(t1[u][:, 0:n], ps[b0][:, 0:n], rr[u][:, 0:n], ALU.mult),
                     reads=[PK(b0), ("rr", u)], writes=[("t1", u)])
                S.op("dve", lambda e: e.tensor_tensor(t2[u][:, 0:n], ps[b0 + 1][:, 0:n], rr[u][:, 0:n], ALU.mult),
                     reads=[PK(b0 + 1), ("rr", u)], writes=[("t2", u)])
                S.op("dve", lambda e: e.scalar_tensor_tensor(t1[u][:, 0:n], t1[u][:, 0:n], smallp[:, l * 8 + gcol:l * 8 + gcol + 1],
                                                             rc[sl][:, 0:n], ALU.mult, ALU.mult),
                     reads=[("t1", u), ("rc", sl), "smallp"], writes=[("t1", u)])
                S.op("dve", lambda e: e.scalar_tensor_tensor(t2[u][:, 0:n], t2[u][:, 0:n], smallp[:, l * 8 + gscol:l * 8 + gscol + 1],
                                                              rs[sl][:, 0:n], ALU.mult, ALU.mult),
                     reads=[("t2", u), ("rs", sl), "smallp"], writes=[("t2", u)])
                dst = QG[:, idx, tok0:tok0 + n] if kind == "q" else KG[:, idx, tok0:tok0 + n]
                S.op("pool", lambda e: e.tensor_tensor(dst, t1[u][:, 0:n], t2[u][:, 0:n], ALU.add),
                     reads=[("t1", u), ("t2", u)], writes=[("QK", kind, idx, tok0)])

            for it, (tok0, n) in enumerate(tok_tiles()):
                is_ctx = tok0 >= S_LAT
                sl = it % 2
                load_rope(rc[sl], rs[sl], ropeA_c_d, ropeA_s_d, tok0, n, sl)
                chunks = []
                if (not is_ctx) or need_ctx:
                    chunks += [("q", c, c, 4 + c, 0, 1) for c in range(4)]
                chunks += [("k", kv, 8 + kv, 10 + kv, 2, 3) for kv in range(2)]
                for ci, (kind, idx, cn, cs, gcol, gscol) in enumerate(chunks):
                    chunk(tok0, n, sl, ci, kind, idx, cn, cs, gcol, gscol)

            def vtile(t):
                b = 6 + t % 2
                for k in range(8):
                    S.op("pe", lambda e, k=k: e.matmul(ps[b][:, 0:128], hT[:, k, t * 128:(t + 1) * 128], win[:, k, 1536:1664],
                                                      start=(k == 0), stop=(k == 7)),
                         reads=["winB", "hTall"], writes=[PK(b)])
                S.op("dve", lambda e: e.tensor_copy(VX[:, :, t, 0:64], ps[b][:, 0:128].rearrange("p (h c) -> p h c", h=2)),
                     reads=[PK(b), "VXones"], writes=[("VX", t)])
            for t in range(NT128):
                vtile(t)
            S.op("pool", lambda e: e.memset(tiny[:, 1:2], 0.0),
                 reads=[("VX", t) for t in range(NT128)] + [("QK", "k", kv, tk) for kv in range(2) for (tk, _n) in tok_tiles()],
                 writes=["KVall"])

            def unit2(c, kv, tok0, n, kcs, a):
                lanes = []
                for hh in range(2):
                    rsl = slice(hh * 64, hh * 64 + 64)
                    lanes.append((KG[rsl, kv, :], QG[rsl, c, tok0:tok0 + n], VX[:, kv], 6 + hh, (hh * 64, 0)))
                attn_multi(lanes, kcs, n, 8.0, PT, [0, 2, 4], ["KVall", ("QK", "q", c, tok0)])
                for hh in range(2):
                    ob = 6 + hh
                    rsl = slice(hh * 64, hh * 64 + 64)
                    S.op("dve", lambda e, ob=ob, hh=hh: e.tensor_copy(rec[hh][0:64, 0:n], ps[ob][64:128, 0:n]),
                         reads=[PK(ob)], writes=[("rec", hh)])
                    S.op("dve", lambda e, ob=ob, hh=hh: e.tensor_copy(osb[hh][0:64, 0:n], ps[ob][0:64, 0:n]),
                         reads=[PK(ob)], writes=[("osb", hh)])
                for hh in range(2):
                    rsl = slice(hh * 64, hh * 64 + 64)
                    S.op("dve", lambda e, hh=hh: e.reciprocal(rec[hh][0:64, 0:n], rec[hh][0:64, 0:n]),
                         reads=[("rec", hh)], writes=[("rec", hh)])
                    S.op("dve", lambda e, hh=hh, rsl=rsl: e.tensor_tensor(AO[a][rsl, 0:n], osb[hh][0:64, 0:n], rec[hh][0:64, 0:n], ALU.mult),
                         reads=[("osb", hh), ("rec", hh)], writes=[("AO", a, hh)])

            def store(a, cidx, tok0, n):
                S.dma("sp", ("AOst", a), lambda e: e.dma_start(out=ao_d[cidx, :, tok0:tok0 + n], in_=AO[a][:, 0:n]),
                      reads=[("AO", a, 0), ("AO", a, 1)], writes=[("ao", cidx, tok0)])
            un = 0
            for c in range(4):
                for qi, (tok0, n, kcs) in enumerate(q_tiles(need_ctx)):
                    a = (c + qi) % 2
                    unit2(c, c // 2, tok0, n, kcs, a)
                    store(a, c, tok0, n)

        def phase_diff(l, need_ctx):
            lam_init = 0.8 - 0.6 * math.exp(-0.3 * l)
            A.reset(H_BYTES)
            hT = hT_view()
            QD = A.bf16(2 * T).rearrange("p (c n) -> p c n", c=2)
            KD = A.bf16(2 * T).rearrange("p (c n) -> p c n", c=2)
            VXf = A.bf16(4 * NT128 * 128)
            VX = VXf.rearrange("p (h t c) -> p h t c", h=4, t=NT128)
            win = A.bf16(8 * 1280).rearrange("p (k n) -> p k n", k=8)
            rc = [A.f32(512) for _ in range(2)]
            rs = [A.f32(512) for _ in range(2)]
            t1 = [A.f32(512) for _ in range(2)]
            t2 = [A.f32(512) for _ in range(2)]
            PT = [A.bf16(1024) for _ in range(3)]
            AO = [A.bf16(512) for _ in range(2)]
            rec = [A.f32(512) for _ in range(2)]
            dd = [A.f32(512) for _ in range(2)]
            sq = [A.bf16(512) for _ in range(2)]
            full = [rec[0], rec[1], dd[0], dd[1]]
            PT4 = [A.bf16(2048) for _ in range(3)]
            load_win(12 * 128, 20 * 128, win[:, :, 0:1024], "winA")
            load_win(VD_OFF, VD_OFF + 256, win[:, :, 1024:1280], "winB")
            S.op("pool", lambda e: e.memset(VXf.rearrange("p (g c) -> p g c", c=128)[:, :, 64:128], 1.0), writes=["VXones"])
            S.op("pool", lambda e: e.memset(tiny[:, 0:1], 0.0), reads=[("hT", t) for t in range(NT128)], writes=["hTall"])
            S.dma("sp", "lamv", lambda e: e.dma_start(out=lamt[:, 0:128], in_=lamv_d[l].partition_broadcast(128)), writes=["lamt"])
            S.op("dve", lambda e: e.tensor_tensor(lamt[:, 128:160], lamt[:, 0:32], lamt[:, 32:64], ALU.mult), reads=["lamt"], writes=["lam1"])
            S.op("dve", lambda e: e.tensor_reduce(tiny[:, 8:9], lamt[:, 128:160], AX.X, ALU.add), reads=["lam1"], writes=["lams1"])
            S.op("dve", lambda e: e.tensor_tensor(lamt[:, 128:160], lamt[:, 64:96], lamt[:, 96:128], ALU.mult), reads=["lamt", "lams1"], writes=["lam1"])
            S.op("dve", lambda e: e.tensor_reduce(tiny[:, 9:10], lamt[:, 128:160], AX.X, ALU.add), reads=["lam1"], writes=["lams2"])
            S.op("act", lambda e: e.activation(tiny[:, 10:12], tiny[:, 8:10], AF.Exp), reads=["lams1", "lams2"], writes=["lame"])
            S.op("dve", lambda e: e.tensor_tensor(tiny[:, 12:13], tiny[:, 11:12], tiny[:, 10:11], ALU.subtract), reads=["lame"], writes=["lamd"])
            S.op("dve", lambda e: e.tensor_scalar(tiny[:, 13:14], tiny[:, 12:13], -lam_init, 0.0, ALU.add, ALU.add), reads=["lamd"], writes=["neglam"])
            S.op("dve", lambda e: e.tensor_scalar(tiny[:, 14:15], smallp[:, l * 8 + 4:l * 8 + 5], 8.0 * (1.0 - lam_init), 0.0, ALU.mult, ALU.add),
                 reads=["smallp"], writes=["gs8"])

            def chunk(tok0, n, sl, ci, kind, idx, cn, cs):
                b0 = (ci % 2) * 2
                u = ci % 2
                proj_fm(b0, win, cn, hT, tok0, n, "winA")
                proj_fm(b0 + 1, win, cs, hT, tok0, n, "winA")
                S.op("dve", lambda e: e.tensor_tensor(t1[u][:, 0:n], ps[b0][:, 0:n], rc[sl][:, 0:n], ALU.mult),
                     reads=[PK(b0), ("rc", sl)], writes=[("t1", u)])
                S.op("dve", lambda e: e.tensor_tensor(t2[u][:, 0:n], ps[b0 + 1][:, 0:n], rs[sl][:, 0:n], ALU.mult),
                     reads=[PK(b0 + 1), ("rs", sl)], writes=[("t2", u)])
                dst = QD[:, idx, tok0:tok0 + n] if kind == "q" else KD[:, idx, tok0:tok0 + n]
                S.op("pool", lambda e: e.tensor_tensor(dst, t1[u][:, 0:n], t2[u][:, 0:n], ALU.add),
                     reads=[("t1", u), ("t2", u)], writes=[("QK", kind, idx, tok0)])

            for it, (tok0, n) in enumerate(tok_tiles()):
                is_ctx = tok0 >= S_LAT
                sl = it % 2
                load_rope(rc[sl], rs[sl], ropeB_c_d, ropeB_s_d, tok0, n, sl)
                chunks = []
                if (not is_ctx) or need_ctx:
                    chunks += [("q", c, c, 2 + c) for c in range(2)]
                chunks += [("k", c, 4 + c, 6 + c) for c in range(2)]
                for ci, (kind, idx, cn, cs) in enumerate(chunks):
                    chunk(tok0, n, sl, ci, kind, idx, cn, cs)

            def vtile(t):
                b = 6 + t % 2
                for k in range(8):
                    S.op("pe", lambda e, k=k: e.matmul(ps[b][:, 0:256], hT[:, k, t * 128:(t + 1) * 128], win[:, k, 1024:1280],
                                                      start=(k == 0), stop=(k == 7)),
                         reads=["winB", "hTall"], writes=[PK(b)])
                S.op("dve", lambda e: e.tensor_copy(VX[:, :, t, 0:64], ps[b][:, 0:256].rearrange("p (h c) -> p h c", h=4)),
                     reads=[PK(b), "VXones"], writes=[("VX", t)])
            for t in range(NT128):
                vtile(t)
            S.op("pool", lambda e: e.memset(tiny[:, 1:2], 0.0),
                 reads=[("VX", t) for t in range(NT128)] + [("QK", "k", c, tk) for c in range(2) for (tk, _n) in tok_tiles()],
                 writes=["KVall"])

            def post_a(n):
                for i in range(4):
                    S.op("dve", lambda e, i=i: e.tensor_copy(full[i][:, 0:n], ps[4 + i][:, 0:n]), reads=[PK(4 + i)], writes=[("full", i)])
                for hh in range(2):
                    f0, f1 = full[2 * hh], full[2 * hh + 1]
                    k0, k1 = ("full", 2 * hh), ("full", 2 * hh + 1)
                    S.op("dve", lambda e, hh=hh, f0=f0: e.reciprocal(t1[hh][0:64, 0:n], f0[64:128, 0:n]), reads=[k0], writes=[("t1", hh)])
                    S.op("dve", lambda e, hh=hh, f1=f1: e.reciprocal(t2[hh][0:64, 0:n], f1[64:128, 0:n]), reads=[k1], writes=[("t2", hh)])
                    S.op("pool", lambda e, hh=hh, f0=f0: e.tensor_tensor(f0[0:64, 0:n], f0[0:64, 0:n], t1[hh][0:64, 0:n], ALU.mult),
                         reads=[k0, ("t1", hh)], writes=[k0])
                    S.op("pool", lambda e, hh=hh, f1=f1: e.tensor_tensor(f1[0:64, 0:n], f1[0:64, 0:n], t2[hh][0:64, 0:n], ALU.mult),
                         reads=[k1, ("t2", hh)], writes=[k1])
                    S.op("dve", lambda e, hh=hh, f0=f0, f1=f1: e.scalar_tensor_tensor(t1[hh][0:64, 0:n], f1[0:64, 0:n], tiny[0:64, 13:14], f0[0:64, 0:n],
                                                                                   ALU.mult, ALU.add),
                         reads=[k0, k1, "neglam"], writes=[("t1", hh)])

            def post_b(n, a, cidx, tok0):
                for hh in range(2):
                    S.op("act", lambda e, hh=hh: e.activation(sq[hh][0:64, 0:n], t1[hh][0:64, 0:n], AF.Square), reads=[("t1", hh)], writes=[("sq", hh)])
                for hh in range(2):
                    S.op("pe", lambda e, hh=hh: e.matmul(ps[hh][0:64, 0:n], ones[0:64, 0:64], sq[hh][0:64, 0:n], start=True, stop=True),
                         reads=[("sq", hh), "cst"], writes=[PK(hh)])
                    S.op("act", lambda e, hh=hh: e.activation(t2[hh][0:64, 0:n], ps[hh][0:64, 0:n], AF.Ln, bias=cvec[0:64, 1:2], scale=1.0),
                         reads=[PK(hh), "cvec"], writes=[("t2", hh)])
                    S.op("act", lambda e, hh=hh: e.activation(t2[hh][0:64, 0:n], t2[hh][0:64, 0:n], AF.Exp, scale=-0.5), reads=[("t2", hh)], writes=[("t2", hh)])
                    S.op("dve", lambda e, hh=hh: e.scalar_tensor_tensor(AO[a][hh * 64:hh * 64 + 64, 0:n], t1[hh][0:64, 0:n], tiny[0:64, 14:15],
                                                                        t2[hh][0:64, 0:n], ALU.mult, ALU.mult),
                         reads=[("t1", hh), ("t2", hh), "gs8"], writes=[("AO", a, hh)])
                S.dma("sp", ("AOst", a), lambda e: e.dma_start(out=ao_d[cidx, :, tok0:tok0 + n], in_=AO[a][:, 0:n]),
                      reads=[("AO", a, 0), ("AO", a, 1)], writes=[("ao", cidx, tok0)])

            pending = []

            def flush():
                while pending:
                    pending.pop(0)()

            def unit4(c, tok0, n, kcs, a):
                lanes = []
                for hh in range(2):
                    for j in range(2):
                        rb = hh * 64 + j * 32
                        lanes.append((KD[rb:rb + 32, c, :], QD[rb:rb + 32, c, tok0:tok0 + n], VX[:, 2 * c + hh], 4 + 2 * hh + j, (rb, 0)))
                attn_multi(lanes, kcs, n, 32.0 ** -0.5, PT4, [0], ["KVall", ("QK", "q", c, tok0)], hook=flush)
                flush()
                post_a(n)
                pending.append(lambda: post_b(n, a, 4 + c, tok0))

            ui = 0
            for c in range(2):
                for (tok0, n, kcs) in q_tiles(need_ctx):
                    unit4(c, tok0, n, kcs, ui % 2)
                    ui += 1
            flush()

        def phase_mla(l, need_ctx):
            A.reset(H_BYTES)
            hT = hT_view()
            cqnA = A.bf16(T)
            cqnB = A.bf16(T)
            ckvn = A.bf16(T)
            krr = A.bf16(T)
            VXf = A.bf16(4 * NT128 * 128)
            VX = VXf.rearrange("p (h t c) -> p h t c", h=4, t=NT128)
            win = A.bf16(8 * 512).rearrange("p (k n) -> p k n", k=8)
            wuq = A.bf16(2 * 384).rearrange("p (k n) -> p k n", k=2)
            wuqs = A.bf16(2 * 384).rearrange("p (k n) -> p k n", k=2)
            wkk = A.bf16(256)
            wkv = A.bf16(256)
            rc = [A.f32(512) for _ in range(2)]
            rs = [A.f32(512) for _ in range(2)]
            sq = [A.bf16(512) for _ in range(2)]
            rr = [A.f32(512) for _ in range(2)]
            t1 = [A.f32(512) for _ in range(2)]
            t2 = [A.f32(512) for _ in range(2)]
            PT = [A.bf16(1024) for _ in range(3)]
            AO = [A.bf16(512) for _ in range(2)]
            rec = [A.f32(512) for _ in range(2)]
            osb = [A.f32(512) for _ in range(2)]
            load_win(20 * 128, 24 * 128, win, "winA")
            S.dma("pool", "wuq0", lambda e: e.dma_start(out=wuq[:, 0, :], in_=w_uq_d[l][0:128, :]), writes=["wuq0"])
            S.dma("pool", "wuq1", lambda e: e.dma_start(out=wuq[0:64, 1, :], in_=w_uq_d[l][128:192, :]), writes=["wuq1"])
            S.dma("pool", "wuqs0", lambda e: e.dma_start(out=wuqs[:, 0, :], in_=w_uqs_d[l][0:128, :]), writes=["wuqs0"])
            S.dma("pool", "wuqs1", lambda e: e.dma_start(out=wuqs[0:64, 1, :], in_=w_uqs_d[l][128:192, :]), writes=["wuqs1"])
            S.dma("pool", "wkk", lambda e: e.dma_start(out=wkk, in_=w_ukvk_d[l]), writes=["wkk"])
            S.dma("pool", "wkv", lambda e: e.dma_start(out=wkv, in_=w_ukvv_d[l]), writes=["wkv"])
            WUQ = ["wuq0", "wuq1", "wuqs0", "wuqs1"]
            S.op("pool", lambda e: e.memset(VXf.rearrange("p (g c) -> p g c", c=128)[:, :, 64:128], 1.0), writes=["VXones"])
            S.op("pool", lambda e: e.memset(tiny[:, 0:1], 0.0), reads=[("hT", t) for t in range(NT128)], writes=["hTall"])
            S.op("dve", lambda e: e.tensor_scalar(tiny[:, 16:18], smallp[:, l * 8 + 5:l * 8 + 7], math.sqrt(192.0), 0.0, ALU.mult, ALU.add),
                 reads=["smallp"], writes=["gcq"])
            S.op("dve", lambda e: e.tensor_scalar(tiny[:, 18:19], smallp[:, l * 8 + 7:l * 8 + 8], math.sqrt(128.0), 0.0, ALU.mult, ALU.add),
                 reads=["smallp"], writes=["gckv"])

            def ptile(tok0, n, sl):
                load_rope(rc[sl], rs[sl], ropeB_c_d, ropeB_s_d, tok0, n, sl)
                proj_fm(0, win, 0, hT, tok0, n, "winA")
                proj_fm(1, win, 1, hT, tok0, n, "winA")
                proj_fm(2, win, 2, hT, tok0, n, "winA")
                proj_fm(3, win, 3, hT, tok0, n, "winA")
                S.op("act", lambda e: e.activation(sq[0][:, 0:n], ps[0][:, 0:n], AF.Square), reads=[PK(0)], writes=[("sq", 0)])
                S.op("act", lambda e: e.activation(sq[1][0:64, 0:n], ps[1][0:64, 0:n], AF.Square), reads=[PK(1)], writes=[("sq", 1)])
                S.op("pe", lambda e: e.matmul(ps[4][:, 0:n], ones, sq[0][:, 0:n], start=True, stop=False), reads=[("sq", 0), "cst"], writes=[PK(4)])
                S.op("pe", lambda e: e.matmul(ps[4][:, 0:n], ones[0:64, :], sq[1][0:64, 0:n], start=False, stop=True), reads=[("sq", 1), "cst"], writes=[PK(4)])
                S.op("act", lambda e: e.activation(rr[0][:, 0:n], ps[4][:, 0:n], AF.Ln, bias=cvec[:, 2:2 + 1], scale=1.0), reads=[PK(4), "cvec"], writes=[("rr", 0)])
                S.op("act", lambda e: e.activation(rr[0][:, 0:n], rr[0][:, 0:n], AF.Exp, scale=-0.5), reads=[("rr", 0)], writes=[("rr", 0)])
                S.op("dve", lambda e: e.scalar_tensor_tensor(cqnA[:, tok0:tok0 + n], ps[0][:, 0:n], tiny[:, 16:17], rr[0][:, 0:n], ALU.mult, ALU.mult),
                     reads=[PK(0), ("rr", 0), "gcq"], writes=[("cqnA", tok0)])
                S.op("dve", lambda e: e.scalar_tensor_tensor(cqnB[0:64, tok0:tok0 + n], ps[1][0:64, 0:n], tiny[0:64, 17:18], rr[0][0:64, 0:n], ALU.mult, ALU.mult),
                     reads=[PK(1), ("rr", 0), "gcq"], writes=[("cqnB", tok0)])
                S.op("act", lambda e: e.activation(sq[0][:, 0:n], ps[3][:, 0:n], AF.Square), reads=[PK(3)], writes=[("sq", 0)])
                S.op("pe", lambda e: e.matmul(ps[5][:, 0:n], ones, sq[0][:, 0:n], start=True, stop=True), reads=[("sq", 0), "cst"], writes=[PK(5)])
                S.op("act", lambda e: e.activation(rr[1][:, 0:n], ps[5][:, 0:n], AF.Ln, bias=cvec[:, 3:3 + 1], scale=1.0), reads=[PK(5), "cvec"], writes=[("rr", 1)])
                S.op("act", lambda e: e.activation(rr[1][:, 0:n], rr[1][:, 0:n], AF.Exp, scale=-0.5), reads=[("rr", 1)], writes=[("rr", 1)])
                S.op("dve", lambda e: e.scalar_tensor_tensor(ckvn[:, tok0:tok0 + n], ps[3][:, 0:n], tiny[:, 18:19], rr[1][:, 0:n], ALU.mult, ALU.mult),
                     reads=[PK(3), ("rr", 1), "gckv"], writes=[("ckvn", tok0)])
                S.op("dve", lambda e: e.tensor_tensor(t1[0][64:96, 0:n], ps[1][64:96, 0:n], rc[sl][64:96, 0:n], ALU.mult),
                     reads=[PK(1), ("rc", sl)], writes=[("t1", 0)])
                S.op("dve", lambda e: e.tensor_tensor(t2[0][64:96, 0:n], ps[2][64:96, 0:n], rs[sl][64:96, 0:n], ALU.mult),
                     reads=[PK(2), ("rs", sl)], writes=[("t2", 0)])
                S.op("pool", lambda e: e.tensor_tensor(krr[64:96, tok0:tok0 + n], t1[0][64:96, 0:n], t2[0][64:96, 0:n], ALU.add),
                     reads=[("t1", 0), ("t2", 0)], writes=[("krr", tok0)])
            for it, (tok0, n) in enumerate(tok_tiles()):
                ptile(tok0, n, it % 2)

            def vtile(t):
                b = 6 + t % 2
                S.op("pe", lambda e: e.matmul(ps[b][:, 0:256], ckvn[:, t * 128:(t + 1) * 128], wkv, start=True, stop=True),
                     reads=["wkv", ("ckvn", (t // 4) * 512)], writes=[PK(b)])
                S.op("dve", lambda e: e.tensor_copy(VX[:, :, t, 0:64], ps[b][:, 0:256].rearrange("p (h c) -> p h c", h=4)),
                     reads=[PK(b), "VXones"], writes=[("VX", t)])
            for t in range(NT128):
                vtile(t)
            S.barrier()
            QM = A.t[:, 0:H_BYTES // 8].bitcast(BF16).rearrange("p (h n) -> p h n", h=4)
            KM = A.t[:, H_BYTES // 8:H_BYTES // 4].bitcast(BF16).rearrange("p (h n) -> p h n", h=4)

            def uhead(tok0, n, sl, h, doq):
                u = h % 2
                if doq:
                    for (bank, w) in ((2 * u, wuq), (2 * u + 1, wuqs)):
                        S.op("pe", lambda e, bank=bank, w=w: e.matmul(ps[bank][0:96, 0:n], w[:, 0, h * 96:(h + 1) * 96], cqnA[:, tok0:tok0 + n],
                                                                     start=True, stop=False),
                             reads=WUQ, writes=[PK(bank)])
                        S.op("pe", lambda e, bank=bank, w=w: e.matmul(ps[bank][0:96, 0:n], w[0:64, 1, h * 96:(h + 1) * 96], cqnB[0:64, tok0:tok0 + n],
                                                                     start=False, stop=True),
                             reads=WUQ, writes=[PK(bank)])
                    S.op("act", lambda e: e.activation(QM[0:64, h, tok0:tok0 + n], ps[2 * u][0:64, 0:n], AF.Copy),
                         reads=[PK(2 * u)], writes=[("QMn", h, tok0)])
                    S.op("dve", lambda e: e.tensor_tensor(t1[u][64:96, 0:n], ps[2 * u][64:96, 0:n], rc[sl][64:96, 0:n], ALU.mult),
                         reads=[PK(2 * u), ("rc", sl)], writes=[("t1", u)])
                    S.op("dve", lambda e: e.tensor_tensor(t2[u][64:96, 0:n], ps[2 * u + 1][64:96, 0:n], rs[sl][64:96, 0:n], ALU.mult),
                         reads=[PK(2 * u + 1), ("rs", sl)], writes=[("t2", u)])
                    S.op("pool", lambda e: e.tensor_tensor(QM[64:96, h, tok0:tok0 + n], t1[u][64:96, 0:n], t2[u][64:96, 0:n], ALU.add),
                         reads=[("t1", u), ("t2", u)], writes=[("QMr", h, tok0)])
                kb = 4 + u
                S.op("pe", lambda e: e.matmul(ps[kb][0:64, 0:n], wkk[:, h * 64:(h + 1) * 64], ckvn[:, tok0:tok0 + n], start=True, stop=True),
                     reads=["wkk"], writes=[PK(kb)])
                S.op("dve", lambda e: e.tensor_copy(KM[0:64, h, tok0:tok0 + n], ps[kb][0:64, 0:n]),
                     reads=[PK(kb)], writes=[("KMn", h, tok0)])
                S.op("pool", lambda e: e.tensor_copy(KM[64:96, h, tok0:tok0 + n], krr[64:96, tok0:tok0 + n]),
                     writes=[("KMr", h, tok0)])
            for it, (tok0, n) in enumerate(tok_tiles()):
                is_ctx = tok0 >= S_LAT
                sl = it % 2
                load_rope(rc[sl], rs[sl], ropeB_c_d, ropeB_s_d, tok0, n, sl)
                for h in range(4):
                    uhead(tok0, n, sl, h, (not is_ctx) or need_ctx)
            S.op("pool", lambda e: e.memset(tiny[:, 1:2], 0.0),
                 reads=[(kk, h, tk) for kk in ("KMn", "KMr") for h in range(4) for (tk, _n) in tok_tiles()],
                 writes=["KVall"])

            def unit(c, hh, tok0, n, kcs, ob, u, a):
                h = 2 * c + hh
                attn_unit(KM[0:96, h, :], QM[0:96, h, tok0:tok0 + n], VX[:, h], kcs, n, 96.0 ** -0.5, ob, PT, [0, 2, 4],
                          ["KVall", ("QMn", h, tok0), ("QMr", h, tok0)], [])
                S.op("dve", lambda e: e.tensor_copy(rec[u][0:64, 0:n], ps[ob][64:128, 0:n]),
                     reads=[PK(ob)], writes=[("rec", u)])
                S.op("dve", lambda e: e.tensor_copy(osb[u][0:64, 0:n], ps[ob][0:64, 0:n]),
                     reads=[PK(ob)], writes=[("osb", u)])
                S.op("dve", lambda e: e.reciprocal(rec[u][0:64, 0:n], rec[u][0:64, 0:n]),
                         reads=[("rec", u)], writes=[("rec", u)])
                S.op("dve", lambda e: e.tensor_tensor(AO[a][hh * 64:hh * 64 + 64, 0:n], osb[u][0:64, 0:n], rec[u][0:64, 0:n], ALU.mult),
                     reads=[("osb", u), ("rec", u)], writes=[("AO", a, hh)])

            def store(a, cidx, tok0, n):
                S.dma("sp", ("AOst", a), lambda e: e.dma_start(out=ao_d[cidx, :, tok0:tok0 + n], in_=AO[a][:, 0:n]),
                      reads=[("AO", a, 0), ("AO", a, 1)], writes=[("ao", cidx, tok0)])
            un = 0
            for c in range(2):
                for qi, (tok0, n, kcs) in enumerate(q_tiles(need_ctx)):
                    a = (c + qi) % 2
                    for hh in range(2):
                        unit(c, hh, tok0, n, kcs, 6 + un % 2, un % 2, a)
                        un += 1
                    store(a, 6 + c, tok0, n)

        def phase_C(l, need_ctx, moe):
            A.reset(H_BYTES)
            hT = hT_view()
            wout = A.bf16(8 * D).rearrange("p (k n) -> p k n", k=8)
            bufs = alloc_norm_bufs()
            xt = [A.f32(D) for _ in range(2)]
            xn = [A.f32(D) for _ in range(2)]
            aot = [A.bf16(8 * 128).rearrange("p (k n) -> p k n", k=8) for _ in range(2)]
            nr = 2 if need_ctx else 1
            G2 = [A.f32(D) for _ in range(nr)]
            Fm = [A.f32(D) for _ in range(nr)]
            Fa = [A.f32(D) for _ in range(nr)]
            S.dma("pool", "wout", lambda e: e.dma_start(out=wout, in_=w_out_d[l].rearrange("(k p) n -> p k n", p=128)), writes=["wout"])
            for r in range(nr):
                load_bc(G2[r], l, r, 2, ("G2", r))
                load_bc(Fm[r], l, r, 4, ("Fm", r))
                load_bc(Fa[r], l, r, 3, ("Fa", r))
            wr = lg = None
            if moe:
                wr = A.bf16(64).rearrange("p (k n) -> p k n", k=8)
                lg = A.f32(8)
                S.dma("pool", "wr", lambda e: e.dma_start(out=wr, in_=moe_router_d[0].rearrange("(k p) n -> p k n", p=128)), writes=["wr"])
            S.op("dve", lambda e: e.memset(ssq[:], 0.0), writes=[("C", "ssq", t) for t in range(NT128)])
            ntl = NT128 if need_ctx else 32
            g3 = gates[:].rearrange("p (t e) -> p t e", e=8)
            tn = tiny
            dfr = []

            def cload(t):
                sl = t % 2
                S.dma("sp", ("aot", sl), lambda e: e.dma_start(out=aot[sl], in_=ao_d[:, :, t * 128:(t + 1) * 128].rearrange("c p n -> p c n")),
                      reads=[("ao", c, (t // 4) * 512) for c in range(8)], writes=[("aot", sl)])
                S.dma("sp", ("xt", sl), lambda e: e.dma_start(out=xt[sl], in_=src_tile(l, t)), reads=[("xs", t)], writes=[("xt", sl)])

            def ctile(t):
                r = 0 if t < 32 else 1
                sl = t % 2
                for half in range(2):
                    b = 2 * sl + half
                    for k in range(8):
                        S.op("pe", lambda e, b=b, k=k, half=half: e.matmul(ps[b][:, :], aot[sl][:, k, :], wout[:, k, half * 512:(half + 1) * 512],
                                                                         start=(k == 0), stop=(k == 7)),
                             reads=[("aot", sl), "wout"], writes=[PK(b)])
                    S.op("dve", lambda e, b=b, half=half: e.tensor_tensor(xn[sl][:, half * 512:(half + 1) * 512], ps[b][:, :],
                                                                        G2[r][:, half * 512:(half + 1) * 512], ALU.mult),
                         reads=[PK(b), ("G2", r)], writes=[("xn", sl)])
                S.op("pool", lambda e: e.tensor_tensor(xn[sl], xn[sl], xt[sl], ALU.add),
                     reads=[("xn", sl), ("xt", sl)], writes=[("xn", sl)])
                S.dma("sp", ("xst", sl), lambda e: e.dma_start(out=xs_d[t * 128:(t + 1) * 128, :], in_=xn[sl]),
                      reads=[("xn", sl)], writes=[("xs", t)])

            def npart(t):
                r = 0 if t < 32 else 1
                sl = t % 2
                norm_mod_T("C", t, xn[sl], ("xn", sl), Fm[r], ("Fm", r), Fa[r], ("Fa", r), bufs, hT, deferred=dfr)
                if moe:
                    dfr.append(lambda: router(t))

            def router(t):
                if True:
                    for k in range(8):
                        S.op("pe", lambda e, k=k: e.matmul(ps[4][:, 0:8], hT[:, k, t * 128:(t + 1) * 128], wr[:, k, :], start=(k == 0), stop=(k == 7)),
                             reads=[("hT", t), "wr"], writes=[PK(4)])
                    S.op("dve", lambda e: e.tensor_copy(lg, ps[4][:, 0:8]), reads=[PK(4)], writes=["lg"])
                    S.op("dve", lambda e: e.tensor_reduce(tn[:, 20:21], lg, AX.X, ALU.max), reads=["lg"], writes=["m1"])
                    S.op("dve", lambda e: e.tensor_scalar(tn[:, 24:32], lg, tn[:, 20:21], 0.0, ALU.is_equal, ALU.add), reads=["lg", "m1"], writes=["eq1"])
                    S.op("dve", lambda e: e.scalar_tensor_tensor(tn[:, 32:40], tn[:, 24:32], -1e30, lg, ALU.mult, ALU.add), reads=["eq1", "lg"], writes=["lg2"])
                    S.op("dve", lambda e: e.tensor_reduce(tn[:, 21:22], tn[:, 32:40], AX.X, ALU.max), reads=["lg2"], writes=["m2"])
                    S.op("dve", lambda e: e.tensor_scalar(tn[:, 40:48], tn[:, 32:40], tn[:, 21:22], 0.0, ALU.is_equal, ALU.add), reads=["lg2", "m2"], writes=["eq2"])
                    S.op("dve", lambda e: e.tensor_tensor(tn[:, 22:23], tn[:, 21:22], tn[:, 20:21], ALU.subtract), reads=["m1", "m2"], writes=["dm"])
                    S.op("act", lambda e: e.activation(tn[:, 23:24], tn[:, 22:23], AF.Exp), reads=["dm"], writes=["edm"])
                    S.op("dve", lambda e: e.tensor_scalar(tn[:, 48:49], tn[:, 23:24], 1.0, 0.0, ALU.add, ALU.add), reads=["edm"], writes=["den"])
                    S.op("dve", lambda e: e.reciprocal(tn[:, 49:50], tn[:, 48:49]), reads=["den"], writes=["w1"])
                    S.op("dve", lambda e: e.tensor_tensor(tn[:, 50:51], tn[:, 23:24], tn[:, 49:50], ALU.mult), reads=["edm", "w1"], writes=["w2"])
                    S.op("dve", lambda e: e.tensor_scalar(tn[:, 52:60], tn[:, 24:32], tn[:, 49:50], 0.0, ALU.mult, ALU.add), reads=["eq1", "w1"], writes=["g1"])
                    S.op("dve", lambda e: e.scalar_tensor_tensor(g3[:, t, :], tn[:, 40:48], tn[:, 50:51], tn[:, 52:60], ALU.mult, ALU.add),
                         reads=["eq2", "w2", "g1"], writes=[("gates", t)])
            nkeep = 2 if moe else 1
            cload(0)
            ctile(0)
            for t in range(ntl):
                if t + 1 < ntl:
                    cload(t + 1)
                    ctile(t + 1)
                npart(t)
                while len(dfr) > nkeep:
                    dfr.pop(0)()
            while dfr:
                dfr.pop(0)()

        def phase_D(l, need_ctx, moe):
            ntok = T if need_ctx else S_LAT
            A.reset(H_BYTES)
            hT = hT_view()
            if moe:
                units = [(moe_w_in_d[0, e][:, 0:1792], moe_w_in_d[0, e][:, 1792:3584], moe_w_out_d[0, e], e) for e in range(8)]
                nf = 14
            else:
                units = [(ffn_w_in_d[0][:, f0:f0 + 1408], ffn_w_in_d[0][:, 2816 + f0:2816 + f0 + 1408], ffn_w_out_d[0][f0:f0 + 1408, :], None)
                         for f0 in (0, 1408)]
                nf = 11
            wg = A.bf16(8 * nf * 128).rearrange("p (k n) -> p k n", k=8)
            wu = A.bf16(8 * nf * 128).rearrange("p (k n) -> p k n", k=8)
            wo = A.bf16(nf * D).rearrange("p (j n) -> p j n", j=nf)
            aT = [A.bf16(nf * 512).rearrange("p (j n) -> p j n", j=nf) for _ in range(2)]
            xt = [A.f32(D) for _ in range(2)]
            t1 = [A.f32(D) for _ in range(2)]
            sg = [A.f32(512) for _ in range(2)]
            nr = 2 if need_ctx else 1
            G5 = [A.f32(D) for _ in range(nr)]
            for r in range(nr):
                load_bc(G5[r], l, r, 5, ("G5", r))
            g3 = gates[:].rearrange("p (t e) -> p t e", e=8)
            tiles = [(tt * 512, 512) for tt in range(8)] + ([(S_LAT, 256)] if need_ctx else [])
            S.op("pool", lambda e: e.memset(tiny[:, 0:1], 0.0), reads=[("hT", t) for t in range(ntok // 128)], writes=["hTall"])

            def fchunk(tok0, n, asl, j):
                u = j % 2
                S.begin_group()
                for k in range(8):
                    S.op("pe", lambda e, k=k: e.matmul(ps[u][:, 0:n], wg[:, k, j * 128:(j + 1) * 128], hT[:, k, tok0:tok0 + n],
                                                      start=(k == 0), stop=(k == 7)),
                         reads=["wg", "hTall"], writes=[PK(u)])
                for k in range(8):
                    S.op("pe", lambda e, k=k: e.matmul(ps[2 + u][:, 0:n], wu[:, k, j * 128:(j + 1) * 128], hT[:, k, tok0:tok0 + n],
                                                      start=(k == 0), stop=(k == 7)),
                         reads=["wu", "hTall"], writes=[PK(2 + u)])
                S.end_group()
                S.op("act", lambda e: e.activation(sg[u][:, 0:n], ps[u][:, 0:n], AF.Silu), reads=[PK(u)], writes=[("sg", u)])
                S.op("dve", lambda e: e.tensor_tensor(aT[asl][:, j, 0:n], sg[u][:, 0:n], ps[2 + u][:, 0:n], ALU.mult),
                     reads=[("sg", u), PK(2 + u)], writes=[("aT", asl, j)])

            def ytile(tok0, asl, sub, eidx):
                t = tok0 // 128 + sub
                r = 0 if t < 32 else 1
                xsl = t % 2
                S.dma("sp", ("xt", xsl), lambda e: e.dma_start(out=xt[xsl], in_=xs_d[t * 128:(t + 1) * 128, :]),
                      reads=[("xs", t)], writes=[("xt", xsl)])
                S.begin_group()
                for half in range(2):
                    b = 4 + 2 * xsl + half
                    for j in range(nf):
                        S.op("pe", lambda e, b=b, j=j, half=half: e.matmul(
                            ps[b][:, :], aT[asl][:, j, sub * 128:(sub + 1) * 128], wo[:, j, half * 512:(half + 1) * 512],
                            start=(j == 0), stop=(j == nf - 1)),
                            reads=[("aT", asl, j), "wo"], writes=[PK(b)])
                    if half == 1:
                        S.end_group()
                    sc = g3[:, t, eidx:eidx + 1] if eidx is not None else 1.0
                    rd = [PK(b), ("G5", r)] + ([("gates", t)] if eidx is not None else [])
                    S.op("dve", lambda e, b=b, half=half, sc=sc: e.scalar_tensor_tensor(
                        t1[xsl][:, half * 512:(half + 1) * 512], ps[b][:, :], sc, G5[r][:, half * 512:(half + 1) * 512], ALU.mult, ALU.mult),
                        reads=rd, writes=[("t1", xsl)])
                S.op("dve", lambda e: e.tensor_tensor(t1[xsl], t1[xsl], xt[xsl], ALU.add),
                     reads=[("t1", xsl), ("xt", xsl)], writes=[("t1", xsl)])
                S.dma("sp", ("xst", xsl), lambda e: e.dma_start(out=xs_d[t * 128:(t + 1) * 128, :], in_=t1[xsl]),
                      reads=[("t1", xsl)], writes=[("xs", t)])

            def load_unit(gd, ud, od):
                S.dma("pool", "wg", lambda e: e.dma_start(out=wg, in_=gd.rearrange("(k p) n -> p k n", p=128)), writes=["wg"])
                S.dma("pool", "wu", lambda e: e.dma_start(out=wu, in_=ud.rearrange("(k p) n -> p k n", p=128)), writes=["wu"])
                S.dma("pool", "wo", lambda e: e.dma_start(out=wo, in_=od.rearrange("(j p) n -> p j n", p=128)), writes=["wo"])
            ti = 0
            for (gd, ud, od, eidx) in units:
                load_unit(gd, ud, od)
                for (tok0, n) in tiles:
                    asl = ti % 2
                    ti += 1
                    for j in range(nf):
                        fchunk(tok0, n, asl, j)
                    for sub in range(n // 128):
                        ytile(tok0, asl, sub, eidx)

        def phase_final():
            A.reset(0)
            gf = A.f32(D)
            xt = [A.f32(D) for _ in range(3)]
            sqj = [A.bf16(D) for _ in range(2)]
            ot = [A.f32(D) for _ in range(2)]
            S.dma("sp", "gf", lambda e: e.dma_start(out=gf, in_=g_final_d[0].partition_broadcast(128)), writes=["gf"])
            S.op("dve", lambda e: e.memset(ssq[:], 0.0), writes=[("F", "ssq", t) for t in range(32)])

            def fload(t):
                s3 = t % 3
                S.dma("sp", ("xt", s3), lambda e: e.dma_start(out=xt[s3], in_=xs_d[t * 128:(t + 1) * 128, :]),
                      reads=[("xs", t)], writes=[("xt", s3)])

            def ftile(t):
                s3 = t % 3
                sl = t % 2
                S.op("act", lambda e: e.activation(sqj[sl], xt[s3], AF.Square, accum_out=ssq[:, t:t + 1]),
                     reads=[("xt", s3)], writes=[("sqj", sl), ("F", "ssq", t)])
                S.op("act", lambda e: e.activation(rstd[:, t:t + 1], ssq[:, t:t + 1], AF.Ln, bias=cvec[:, 0:1], scale=1.0 / D),
                     reads=[("F", "ssq", t)], writes=[("F", "rstd", t)])
                S.op("act", lambda e: e.activation(rstd[:, t:t + 1], rstd[:, t:t + 1], AF.Exp, scale=-0.5),
                     reads=[("F", "rstd", t)], writes=[("F", "rstd", t)])
                S.op("dve", lambda e: e.scalar_tensor_tensor(ot[sl], xt[s3], rstd[:, t:t + 1], gf, ALU.mult, ALU.mult),
                     reads=[("xt", s3), ("F", "rstd", t), "gf"], writes=[("ot", sl)])
                S.dma("sp", ("ost", sl), lambda e: e.dma_start(out=out_d[t * 128:(t + 1) * 128, :], in_=ot[sl]),
                      reads=[("ot", sl)], writes=[("out", t)])
            fload(0)
            fload(1)
            for t in range(32):
                if t + 2 < 32:
                    fload(t + 2)
                ftile(t)
            S.final_waits += [("ost", 0), ("ost", 1)]

        for l in range(n_layers):
            need_ctx = l < 1
            moe = (l % 2 == 1)
            w_inx_d_l[0] = w_inx_d[l]
            phase_A(l)
            S.barrier()
            if debug == "hT" and l == 0:
                S.dma("sp", "dbg", lambda e: e.dma_start(out=dbg_d, in_=A.t[:, 0:H_BYTES // 4].bitcast(BF16)), writes=["dbg"])
                S.final_waits += ["dbg"]
                break
            phase_gqa(l, need_ctx)
            S.barrier()
            phase_diff(l, need_ctx)
            S.barrier()
            phase_mla(l, need_ctx)
            S.barrier()
            phase_C(l, need_ctx, moe)
            S.barrier()
            phase_D(l, need_ctx, moe)
            S.barrier()
        phase_final()
        S.run()
    return nc


_CACHE = {}


def _host_inputs(inputs):
    f = lambda a: np.ascontiguousarray(np.asarray(a, dtype=np.float32))
    cols = _winx_cols()
    w_in = f(inputs["w_in"])
    shared = {
        "w_mod": f(inputs["w_mod"]), "b_mod": f(inputs["b_mod"]),
        "g_attn": f(inputs["g_attn"]), "g_ffn": f(inputs["g_ffn"]),
        "g_final": f(inputs["g_final"]).reshape(1, D),
        "w_inx": np.ascontiguousarray(w_in[:, :, cols]),
        "w_out": f(inputs["w_out"]),
        "ffn_w_in": f(inputs["ffn_w_in"]), "ffn_w_out": f(inputs["ffn_w_out"]),
        "moe_router": f(inputs["moe_router"]), "moe_w_in": f(inputs["moe_w_in"]), "moe_w_out": f(inputs["moe_w_out"]),
    }
    p = np.arange(128)
    sp = np.zeros((128, 16), np.float32)
    gq, gk = f(inputs["gqa_gq"]), f(inputs["gqa_gk"])
    gs, gcq, gckv = f(inputs["diff_gsub"]), f(inputs["mla_gcq"]), f(inputs["mla_gckv"])
    for l in range(2):
        sp[:, l * 8 + 0] = gq[l][p % 64]
        sp[:, l * 8 + 1] = gq[l][(p % 64) ^ 1]
        sp[:, l * 8 + 2] = gk[l][p % 64]
        sp[:, l * 8 + 3] = gk[l][(p % 64) ^ 1]
        sp[:, l * 8 + 4] = gs[l][p % 64]
        sp[:, l * 8 + 5] = gcq[l][0:128]
        sp[:, l * 8 + 6] = gcq[l][128 + (p % 64)]
        sp[:, l * 8 + 7] = gckv[l]
    shared["smallp"] = sp
    shared["lamv"] = np.ascontiguousarray(np.concatenate(
        [f(inputs["diff_lq1"]), f(inputs["diff_lk1"]), f(inputs["diff_lq2"]), f(inputs["diff_lk2"])], axis=1))
    wuq = f(inputs["mla_wuq"])
    swc = np.arange(384)
    hh, rr = swc // 96, swc % 96
    swc = np.where(rr >= 64, hh * 96 + 64 + ((rr - 64) ^ 1), swc)
    shared["w_uq"] = wuq
    shared["w_uqs"] = np.ascontiguousarray(wuq[:, :, swc])
    wukv = f(inputs["mla_wukv"])
    kc = np.concatenate([np.arange(h * 128, h * 128 + 64) for h in range(4)])
    vc = np.concatenate([np.arange(h * 128 + 64, h * 128 + 128) for h in range(4)])
    shared["w_ukvk"] = np.ascontiguousarray(wukv[:, :, kc])
    shared["w_ukvv"] = np.ascontiguousarray(wukv[:, :, vc])
    ac, as_, bc, bs = _rope_tables()
    shared.update({"ropeA_c": ac, "ropeA_s": as_, "ropeB_c": bc, "ropeB_s": bs})
    cst = np.zeros((128, 384), np.float32)
    cst[:, 0:128] = np.eye(128, dtype=np.float32)
    cst[:, 128:256] = 1.0
    cst[0:64, 256:320] = 1.0
    cst[64:128, 320:384] = 1.0
    shared["cst"] = cst
    x = f(inputs["x"])
    ctx = f(inputs["ctx"])
    c = f(inputs["c"])
    c_ctx = f(inputs["c_ctx"])
    maps = []
    for b in range(x.shape[0]):
        m = dict(shared)
        m["x"] = x[b]
        m["ctx"] = ctx[b]
        m["cc"] = np.ascontiguousarray(np.stack([c[b], c_ctx], axis=0))
        maps.append(m)
    return maps


def kernel(**inputs):
    maps = _host_inputs(inputs)
    if "nc" not in _CACHE:
        _CACHE["nc"] = build()
    nc = _CACHE["nc"]
    res = run_bass_kernel_spmd(nc, maps, core_ids=list(range(len(maps))))
    return np.stack([np.asarray(r["out"]).astype(np.float32) for r in res.results], axis=0)
```

```python
import math
from contextlib import ExitStack

import numpy as np
import concourse.bass as bass
import concourse.mybir as mybir
from concourse.bass_utils import run_bass_kernel_spmd

F32 = mybir.dt.float32
BF16 = mybir.dt.bfloat16
AF = mybir.ActivationFunctionType
ALU = mybir.AluOpType
AX = mybir.AxisListType

D = 1024
S_LAT = 4096
CTX = 256
T = S_LAT + CTX
NT128 = T // 128
EPS = 1e-6
ENGINES = ("pe", "act", "dve", "pool", "sp")

FM_CHUNKS = 24
NX = FM_CHUNKS * 128 + 384
VG_OFF = FM_CHUNKS * 128
VD_OFF = VG_OFF + 128


def _winx_cols():
    def sw(a):
        return [i ^ 1 for i in a]
    cols = []
    qg = list(range(0, 512))
    for c in range(4):
        cols += qg[c * 128:(c + 1) * 128]
    for c in range(4):
        cols += sw(qg[c * 128:(c + 1) * 128])
    for kv in range(2):
        k = list(range(512 + kv * 64, 512 + kv * 64 + 64))
        cols += k + k
    for kv in range(2):
        k = sw(list(range(512 + kv * 64, 512 + kv * 64 + 64)))
        cols += k + k
    qd = list(range(768, 1024))
    for c in range(2):
        cols += qd[c * 128:(c + 1) * 128]
    for c in range(2):
        cols += sw(qd[c * 128:(c + 1) * 128])
    kd = list(range(1024, 1280))
    for c in range(2):
        cols += kd[c * 128:(c + 1) * 128]
    for c in range(2):
        cols += sw(kd[c * 128:(c + 1) * 128])
    cols += list(range(1536, 1664))
    b = list(range(1664, 1728)) + list(range(1856, 1888))
    cols += b + [1664] * 32
    bs = list(range(1664, 1728)) + sw(list(range(1856, 1888)))
    cols += bs + [1664] * 32
    cols += list(range(1728, 1856))
    assert len(cols) == FM_CHUNKS * 128
    cols += list(range(640, 768))
    cols += list(range(1280, 1536))
    assert len(cols) == NX
    return np.array(cols, dtype=np.int64)


def _rope_tables():
    t = np.arange(S_LAT)
    rows = (t // 64).astype(np.float32)
    colsf = (t % 64).astype(np.float32)

    def tab(dim):
        quarter = dim // 4
        inv = (np.float32(10000.0) ** (-np.arange(quarter, dtype=np.float32) / np.float32(quarter))).astype(np.float32)
        ang = np.concatenate([rows[:, None] * inv, colsf[:, None] * inv], axis=-1).astype(np.float32)
        cos = np.cos(ang).astype(np.float32)
        sin = np.sin(ang).astype(np.float32)
        cd = np.repeat(cos, 2, axis=1)
        sd = np.repeat(sin, 2, axis=1)
        sd[:, 0::2] *= -1.0
        reps = 128 // dim
        cfull = np.ones((128, T), np.float32)
        sfull = np.zeros((128, T), np.float32)
        cfull[:, :S_LAT] = np.tile(cd.T, (reps, 1))
        sfull[:, :S_LAT] = np.tile(sd.T, (reps, 1))
        return cfull, sfull
    return tab(64) + tab(32)


class Op:
    __slots__ = ("eng", "emit", "deps", "needed", "tok", "dma", "gidx", "grp")

    def __init__(self, eng, emit, dma=None):
        self.eng = eng
        self.emit = emit
        self.deps = []
        self.needed = False
        self.tok = None
        self.dma = dma
        self.gidx = 0
        self.grp = None


class Sched:
    def __init__(self, nc, stack):
        self.nc = nc
        self.stack = stack
        self.ops = {e: [] for e in ENGINES}
        self.last_w = {}
        self.readers = {}
        self.esem = {e: stack.enter_context(nc.semaphore("es_" + e)) for e in ENGINES}
        self.dsem = {}
        self.dcum = {}
        self.dlast = {}
        self.final_waits = []
        self.bar_deps = []
        self.bar_gen = 0
        self.bar_seen = {e: 0 for e in ENGINES}
        self.nrec = 0
        self.cur_grp = None
        self.ngrp = 0

    def begin_group(self):
        self.ngrp += 1
        self.cur_grp = (self.ngrp, self.nrec)

    def end_group(self):
        self.cur_grp = None

    def _rec(self, op, reads, writes):
        deps = op.deps
        e = op.eng
        self.nrec += 1
        op.gidx = self.nrec
        op.grp = self.cur_grp
        if self.bar_seen[e] != self.bar_gen:
            self.bar_seen[e] = self.bar_gen
            deps.extend(self.bar_deps)
        lw = self.last_w
        rd = self.readers
        for k in reads:
            w = lw.get(k)
            if w is not None:
                deps.append(w)
            rd.setdefault(k, []).append(op)
        for k in writes:
            w = lw.get(k)
            if w is not None:
                deps.append(w)
            r = rd.get(k)
            if r:
                deps.extend(x for x in r if x is not op)
            rd[k] = []
            lw[k] = op
        self.ops[e].append(op)
        return op

    def op(self, eng, emit, reads=(), writes=()):
        return self._rec(Op(eng, emit), reads, writes)

    def dma(self, eng, chan, emit, reads=(), writes=()):
        if chan not in self.dsem:
            self.dsem[chan] = self.stack.enter_context(self.nc.semaphore("ds%d" % len(self.dsem)))
            self.dcum[chan] = 0
        self.dcum[chan] += 16
        op = Op(eng, emit, dma=(chan, self.dcum[chan]))
        self.dlast[chan] = op
        return self._rec(op, reads, writes)

    def barrier(self):
        deps = []
        for e in ENGINES:
            for op in reversed(self.ops[e]):
                if op.dma is None:
                    deps.append(op)
                    break
        deps.extend(self.dlast.values())
        self.bar_deps = deps
        self.bar_gen += 1
        self.last_w = {}
        self.readers = {}

    def finalize(self):
        for e in ENGINES:
            for op in self.ops[e]:
                best = {}
                for d in op.deps:
                    if d.dma is not None or (d.eng == "pe" and e == "pe"):
                        continue
                    cur = best.get(d.eng)
                    if cur is None or d.gidx > cur.gidx:
                        best[d.eng] = d
                for d in best.values():
                    d.needed = True
        for e in ENGINES:
            n = 0
            for op in self.ops[e]:
                if op.dma is not None:
                    op.tok = (("dma", op.dma[0]), op.dma[1])
                elif op.needed:
                    n += 1
                    op.tok = (e, n)
        self.plan = {}
        for e in ENGINES:
            seen = {}
            plan = []
            hoist = {}
            for op in self.ops[e]:
                if op.grp is not None:
                    h = hoist.setdefault(op.grp[0], {})
                    for d in op.deps:
                        if d.tok is None or d.gidx > op.grp[1]:
                            continue
                        sk, v = d.tok
                        if h.get(sk, 0) < v:
                            h[sk] = v
            done_grp = set()
            for op in self.ops[e]:
                need = {}
                if op.grp is not None and op.grp[0] not in done_grp:
                    done_grp.add(op.grp[0])
                    need.update(hoist[op.grp[0]])
                for d in op.deps:
                    if d.tok is None:
                        continue
                    sk, v = d.tok
                    if need.get(sk, 0) < v:
                        need[sk] = v
                waits = []
                for sk, v in need.items():
                    if seen.get(sk, 0) < v:
                        seen[sk] = v
                        waits.append((sk, v))
                plan.append((waits, op))
            self.plan[e] = plan

    def _sem(self, sk):
        if isinstance(sk, tuple):
            return self.dsem[sk[1]]
        return self.esem[sk]

    def emit_engine(self, e, eng):
        for waits, op in self.plan[e]:
            for sk, v in waits:
                eng.wait_ge(self._sem(sk), v)
            ins = op.emit(eng)
            if op.dma is not None:
                ins.then_inc(self.dsem[op.dma[0]], 16)
            elif op.needed:
                ins.then_inc(self.esem[e], 1)
        if e == "sp":
            for chan in self.final_waits:
                eng.wait_ge(self.dsem[chan], self.dcum[chan])

    def run(self):
        self.finalize()
        with self.nc.Block() as block:
            @block.tensor
            def _(eng):
                self.emit_engine("pe", eng)

            @block.scalar
            def _(eng):
                self.emit_engine("act", eng)

            @block.vector
            def _(eng):
                self.emit_engine("dve", eng)

            @block.gpsimd
            def _(eng):
                self.emit_engine("pool", eng)

            @block.sync
            def _(eng):
                self.emit_engine("sp", eng)


class Arena:
    def __init__(self, nc, stack, nbytes):
        self.n = nbytes
        self.t = stack.enter_context(nc.sbuf_tensor("arena", [128, nbytes // 4], F32))
        self.off = 0

    def reset(self, off=0):
        self.off = off

    def f32(self, cols):
        a = self.off
        self.off += cols * 4
        assert self.off <= self.n, ("arena overflow", self.off, self.n)
        return self.t[:, a // 4:a // 4 + cols]

    def bf16(self, cols):
        c2 = (cols + 1) // 2 * 2
        a = self.off
        self.off += c2 * 2
        assert self.off <= self.n, ("arena overflow", self.off, self.n)
        return self.t[:, a // 4:a // 4 + c2 // 2].bitcast(BF16)[:, 0:cols]


ARENA_BYTES = 209200
H_BYTES = 8 * T * 2


def build(n_layers=2, debug=False):
    nc = bass.Bass("TRN2", target_bir_lowering=False)

    def din(name, shape):
        return nc.dram_tensor(name, list(shape), F32, kind="ExternalInput").ap()

    x_d = din("x", [S_LAT, D])
    ctx_d = din("ctx", [CTX, D])
    cc_d = din("cc", [2, D])
    w_mod_d = din("w_mod", [2, D, 6 * D])
    b_mod_d = din("b_mod", [2, 6 * D])
    g_attn_d = din("g_attn", [2, D])
    g_ffn_d = din("g_ffn", [2, D])
    g_final_d = din("g_final", [1, D])
    w_inx_d = din("w_inx", [2, D, NX])
    w_out_d = din("w_out", [2, D, D])
    smallp_d = din("smallp", [128, 16])
    lamv_d = din("lamv", [2, 128])
    w_uq_d = din("w_uq", [2, 192, 384])
    w_uqs_d = din("w_uqs", [2, 192, 384])
    w_ukvk_d = din("w_ukvk", [2, 128, 256])
    w_ukvv_d = din("w_ukvv", [2, 128, 256])
    ffn_w_in_d = din("ffn_w_in", [1, D, 5632])
    ffn_w_out_d = din("ffn_w_out", [1, 2816, D])
    moe_router_d = din("moe_router", [1, D, 8])
    moe_w_in_d = din("moe_w_in", [1, 8, D, 3584])
    moe_w_out_d = din("moe_w_out", [1, 8, 1792, D])
    ropeA_c_d = din("ropeA_c", [128, T])
    ropeA_s_d = din("ropeA_s", [128, T])
    ropeB_c_d = din("ropeB_c", [128, T])
    ropeB_s_d = din("ropeB_s", [128, T])
    cst_d = din("cst", [128, 384])
    out_d = nc.dram_tensor("out", [S_LAT, D], F32, kind="ExternalOutput").ap()
    skind = "ExternalOutput" if debug else "Internal"
    xs_d = nc.dram_tensor("xs", [T, D], F32, kind=skind).ap()
    ao_d = nc.dram_tensor("ao", [8, 128, T], BF16, kind=skind).ap()
    modv_d = nc.dram_tensor("modv", [2, 2, 6 * D], F32, kind=skind).ap()
    if debug:
        dbg_d = nc.dram_tensor("dbg", [128, 8 * T], BF16, kind="ExternalOutput").ap()

    with ExitStack() as st:
        S = Sched(nc, st)
        A = Arena(nc, st, ARENA_BYTES)
        psall = st.enter_context(nc.psum_tensor("psall", [128, 4096], F32))
        ps = [psall[:, i * 512:(i + 1) * 512] for i in range(8)]
        cstb = st.enter_context(nc.sbuf_tensor("cstb", [128, 384], BF16))
        smallp = st.enter_context(nc.sbuf_tensor("smallp_sb", [128, 16], F32))
        scc = st.enter_context(nc.sbuf_tensor("scc", [128, 16], F32))
        ssq = st.enter_context(nc.sbuf_tensor("ssq", [128, 40], F32))
        rstd = st.enter_context(nc.sbuf_tensor("rstd", [128, 40], F32))
        gates = st.enter_context(nc.sbuf_tensor("gates", [128, 32 * 8], F32))
        tiny = st.enter_context(nc.sbuf_tensor("tiny", [128, 64], F32))
        cvec = st.enter_context(nc.sbuf_tensor("cvec", [128, 8], F32))
        lamt = st.enter_context(nc.sbuf_tensor("lamt", [128, 160], F32))
        ident = cstb[:, 0:128]
        ones = cstb[:, 128:256]
        bd64 = cstb[:, 256:384]
        PK = lambda i: ("ps", i)

        def hT_view():
            return A.t[:, 0:H_BYTES // 4].bitcast(BF16).rearrange("p (k n) -> p k n", k=8)

        for ci_, cv_ in enumerate((EPS, 64 * EPS, 192 * EPS, 128 * EPS)):
            S.op("dve", lambda e, ci_=ci_, cv_=cv_: e.memset(cvec[:, ci_:ci_ + 1], cv_), writes=["cvec"])
        S.dma("pool", "cst", lambda e: e.dma_start(out=cstb[:], in_=cst_d), writes=["cst"])
        S.dma("sp", "smallp", lambda e: e.dma_start(out=smallp[:], in_=smallp_d), writes=["smallp"])
        scc3 = scc[:].rearrange("p (k r) -> p k r", r=2)
        S.dma("sp", "cc0", lambda e: e.dma_start(out=scc3[:, :, 0], in_=cc_d[0].rearrange("(k p) -> p k", p=128),
                                                  allow_slow_non_contiguous=True), writes=["scc0"])
        S.dma("sp", "cc1", lambda e: e.dma_start(out=scc3[:, :, 1], in_=cc_d[1].rearrange("(k p) -> p k", p=128),
                                                  allow_slow_non_contiguous=True), writes=["scc1"])
        S.op("act", lambda e: e.activation(scc[:], scc[:], AF.Silu), reads=["scc0", "scc1"], writes=["scc"])

        def phase_mod(l):
            A.reset(0)
            wm = [A.f32(8 * 512).rearrange("p (k n) -> p k n", k=8) for _ in range(2)]
            modsb = A.f32(6 * D)
            bmod = A.f32(6 * D)
            gt = A.f32(2 * D)
            S.dma("sp", "bmod", lambda e: e.dma_start(out=bmod[0:2, :], in_=b_mod_d[l].partition_broadcast(2)), writes=["bmod"])
            S.dma("sp", "gt0", lambda e: e.dma_start(out=gt[0:2, 0:D], in_=g_attn_d[l].partition_broadcast(2)), writes=["gt0"])
            S.dma("sp", "gt1", lambda e: e.dma_start(out=gt[0:2, D:2 * D], in_=g_ffn_d[l].partition_broadcast(2)), writes=["gt1"])
            for nt in range(12):
                sl = nt % 2
                S.dma("sp", ("wm", sl), lambda e, nt=nt, sl=sl: e.dma_start(
                    out=wm[sl], in_=w_mod_d[l][:, nt * 512:(nt + 1) * 512].rearrange("(k p) n -> p k n", p=128)),
                    writes=[("wm", sl)])
                for k in range(8):
                    S.op("pe", lambda e, sl=sl, k=k: e.matmul(ps[sl][0:2, :], scc3[:, k, :], wm[sl][:, k, :],
                                                             start=(k == 0), stop=(k == 7)),
                         reads=[("wm", sl), "scc"], writes=[PK(sl)])
                S.op("dve", lambda e, sl=sl, nt=nt: e.tensor_tensor(modsb[0:2, nt * 512:(nt + 1) * 512], ps[sl][0:2, :],
                                                                   bmod[0:2, nt * 512:(nt + 1) * 512], ALU.add),
                     reads=[PK(sl), "bmod"], writes=["modsb"])
            for j, g0 in ((1, 0), (4, D)):
                S.op("dve", lambda e, j=j, g0=g0: e.scalar_tensor_tensor(
                    modsb[0:2, j * D:(j + 1) * D], modsb[0:2, j * D:(j + 1) * D], 1.0, gt[0:2, g0:g0 + D], ALU.add, ALU.mult),
                    reads=["modsb", "gt0", "gt1"], writes=["modsb"])
            S.dma("sp", "modv", lambda e: e.dma_start(out=modv_d[l], in_=modsb[0:2, :]), reads=["modsb"], writes=[("modv", l)])

        for l in range(n_layers):
            phase_mod(l)
            S.barrier()

        def load_bc(dst, l, r, j, key):
            S.dma("sp", key, lambda e: e.dma_start(out=dst, in_=modv_d[l][r, j * D:(j + 1) * D].partition_broadcast(128)),
                  reads=[("modv", l)], writes=[key])

        def src_tile(l, t):
            if l == 0:
                if t < 32:
                    return x_d[t * 128:(t + 1) * 128, :]
                return ctx_d[(t - 32) * 128:(t - 31) * 128, :]
            return xs_d[t * 128:(t + 1) * 128, :]

        def norm_mod_T(tag, t, xt_ap, xt_key, mul_bc, mul_key, add_bc, add_key, bufs, hT, deferred=None):
            sl = t % 2
            sqj, t1, hb = bufs["sqj"][sl], bufs["t1"][sl], bufs["hb"][sl]
            S.op("act", lambda e: e.activation(sqj, xt_ap, AF.Square, accum_out=ssq[:, t:t + 1]),
                 reads=[xt_key], writes=[(tag, "sqj", sl), (tag, "ssq", t)])
            S.op("act", lambda e: e.activation(rstd[:, t:t + 1], ssq[:, t:t + 1], AF.Ln, bias=cvec[:, 0:1], scale=1.0 / D),
                 reads=[(tag, "ssq", t)], writes=[(tag, "rstd", t)])
            S.op("act", lambda e: e.activation(rstd[:, t:t + 1], rstd[:, t:t + 1], AF.Exp, scale=-0.5),
                 reads=[(tag, "rstd", t)], writes=[(tag, "rstd", t)])
            S.op("dve", lambda e: e.scalar_tensor_tensor(t1, xt_ap, rstd[:, t:t + 1], mul_bc, ALU.mult, ALU.mult),
                 reads=[xt_key, (tag, "rstd", t), mul_key], writes=[(tag, "t1", sl)])
            S.op("pool", lambda e: e.tensor_tensor(hb, t1, add_bc, ALU.add),
                 reads=[(tag, "t1", sl), add_key], writes=[(tag, "hb", sl)])
            pb = 6 + sl
            psT = ps[pb][:].bitcast(BF16)
            for k in range(8):
                S.op("pe", lambda e, k=k: e.transpose(psT[:, k * 128:(k + 1) * 128], hb[:, k * 128:(k + 1) * 128], ident),
                     reads=[(tag, "hb", sl), "cst"], writes=[PK(pb)])
            def evac():
                S.op("act", lambda e: e.activation(hT[:, :, t * 128:(t + 1) * 128],
                                                   psT[:, 0:1024].rearrange("p (k n) -> p k n", k=8), AF.Copy),
                     reads=[PK(pb)], writes=[("hT", t)])
            if deferred is None:
                evac()
            else:
                deferred.append(evac)

        def alloc_norm_bufs():
            return {"sqj": [A.bf16(D) for _ in range(2)], "t1": [A.f32(D) for _ in range(2)],
                    "hb": [A.bf16(D) for _ in range(2)]}

        def phase_A(l):
            A.reset(H_BYTES)
            hT = hT_view()
            bufs = alloc_norm_bufs()
            xt = [A.f32(D) for _ in range(3)]
            mul = [A.f32(D) for _ in range(2)]
            add = [A.f32(D) for _ in range(2)]
            for r in range(2):
                load_bc(mul[r], l, r, 1, ("Amul", r))
                load_bc(add[r], l, r, 0, ("Aadd", r))
            S.op("dve", lambda e: e.memset(ssq[:], 0.0), writes=[("A", "ssq", t) for t in range(NT128)])
            dfr = []
            for t in range(NT128):
                r = 0 if t < 32 else 1
                s3 = t % 3
                S.dma("sp", ("xt", s3), lambda e, t=t, s3=s3: e.dma_start(out=xt[s3], in_=src_tile(l, t)),
                      reads=[("xs", t)], writes=[("xt", s3)])
                norm_mod_T("A", t, xt[s3], ("xt", s3), mul[r], ("Amul", r), add[r], ("Aadd", r), bufs, hT, deferred=dfr)
                while len(dfr) > 1:
                    dfr.pop(0)()
            while dfr:
                dfr.pop(0)()

        def attn_unit(KT, QT, VXh, kcs, n, scale, obank, PT, sbufs, rk, wk, tp=None):
            tpk = {"tile_position": tp} if tp is not None else {}
            groups = [kcs[i:i + 2] for i in range(0, len(kcs), 2)]
            ng = len(groups)

            def qk(g):
                sb = sbufs[g % len(sbufs)]
                w = len(groups[g])
                for i, kc in enumerate(groups[g]):
                    S.op("pe", lambda e, i=i, kc=kc: e.matmul(ps[sb + i][:, 0:n], KT[:, kc * 128:(kc + 1) * 128], QT, start=True, stop=True, **tpk),
                         reads=rk, writes=[PK(sb + i)])
                sl = g % len(PT)
                if n == 512:
                    srcap = psall[:, sb * 512:(sb + w) * 512]
                    dstap = PT[sl][:, 0:w * 512]
                else:
                    srcap = psall[:, sb * 512:(sb + w) * 512].rearrange("p (b c) -> p b c", c=512)[:, :, 0:n]
                    dstap = PT[sl][:, 0:w * 512].rearrange("p (b c) -> p b c", c=512)[:, :, 0:n]
                S.op("act", lambda e: e.activation(dstap, srcap, AF.Exp, scale=scale),
                     reads=[PK(sb + i) for i in range(w)], writes=[("PT", sl)])

            def pv(g):
                sl = g % len(PT)
                for i, kc in enumerate(groups[g]):
                    first = (g == 0 and i == 0)
                    last = (g == ng - 1 and i == len(groups[g]) - 1)
                    S.op("pe", lambda e, i=i, kc=kc, first=first, last=last: e.matmul(
                        ps[obank][:, 0:n], VXh[:, kc, :], PT[sl][:, i * 512:i * 512 + n], start=first, stop=last),
                        reads=[("PT", sl)] + rk, writes=[PK(obank)] + wk)

            qk(0)
            if ng > 1:
                qk(1)
            for g in range(ng):
                if g + 2 < ng:
                    qk(g + 2)
                pv(g)

        def attn_multi(lanes, kcs, n, scale, PT, sbufs, rk, hook=None, split=False):
            nk = len(kcs)
            L = len(lanes)
            grps = [[0, 1], [2, 3]] if split else [list(range(L))]

            def qk(g):
                sb = sbufs[g % len(sbufs)]
                kc = kcs[g]
                sl = g % len(PT)
                for gi, idxs in enumerate(grps):
                    for i in idxs:
                        KT, QT, VXh, ob, tp = lanes[i]
                        S.op("pe", lambda e, i=i, KT=KT, QT=QT, tp=tp: e.matmul(ps[sb + i][:, 0:n], KT[:, kc * 128:(kc + 1) * 128], QT,
                                                                              start=True, stop=True, tile_position=tp),
                             reads=rk, writes=[PK(sb + i)])
                    i0 = idxs[0]
                    w = len(idxs)
                    if n == 512:
                        srcap = psall[:, (sb + i0) * 512:(sb + i0 + w) * 512]
                        dstap = PT[sl][:, i0 * 512:(i0 + w) * 512]
                    else:
                        srcap = psall[:, (sb + i0) * 512:(sb + i0 + w) * 512].rearrange("p (b c) -> p b c", c=512)[:, :, 0:n]
                        dstap = PT[sl][:, i0 * 512:(i0 + w) * 512].rearrange("p (b c) -> p b c", c=512)[:, :, 0:n]
                    S.op("act", lambda e, srcap=srcap, dstap=dstap: e.activation(dstap, srcap, AF.Exp, scale=scale),
                         reads=[PK(sb + i) for i in idxs], writes=[("PT", sl, gi)])

            def pv(g):
                kc = kcs[g]
                sl = g % len(PT)
                for i, (KT, QT, VXh, ob, tp) in enumerate(lanes):
                    gi = (i // 2) if split else 0
                    S.op("pe", lambda e, i=i, VXh=VXh, ob=ob: e.matmul(ps[ob][:, 0:n], VXh[:, kc, :], PT[sl][:, i * 512:i * 512 + n],
                                                                      start=(g == 0), stop=(g == nk - 1)),
                         reads=[("PT", sl, gi)] + rk, writes=[PK(ob)])

            qk(0)
            if nk > 1:
                qk(1)
            hk = min(4, nk - 1)
            for g in range(nk):
                if g + 2 < nk:
                    qk(g + 2)
                pv(g)
                if hook is not None and g == hk:
                    hook()

        def q_tiles(need_ctx):
            tl = [(tt * 512, 512, list(range(NT128))) for tt in range(8)]
            if need_ctx:
                tl.append((S_LAT, 256, [32, 33]))
            return tl

        def tok_tiles():
            return [(tt * 512, 512) for tt in range(8)] + [(S_LAT, 256)]

        def load_win(c0, c1, dst, key):
            wd = w_inx_d_l[0]
            S.dma("pool", key, lambda e: e.dma_start(out=dst, in_=wd[:, c0:c1].rearrange("(k p) n -> p k n", p=128)),
                  writes=[key])

        w_inx_d_l = [None]

        def proj_fm(pbank, win, c, hT, tok0, n, wkey):
            for k in range(8):
                S.op("pe", lambda e, k=k: e.matmul(ps[pbank][:, 0:n], win[:, k, c * 128:(c + 1) * 128], hT[:, k, tok0:tok0 + n],
                                                  start=(k == 0), stop=(k == 7)),
                     reads=[wkey, "hTall"], writes=[PK(pbank)])

        def load_rope(cdst, sdst, cd, sd, tok0, n, sl):
            S.dma("sp", ("rc", sl), lambda e: e.dma_start(out=cdst[:, 0:n], in_=cd[:, tok0:tok0 + n]), writes=[("rc", sl)])
            S.dma("sp", ("rs", sl), lambda e: e.dma_start(out=sdst[:, 0:n], in_=sd[:, tok0:tok0 + n]), writes=[("rs", sl)])

        def phase_gqa(l, need_ctx):
            A.reset(H_BYTES)
            hT = hT_view()
            QG = A.bf16(4 * T).rearrange("p (c n) -> p c n", c=4)
            KG = A.bf16(2 * T).rearrange("p (c n) -> p c n", c=2)
            VXf = A.bf16(2 * NT128 * 128)
            VX = VXf.rearrange("p (h t c) -> p h t c", h=2, t=NT128)
            win = A.bf16(8 * 1664).rearrange("p (k n) -> p k n", k=8)
            rc = [A.f32(512) for _ in range(2)]
            rs = [A.f32(512) for _ in range(2)]
            sq = [A.bf16(512) for _ in range(2)]
            rr = [A.f32(512) for _ in range(2)]
            t1 = [A.f32(512) for _ in range(2)]
            t2 = [A.f32(512) for _ in range(2)]
            PT = [A.bf16(1024) for _ in range(3)]
            AO = [A.bf16(512) for _ in range(2)]
            rec = [A.f32(512) for _ in range(2)]
            osb = [A.f32(512) for _ in range(2)]
            load_win(0, 1536, win[:, :, 0:1536], "winA")
            load_win(VG_OFF, VG_OFF + 128, win[:, :, 1536:1664], "winB")
            S.op("pool", lambda e: e.memset(VXf.rearrange("p (g c) -> p g c", c=128)[:, :, 64:128], 1.0), writes=["VXones"])
            S.op("pool", lambda e: e.memset(tiny[:, 0:1], 0.0), reads=[("hT", t) for t in range(NT128)], writes=["hTall"])

            def chunk(tok0, n, sl, ci, kind, idx, cn, cs, gcol, gscol):
                b0 = (ci % 2) * 3
                u = ci % 2
                proj_fm(b0, win, cn, hT, tok0, n, "winA")
                proj_fm(b0 + 1, win, cs, hT, tok0, n, "winA")
                S.op("act", lambda e: e.activation(sq[u][:, 0:n], ps[b0][:, 0:n], AF.Square),
                     reads=[PK(b0)], writes=[("sq", u)])
                S.op("pe", lambda e: e.matmul(ps[b0 + 2][:, 0:n], bd64, sq[u][:, 0:n], start=True, stop=True),
                     reads=[("sq", u), "cst"], writes=[PK(b0 + 2)])
                S.op("act", lambda e: e.activation(rr[u][:, 0:n], ps[b0 + 2][:, 0:n], AF.Ln, bias=cvec[:, 1:1 + 1], scale=1.0), reads=[PK(b0 + 2), "cvec"], writes=[("rr", u)])
                S.op("act", lambda e: e.activation(rr[u][:, 0:n], rr[u][:, 0:n], AF.Exp, scale=-0.5), reads=[("rr", u)], writes=[("rr", u)])
                S.op("dve", lambda e: e.tensor_tensor(t1[u][:, 0:n], ps[b0][:, 0:n], rr[u][:, 0:n], ALU.mult),
                     reads=[PK(b0), ("rr", u)], writes=[("t1", u)])
                S.op("dve", lambda e: e.tensor_tensor(t2[u][:, 0:n], ps[b0 + 1][:, 0:n], rr[u][:, 0:n], ALU.mult),
                     reads=[PK(b0 + 1), ("rr", u)], writes=[("t2", u)])
                S.op("dve", lambda e: e.scalar_tensor_tensor(t1[u][:, 0:n], t1[u][:, 0:n], smallp[:, l * 8 + gcol:l * 8 + gcol + 1],
                                                             rc[sl][:, 0:n], ALU.mult, ALU.mult),
                     reads=[("t1", u), ("rc", sl), "smallp"], writes=[("t1", u)])
                S.op("dve", lambda e: e.scalar_tensor_tensor(t2[u][:, 0:n], t2[u][:, 0:n], smallp[:, l * 8 + gscol:l * 8 + gscol + 1],
                                                              rs[sl][:, 0:n], ALU.mult, ALU.mult),
                     reads=[("t2", u), ("rs", sl), "smallp"], writes=[("t2", u)])
                dst = QG[:, idx, tok0:tok0 + n] if kind == "q" else KG[:, idx, tok0:tok0 + n]
                S.op("pool", lambda e: e.tensor_tensor(dst, t1[u][:, 0:n], t2[u][:, 0:n], ALU.add),
                     reads=[("t1", u), ("t2", u)], writes=[("QK", kind, idx, tok0)])

            for it, (tok0, n) in enumerate(tok_tiles()):
                is_ctx = tok0 >= S_LAT
                sl = it % 2
                load_rope(rc[sl], rs[sl], ropeA_c_d, ropeA_s_d, tok0, n, sl)
                chunks = []
                if (not is_ctx) or need_ctx:
                    chunks += [("q", c, c, 4 + c, 0, 1) for c in range(4)]
                chunks += [("k", kv, 8 + kv, 10 + kv, 2, 3) for kv in range(2)]
                for ci, (kind, idx, cn, cs, gcol, gscol) in enumerate(chunks):
                    chunk(tok0, n, sl, ci, kind, idx, cn, cs, gcol, gscol)

            def vtile(t):
                b = 6 + t % 2
                for k in range(8):
                    S.op("pe", lambda e, k=k: e.matmul(ps[b][:, 0:128], hT[:, k, t * 128:(t + 1) * 128], win[:, k, 1536:1664],
                                                      start=(k == 0), stop=(k == 7)),
                         reads=["winB", "hTall"], writes=[PK(b)])
                S.op("dve", lambda e: e.tensor_copy(VX[:, :, t, 0:64], ps[b][:, 0:128].rearrange("p (h c) -> p h c", h=2)),
                     reads=[PK(b), "VXones"], writes=[("VX", t)])
            for t in range(NT128):
                vtile(t)
            S.op("pool", lambda e: e.memset(tiny[:, 1:2], 0.0),
                 reads=[("VX", t) for t in range(NT128)] + [("QK", "k", kv, tk) for kv in range(2) for (tk, _n) in tok_tiles()],
                 writes=["KVall"])

            def unit2(c, kv, tok0, n, kcs, a):
                lanes = []
                for hh in range(2):
                    rsl = slice(hh * 64, hh * 64 + 64)
                    lanes.append((KG[rsl, kv, :], QG[rsl, c, tok0:tok0 + n], VX[:, kv], 6 + hh, (hh * 64, 0)))
                attn_multi(lanes, kcs, n, 8.0, PT, [0, 2, 4], ["KVall", ("QK", "q", c, tok0)])
                for hh in range(2):
                    ob = 6 + hh
                    rsl = slice(hh * 64, hh * 64 + 64)
                    S.op("dve", lambda e, ob=ob, hh=hh: e.tensor_copy(rec[hh][0:64, 0:n], ps[ob][64:128, 0:n]),
                         reads=[PK(ob)], writes=[("rec", hh)])
                    S.op("dve", lambda e, ob=ob, hh=hh: e.tensor_copy(osb[hh][0:64, 0:n], ps[ob][0:64, 0:n]),
                         reads=[PK(ob)], writes=[("osb", hh)])
                for hh in range(2):
                    rsl = slice(hh * 64, hh * 64 + 64)
                    S.op("dve", lambda e, hh=hh: e.reciprocal(rec[hh][0:64, 0:n], rec[hh][0:64, 0:n]),
                         reads=[("rec", hh)], writes=[("rec", hh)])
                    S.op("dve", lambda e, hh=hh, rsl=rsl: e.tensor_tensor(AO[a][rsl, 0:n], osb[hh][0:64, 0:n], rec[hh][0:64, 0:n], ALU.mult),
                         reads=[("osb", hh), ("rec", hh)], writes=[("AO", a, hh)])

            def store(a, cidx, tok0, n):
                S.dma("sp", ("AOst", a), lambda e: e.dma_start(out=ao_d[cidx, :, tok0:tok0 + n], in_=AO[a][:, 0:n]),
                      reads=[("AO", a, 0), ("AO", a, 1)], writes=[("ao", cidx, tok0)])
            un = 0
            for c in range(4):
                for qi, (tok0, n, kcs) in enumerate(q_tiles(need_ctx)):
                    a = (c + qi) % 2
                    unit2(c, c // 2, tok0, n, kcs, a)
                    store(a, c, tok0, n)

        def phase_diff(l, need_ctx):
            lam_init = 0.8 - 0.6 * math.exp(-0.3 * l)
            A.reset(H_BYTES)
            hT = hT_view()
            QD = A.bf16(2 * T).rearrange("p (c n) -> p c n", c=2)
            KD = A.bf16(2 * T).rearrange("p (c n) -> p c n", c=2)
            VXf = A.bf16(4 * NT128 * 128)
            VX = VXf.rearrange("p (h t c) -> p h t c", h=4, t=NT128)
            win = A.bf16(8 * 1280).rearrange("p (k n) -> p k n", k=8)
            rc = [A.f32(512) for _ in range(2)]
            rs = [A.f32(512) for _ in range(2)]
            t1 = [A.f32(512) for _ in range(2)]
            t2 = [A.f32(512) for _ in range(2)]
            PT = [A.bf16(1024) for _ in range(3)]
            AO = [A.bf16(512) for _ in range(2)]
            rec = [A.f32(512) for _ in range(2)]
            dd = [A.f32(512) for _ in range(2)]
            sq = [A.bf16(512) for _ in range(2)]
            full = [rec[0], rec[1], dd[0], dd[1]]
            PT4 = [A.bf16(2048) for _ in range(3)]
            load_win(12 * 128, 20 * 128, win[:, :, 0:1024], "winA")
            load_win(VD_OFF, VD_OFF + 256, win[:, :, 1024:1280], "winB")
            S.op("pool", lambda e: e.memset(VXf.rearrange("p (g c) -> p g c", c=128)[:, :, 64:128], 1.0), writes=["VXones"])
            S.op("pool", lambda e: e.memset(tiny[:, 0:1], 0.0), reads=[("hT", t) for t in range(NT128)], writes=["hTall"])
            S.dma("sp", "lamv", lambda e: e.dma_start(out=lamt[:, 0:128], in_=lamv_d[l].partition_broadcast(128)), writes=["lamt"])
            S.op("dve", lambda e: e.tensor_tensor(lamt[:, 128:160], lamt[:, 0:32], lamt[:, 32:64], ALU.mult), reads=["lamt"], writes=["lam1"])
            S.op("dve", lambda e: e.tensor_reduce(tiny[:, 8:9], lamt[:, 128:160], AX.X, ALU.add), reads=["lam1"], writes=["lams1"])
            S.op("dve", lambda e: e.tensor_tensor(lamt[:, 128:160], lamt[:, 64:96], lamt[:, 96:128], ALU.mult), reads=["lamt", "lams1"], writes=["lam1"])
            S.op("dve", lambda e: e.tensor_reduce(tiny[:, 9:10], lamt[:, 128:160], AX.X, ALU.add), reads=["lam1"], writes=["lams2"])
            S.op("act", lambda e: e.activation(tiny[:, 10:12], tiny[:, 8:10], AF.Exp), reads=["lams1", "lams2"], writes=["lame"])
            S.op("dve", lambda e: e.tensor_tensor(tiny[:, 12:13], tiny[:, 11:12], tiny[:, 10:11], ALU.subtract), reads=["lame"], writes=["lamd"])
            S.op("dve", lambda e: e.tensor_scalar(tiny[:, 13:14], tiny[:, 12:13], -lam_init, 0.0, ALU.add, ALU.add), reads=["lamd"], writes=["neglam"])
            S.op("dve", lambda e: e.tensor_scalar(tiny[:, 14:15], smallp[:, l * 8 + 4:l * 8 + 5], 8.0 * (1.0 - lam_init), 0.0, ALU.mult, ALU.add),
                 reads=["smallp"], writes=["gs8"])

            def chunk(tok0, n, sl, ci, kind, idx, cn, cs):
                b0 = (ci % 2) * 2
                u = ci % 2
                proj_fm(b0, win, cn, hT, tok0, n, "winA")
                proj_fm(b0 + 1, win, cs, hT, tok0, n, "winA")
                S.op("dve", lambda e: e.tensor_tensor(t1[u][:, 0:n], ps[b0][:, 0:n], rc[sl][:, 0:n], ALU.mult),
                     reads=[PK(b0), ("rc", sl)], writes=[("t1", u)])
                S.op("dve", lambda e: e.tensor_tensor(t2[u][:, 0:n], ps[b0 + 1][:, 0:n], rs[sl][:, 0:n], ALU.mult),
                     reads=[PK(b0 + 1), ("rs", sl)], writes=[("t2", u)])
                dst = QD[:, idx, tok0:tok0 + n] if kind == "q" else KD[:, idx, tok0:tok0 + n]
                S.op("pool", lambda e: e.tensor_tensor(dst, t1[u][:, 0:n], t2[u][:, 0:n], ALU.add),
                     reads=[("t1", u), ("t2", u)], writes=[("QK", kind, idx, tok0)])

            for it, (tok0, n) in enumerate(tok_tiles()):
                is_ctx = tok0 >= S_LAT
                sl = it % 2
                load_rope(rc[sl], rs[sl], ropeB_c_d, ropeB_s_d, tok0, n, sl)
                chunks = []
                if (not is_ctx) or need_ctx:
                    chunks += [("q", c, c, 2 + c) for c in range(2)]
                chunks += [("k", c, 4 + c, 6 + c) for c in range(2)]
                for ci, (kind, idx, cn, cs) in enumerate(chunks):
                    chunk(tok0, n, sl, ci, kind, idx, cn, cs)

            def vtile(t):
                b = 6 + t % 2
                for k in range(8):
                    S.op("pe", lambda e, k=k: e.matmul(ps[b][:, 0:256], hT[:, k, t * 128:(t + 1) * 128], win[:, k, 1024:1280],
                                                      start=(k == 0), stop=(k == 7)),
                         reads=["winB", "hTall"], writes=[PK(b)])
                S.op("dve", lambda e: e.tensor_copy(VX[:, :, t, 0:64], ps[b][:, 0:256].rearrange("p (h c) -> p h c", h=4)),
                     reads=[PK(b), "VXones"], writes=[("VX", t)])
            for t in range(NT128):
                vtile(t)
            S.op("pool", lambda e: e.memset(tiny[:, 1:2], 0.0),
                 reads=[("VX", t) for t in range(NT128)] + [("QK", "k", c, tk) for c in range(2) for (tk, _n) in tok_tiles()],
                 writes=["KVall"])

            def post_a(n):
                for i in range(4):
                    S.op("dve", lambda e, i=i: e.tensor_copy(full[i][:, 0:n], ps[4 + i][:, 0:n]), reads=[PK(4 + i)], writes=[("full", i)])
                for hh in range(2):
                    f0, f1 = full[2 * hh], full[2 * hh + 1]
                    k0, k1 = ("full", 2 * hh), ("full", 2 * hh + 1)
                    S.op("dve", lambda e, hh=hh, f0=f0: e.reciprocal(t1[hh][0:64, 0:n], f0[64:128, 0:n]), reads=[k0], writes=[("t1", hh)])
                    S.op("dve", lambda e, hh=hh, f1=f1: e.reciprocal(t2[hh][0:64, 0:n], f1[64:128, 0:n]), reads=[k1], writes=[("t2", hh)])
                    S.op("pool", lambda e, hh=hh, f0=f0: e.tensor_tensor(f0[0:64, 0:n], f0[0:64, 0:n], t1[hh][0:64, 0:n], ALU.mult),
                         reads=[k0, ("t1", hh)], writes=[k0])
                    S.op("pool", lambda e, hh=hh, f1=f1: e.tensor_tensor(f1[0:64, 0:n], f1[0:64, 0:n], t2[hh][0:64, 0:n], ALU.mult),
                         reads=[k1, ("t2", hh)], writes=[k1])
                    S.op("dve", lambda e, hh=hh, f0=f0, f1=f1: e.scalar_tensor_tensor(t1[hh][0:64, 0:n], f1[0:64, 0:n], tiny[0:64, 13:14], f0[0:64, 0:n],
                                                                                   ALU.mult, ALU.add),
                         reads=[k0, k1, "neglam"], writes=[("t1", hh)])

            def post_b(n, a, cidx, tok0):
                for hh in range(2):
                    S.op("act", lambda e, hh=hh: e.activation(sq[hh][0:64, 0:n], t1[hh][0:64, 0:n], AF.Square), reads=[("t1", hh)], writes=[("sq", hh)])
                for hh in range(2):
                    S.op("pe", lambda e, hh=hh: e.matmul(ps[hh][0:64, 0:n], ones[0:64, 0:64], sq[hh][0:64, 0:n], start=True, stop=True),
                         reads=[("sq", hh), "cst"], writes=[PK(hh)])
                    S.op("act", lambda e, hh=hh: e.activation(t2[hh][0:64, 0:n], ps[hh][0:64, 0:n], AF.Ln, bias=cvec[0:64, 1:2], scale=1.0),
                         reads=[PK(hh), "cvec"], writes=[("t2", hh)])
                    S.op("act", lambda e, hh=hh: e.activation(t2[hh][0:64, 0:n], t2[hh][0:64, 0:n], AF.Exp, scale=-0.5), reads=[("t2", hh)], writes=[("t2", hh)])
                    S.op("dve", lambda e, hh=hh: e.scalar_tensor_tensor(AO[a][hh * 64:hh * 64 + 64, 0:n], t1[hh][0:64, 0:n], tiny[0:64, 14:15],
                                                                        t2[hh][0:64, 0:n], ALU.mult, ALU.mult),
                         reads=[("t1", hh), ("t2", hh), "gs8"], writes=[("AO", a, hh)])
                S.dma("sp", ("AOst", a), lambda e: e.dma_start(out=ao_d[cidx, :, tok0:tok0 + n], in_=AO[a][:, 0:n]),
                      reads=[("AO", a, 0), ("AO", a, 1)], writes=[("ao", cidx, tok0)])

            pending = []

            def flush():
                while pending:
                    pending.pop(0)()

            def unit4(c, tok0, n, kcs, a):
                lanes = []
                for hh in range(2):
                    for j in range(2):
                        rb = hh * 64 + j * 32
                        lanes.append((KD[rb:rb + 32, c, :], QD[rb:rb + 32, c, tok0:tok0 + n], VX[:, 2 * c + hh], 4 + 2 * hh + j, (rb, 0)))
                attn_multi(lanes, kcs, n, 32.0 ** -0.5, PT4, [0], ["KVall", ("QK", "q", c, tok0)], hook=flush, split=True)
                flush()
                post_a(n)
                pending.append(lambda: post_b(n, a, 4 + c, tok0))

            ui = 0
            for c in range(2):
                for (tok0, n, kcs) in q_tiles(need_ctx):
                    unit4(c, tok0, n, kcs, ui % 2)
                    ui += 1
            flush()

        def phase_mla(l, need_ctx):
            A.reset(H_BYTES)
            hT = hT_view()
            cqnA = A.bf16(T)
            cqnB = A.bf16(T)
            ckvn = A.bf16(T)
            krr = A.bf16(T)
            VXf = A.bf16(4 * NT128 * 128)
            VX = VXf.rearrange("p (h t c) -> p h t c", h=4, t=NT128)
            win = A.bf16(8 * 512).rearrange("p (k n) -> p k n", k=8)
            wuq = A.bf16(2 * 384).rearrange("p (k n) -> p k n", k=2)
            wuqs = A.bf16(2 * 384).rearrange("p (k n) -> p k n", k=2)
            wkk = A.bf16(256)
            wkv = A.bf16(256)
            rc = [A.f32(512) for _ in range(2)]
            rs = [A.f32(512) for _ in range(2)]
            sq = [A.bf16(512) for _ in range(2)]
            rr = [A.f32(512) for _ in range(2)]
            t1 = [A.f32(512) for _ in range(2)]
            t2 = [A.f32(512) for _ in range(2)]
            PT = [A.bf16(1024) for _ in range(3)]
            AO = [A.bf16(512) for _ in range(2)]
            rec = [A.f32(512) for _ in range(2)]
            osb = [A.f32(512) for _ in range(2)]
            load_win(20 * 128, 24 * 128, win, "winA")
            S.dma("pool", "wuq0", lambda e: e.dma_start(out=wuq[:, 0, :], in_=w_uq_d[l][0:128, :]), writes=["wuq0"])
            S.dma("pool", "wuq1", lambda e: e.dma_start(out=wuq[0:64, 1, :], in_=w_uq_d[l][128:192, :]), writes=["wuq1"])
            S.dma("pool", "wuqs0", lambda e: e.dma_start(out=wuqs[:, 0, :], in_=w_uqs_d[l][0:128, :]), writes=["wuqs0"])
            S.dma("pool", "wuqs1", lambda e: e.dma_start(out=wuqs[0:64, 1, :], in_=w_uqs_d[l][128:192, :]), writes=["wuqs1"])
            S.dma("pool", "wkk", lambda e: e.dma_start(out=wkk, in_=w_ukvk_d[l]), writes=["wkk"])
            S.dma("pool", "wkv", lambda e: e.dma_start(out=wkv, in_=w_ukvv_d[l]), writes=["wkv"])
            WUQ = ["wuq0", "wuq1", "wuqs0", "wuqs1"]
            S.op("pool", lambda e: e.memset(VXf.rearrange("p (g c) -> p g c", c=128)[:, :, 64:128], 1.0), writes=["VXones"])
            S.op("pool", lambda e: e.memset(tiny[:, 0:1], 0.0), reads=[("hT", t) for t in range(NT128)], writes=["hTall"])
            S.op("dve", lambda e: e.tensor_scalar(tiny[:, 16:18], smallp[:, l * 8 + 5:l * 8 + 7], math.sqrt(192.0), 0.0, ALU.mult, ALU.add),
                 reads=["smallp"], writes=["gcq"])
            S.op("dve", lambda e: e.tensor_scalar(tiny[:, 18:19], smallp[:, l * 8 + 7:l * 8 + 8], math.sqrt(128.0), 0.0, ALU.mult, ALU.add),
                 reads=["smallp"], writes=["gckv"])

            def ptile(tok0, n, sl):
                load_rope(rc[sl], rs[sl], ropeB_c_d, ropeB_s_d, tok0, n, sl)
                proj_fm(0, win, 0, hT, tok0, n, "winA")
                proj_fm(1, win, 1, hT, tok0, n, "winA")
                proj_fm(2, win, 2, hT, tok0, n, "winA")
                proj_fm(3, win, 3, hT, tok0, n, "winA")
                S.op("act", lambda e: e.activation(sq[0][:, 0:n], ps[0][:, 0:n], AF.Square), reads=[PK(0)], writes=[("sq", 0)])
                S.op("act", lambda e: e.activation(sq[1][0:64, 0:n], ps[1][0:64, 0:n], AF.Square), reads=[PK(1)], writes=[("sq", 1)])
                S.op("pe", lambda e: e.matmul(ps[4][:, 0:n], ones, sq[0][:, 0:n], start=True, stop=False), reads=[("sq", 0), "cst"], writes=[PK(4)])
                S.op("pe", lambda e: e.matmul(ps[4][:, 0:n], ones[0:64, :], sq[1][0:64, 0:n], start=False, stop=True), reads=[("sq", 1), "cst"], writes=[PK(4)])
                S.op("act", lambda e: e.activation(rr[0][:, 0:n], ps[4][:, 0:n], AF.Ln, bias=cvec[:, 2:2 + 1], scale=1.0), reads=[PK(4), "cvec"], writes=[("rr", 0)])
                S.op("act", lambda e: e.activation(rr[0][:, 0:n], rr[0][:, 0:n], AF.Exp, scale=-0.5), reads=[("rr", 0)], writes=[("rr", 0)])
                S.op("dve", lambda e: e.scalar_tensor_tensor(cqnA[:, tok0:tok0 + n], ps[0][:, 0:n], tiny[:, 16:17], rr[0][:, 0:n], ALU.mult, ALU.mult),
                     reads=[PK(0), ("rr", 0), "gcq"], writes=[("cqnA", tok0)])
                S.op("dve", lambda e: e.scalar_tensor_tensor(cqnB[0:64, tok0:tok0 + n], ps[1][0:64, 0:n], tiny[0:64, 17:18], rr[0][0:64, 0:n], ALU.mult, ALU.mult),
                     reads=[PK(1), ("rr", 0), "gcq"], writes=[("cqnB", tok0)])
                S.op("act", lambda e: e.activation(sq[0][:, 0:n], ps[3][:, 0:n], AF.Square), reads=[PK(3)], writes=[("sq", 0)])
                S.op("pe", lambda e: e.matmul(ps[5][:, 0:n], ones, sq[0][:, 0:n], start=True, stop=True), reads=[("sq", 0), "cst"], writes=[PK(5)])
                S.op("act", lambda e: e.activation(rr[1][:, 0:n], ps[5][:, 0:n], AF.Ln, bias=cvec[:, 3:3 + 1], scale=1.0), reads=[PK(5), "cvec"], writes=[("rr", 1)])
                S.op("act", lambda e: e.activation(rr[1][:, 0:n], rr[1][:, 0:n], AF.Exp, scale=-0.5), reads=[("rr", 1)], writes=[("rr", 1)])
                S.op("dve", lambda e: e.scalar_tensor_tensor(ckvn[:, tok0:tok0 + n], ps[3][:, 0:n], tiny[:, 18:19], rr[1][:, 0:n], ALU.mult, ALU.mult),
                     reads=[PK(3), ("rr", 1), "gckv"], writes=[("ckvn", tok0)])
                S.op("dve", lambda e: e.tensor_tensor(t1[0][64:96, 0:n], ps[1][64:96, 0:n], rc[sl][64:96, 0:n], ALU.mult),
                     reads=[PK(1), ("rc", sl)], writes=[("t1", 0)])
                S.op("dve", lambda e: e.tensor_tensor(t2[0][64:96, 0:n], ps[2][64:96, 0:n], rs[sl][64:96, 0:n], ALU.mult),
                     reads=[PK(2), ("rs", sl)], writes=[("t2", 0)])
                S.op("pool", lambda e: e.tensor_tensor(krr[64:96, tok0:tok0 + n], t1[0][64:96, 0:n], t2[0][64:96, 0:n], ALU.add),
                     reads=[("t1", 0), ("t2", 0)], writes=[("krr", tok0)])
            for it, (tok0, n) in enumerate(tok_tiles()):
                ptile(tok0, n, it % 2)

            def vtile(t):
                b = 6 + t % 2
                S.op("pe", lambda e: e.matmul(ps[b][:, 0:256], ckvn[:, t * 128:(t + 1) * 128], wkv, start=True, stop=True),
                     reads=["wkv", ("ckvn", (t // 4) * 512)], writes=[PK(b)])
                S.op("dve", lambda e: e.tensor_copy(VX[:, :, t, 0:64], ps[b][:, 0:256].rearrange("p (h c) -> p h c", h=4)),
                     reads=[PK(b), "VXones"], writes=[("VX", t)])
            for t in range(NT128):
                vtile(t)
            S.barrier()
            QM = A.t[:, 0:H_BYTES // 8].bitcast(BF16).rearrange("p (h n) -> p h n", h=4)
            KM = A.t[:, H_BYTES // 8:H_BYTES // 4].bitcast(BF16).rearrange("p (h n) -> p h n", h=4)

            def uhead(tok0, n, sl, h, doq):
                u = h % 2
                if doq:
                    for (bank, w) in ((2 * u, wuq), (2 * u + 1, wuqs)):
                        S.op("pe", lambda e, bank=bank, w=w: e.matmul(ps[bank][0:96, 0:n], w[:, 0, h * 96:(h + 1) * 96], cqnA[:, tok0:tok0 + n],
                                                                     start=True, stop=False),
                             reads=WUQ, writes=[PK(bank)])
                        S.op("pe", lambda e, bank=bank, w=w: e.matmul(ps[bank][0:96, 0:n], w[0:64, 1, h * 96:(h + 1) * 96], cqnB[0:64, tok0:tok0 + n],
                                                                     start=False, stop=True),
                             reads=WUQ, writes=[PK(bank)])
                    S.op("act", lambda e: e.activation(QM[0:64, h, tok0:tok0 + n], ps[2 * u][0:64, 0:n], AF.Copy),
                         reads=[PK(2 * u)], writes=[("QMn", h, tok0)])
                    S.op("dve", lambda e: e.tensor_tensor(t1[u][64:96, 0:n], ps[2 * u][64:96, 0:n], rc[sl][64:96, 0:n], ALU.mult),
                         reads=[PK(2 * u), ("rc", sl)], writes=[("t1", u)])
                    S.op("dve", lambda e: e.tensor_tensor(t2[u][64:96, 0:n], ps[2 * u + 1][64:96, 0:n], rs[sl][64:96, 0:n], ALU.mult),
                         reads=[PK(2 * u + 1), ("rs", sl)], writes=[("t2", u)])
                    S.op("pool", lambda e: e.tensor_tensor(QM[64:96, h, tok0:tok0 + n], t1[u][64:96, 0:n], t2[u][64:96, 0:n], ALU.add),
                         reads=[("t1", u), ("t2", u)], writes=[("QMr", h, tok0)])
                kb = 4 + u
                S.op("pe", lambda e: e.matmul(ps[kb][0:64, 0:n], wkk[:, h * 64:(h + 1) * 64], ckvn[:, tok0:tok0 + n], start=True, stop=True),
                     reads=["wkk"], writes=[PK(kb)])
                S.op("dve", lambda e: e.tensor_copy(KM[0:64, h, tok0:tok0 + n], ps[kb][0:64, 0:n]),
                     reads=[PK(kb)], writes=[("KMn", h, tok0)])
                S.op("pool", lambda e: e.tensor_copy(KM[64:96, h, tok0:tok0 + n], krr[64:96, tok0:tok0 + n]),
                     writes=[("KMr", h, tok0)])
            for it, (tok0, n) in enumerate(tok_tiles()):
                is_ctx = tok0 >= S_LAT
                sl = it % 2
                load_rope(rc[sl], rs[sl], ropeB_c_d, ropeB_s_d, tok0, n, sl)
                for h in range(4):
                    uhead(tok0, n, sl, h, (not is_ctx) or need_ctx)
            S.op("pool", lambda e: e.memset(tiny[:, 1:2], 0.0),
                 reads=[(kk, h, tk) for kk in ("KMn", "KMr") for h in range(4) for (tk, _n) in tok_tiles()],
                 writes=["KVall"])

            def unit(c, hh, tok0, n, kcs, ob, u, a):
                h = 2 * c + hh
                attn_unit(KM[0:96, h, :], QM[0:96, h, tok0:tok0 + n], VX[:, h], kcs, n, 96.0 ** -0.5, ob, PT, [0, 2, 4],
                          ["KVall", ("QMn", h, tok0), ("QMr", h, tok0)], [])
                S.op("dve", lambda e: e.tensor_copy(rec[u][0:64, 0:n], ps[ob][64:128, 0:n]),
                     reads=[PK(ob)], writes=[("rec", u)])
                S.op("dve", lambda e: e.tensor_copy(osb[u][0:64, 0:n], ps[ob][0:64, 0:n]),
                     reads=[PK(ob)], writes=[("osb", u)])
                S.op("dve", lambda e: e.reciprocal(rec[u][0:64, 0:n], rec[u][0:64, 0:n]),
                         reads=[("rec", u)], writes=[("rec", u)])
                S.op("dve", lambda e: e.tensor_tensor(AO[a][hh * 64:hh * 64 + 64, 0:n], osb[u][0:64, 0:n], rec[u][0:64, 0:n], ALU.mult),
                     reads=[("osb", u), ("rec", u)], writes=[("AO", a, hh)])

            def store(a, cidx, tok0, n):
                S.dma("sp", ("AOst", a), lambda e: e.dma_start(out=ao_d[cidx, :, tok0:tok0 + n], in_=AO[a][:, 0:n]),
                      reads=[("AO", a, 0), ("AO", a, 1)], writes=[("ao", cidx, tok0)])
            un = 0
            for c in range(2):
                for qi, (tok0, n, kcs) in enumerate(q_tiles(need_ctx)):
                    a = (c + qi) % 2
                    for hh in range(2):
                        unit(c, hh, tok0, n, kcs, 6 + un % 2, un % 2, a)
                        un += 1
                    store(a, 6 + c, tok0, n)

        def phase_C(l, need_ctx, moe):
            A.reset(H_BYTES)
            hT = hT_view()
            wout = A.bf16(8 * D).rearrange("p (k n) -> p k n", k=8)
            bufs = alloc_norm_bufs()
            xt = [A.f32(D) for _ in range(2)]
            xn = [A.f32(D) for _ in range(2)]
            aot = [A.bf16(8 * 128).rearrange("p (k n) -> p k n", k=8) for _ in range(2)]
            nr = 2 if need_ctx else 1
            G2 = [A.f32(D) for _ in range(nr)]
            Fm = [A.f32(D) for _ in range(nr)]
            Fa = [A.f32(D) for _ in range(nr)]
            S.dma("pool", "wout", lambda e: e.dma_start(out=wout, in_=w_out_d[l].rearrange("(k p) n -> p k n", p=128)), writes=["wout"])
            for r in range(nr):
                load_bc(G2[r], l, r, 2, ("G2", r))
                load_bc(Fm[r], l, r, 4, ("Fm", r))
                load_bc(Fa[r], l, r, 3, ("Fa", r))
            wr = lg = None
            if moe:
                wr = A.bf16(64).rearrange("p (k n) -> p k n", k=8)
                lg = A.f32(8)
                S.dma("pool", "wr", lambda e: e.dma_start(out=wr, in_=moe_router_d[0].rearrange("(k p) n -> p k n", p=128)), writes=["wr"])
            S.op("dve", lambda e: e.memset(ssq[:], 0.0), writes=[("C", "ssq", t) for t in range(NT128)])
            ntl = NT128 if need_ctx else 32
            g3 = gates[:].rearrange("p (t e) -> p t e", e=8)
            tn = tiny
            dfr = []

            def cload(t):
                sl = t % 2
                S.dma("sp", ("aot", sl), lambda e: e.dma_start(out=aot[sl], in_=ao_d[:, :, t * 128:(t + 1) * 128].rearrange("c p n -> p c n")),
                      reads=[("ao", c, (t // 4) * 512) for c in range(8)], writes=[("aot", sl)])
                S.dma("sp", ("xt", sl), lambda e: e.dma_start(out=xt[sl], in_=src_tile(l, t)), reads=[("xs", t)], writes=[("xt", sl)])

            def ctile(t):
                r = 0 if t < 32 else 1
                sl = t % 2
                for half in range(2):
                    b = 2 * sl + half
                    for k in range(8):
                        S.op("pe", lambda e, b=b, k=k, half=half: e.matmul(ps[b][:, :], aot[sl][:, k, :], wout[:, k, half * 512:(half + 1) * 512],
                                                                         start=(k == 0), stop=(k == 7)),
                             reads=[("aot", sl), "wout"], writes=[PK(b)])
                    S.op("dve", lambda e, b=b, half=half: e.tensor_tensor(xn[sl][:, half * 512:(half + 1) * 512], ps[b][:, :],
                                                                        G2[r][:, half * 512:(half + 1) * 512], ALU.mult),
                         reads=[PK(b), ("G2", r)], writes=[("xn", sl)])
                S.op("pool", lambda e: e.tensor_tensor(xn[sl], xn[sl], xt[sl], ALU.add),
                     reads=[("xn", sl), ("xt", sl)], writes=[("xn", sl)])
                S.dma("sp", ("xst", sl), lambda e: e.dma_start(out=xs_d[t * 128:(t + 1) * 128, :], in_=xn[sl]),
                      reads=[("xn", sl)], writes=[("xs", t)])

            def npart(t):
                r = 0 if t < 32 else 1
                sl = t % 2
                norm_mod_T("C", t, xn[sl], ("xn", sl), Fm[r], ("Fm", r), Fa[r], ("Fa", r), bufs, hT, deferred=dfr)
                if moe:
                    dfr.append(lambda: router(t))

            def router(t):
                if True:
                    for k in range(8):
                        S.op("pe", lambda e, k=k: e.matmul(ps[4][:, 0:8], hT[:, k, t * 128:(t + 1) * 128], wr[:, k, :], start=(k == 0), stop=(k == 7)),
                             reads=[("hT", t), "wr"], writes=[PK(4)])
                    S.op("dve", lambda e: e.tensor_copy(lg, ps[4][:, 0:8]), reads=[PK(4)], writes=["lg"])
                    S.op("dve", lambda e: e.tensor_reduce(tn[:, 20:21], lg, AX.X, ALU.max), reads=["lg"], writes=["m1"])
                    S.op("dve", lambda e: e.tensor_scalar(tn[:, 24:32], lg, tn[:, 20:21], 0.0, ALU.is_equal, ALU.add), reads=["lg", "m1"], writes=["eq1"])
                    S.op("dve", lambda e: e.scalar_tensor_tensor(tn[:, 32:40], tn[:, 24:32], -1e30, lg, ALU.mult, ALU.add), reads=["eq1", "lg"], writes=["lg2"])
                    S.op("dve", lambda e: e.tensor_reduce(tn[:, 21:22], tn[:, 32:40], AX.X, ALU.max), reads=["lg2"], writes=["m2"])
                    S.op("dve", lambda e: e.tensor_scalar(tn[:, 40:48], tn[:, 32:40], tn[:, 21:22], 0.0, ALU.is_equal, ALU.add), reads=["lg2", "m2"], writes=["eq2"])
                    S.op("dve", lambda e: e.tensor_tensor(tn[:, 22:23], tn[:, 21:22], tn[:, 20:21], ALU.subtract), reads=["m1", "m2"], writes=["dm"])
                    S.op("act", lambda e: e.activation(tn[:, 23:24], tn[:, 22:23], AF.Exp), reads=["dm"], writes=["edm"])
                    S.op("dve", lambda e: e.tensor_scalar(tn[:, 48:49], tn[:, 23:24], 1.0, 0.0, ALU.add, ALU.add), reads=["edm"], writes=["den"])
                    S.op("dve", lambda e: e.reciprocal(tn[:, 49:50], tn[:, 48:49]), reads=["den"], writes=["w1"])
                    S.op("dve", lambda e: e.tensor_tensor(tn[:, 50:51], tn[:, 23:24], tn[:, 49:50], ALU.mult), reads=["edm", "w1"], writes=["w2"])
                    S.op("dve", lambda e: e.tensor_scalar(tn[:, 52:60], tn[:, 24:32], tn[:, 49:50], 0.0, ALU.mult, ALU.add), reads=["eq1", "w1"], writes=["g1"])
                    S.op("dve", lambda e: e.scalar_tensor_tensor(g3[:, t, :], tn[:, 40:48], tn[:, 50:51], tn[:, 52:60], ALU.mult, ALU.add),
                         reads=["eq2", "w2", "g1"], writes=[("gates", t)])
            nkeep = 2 if moe else 1
            cload(0)
            ctile(0)
            for t in range(ntl):
                if t + 1 < ntl:
                    cload(t + 1)
                    ctile(t + 1)
                npart(t)
                while len(dfr) > nkeep:
                    dfr.pop(0)()
            while dfr:
                dfr.pop(0)()

        def phase_D(l, need_ctx, moe):
            ntok = T if need_ctx else S_LAT
            A.reset(H_BYTES)
            hT = hT_view()
            if moe:
                units = [(moe_w_in_d[0, e][:, 0:1792], moe_w_in_d[0, e][:, 1792:3584], moe_w_out_d[0, e], e) for e in range(8)]
                nf = 14
            else:
                units = [(ffn_w_in_d[0][:, f0:f0 + 1408], ffn_w_in_d[0][:, 2816 + f0:2816 + f0 + 1408], ffn_w_out_d[0][f0:f0 + 1408, :], None)
                         for f0 in (0, 1408)]
                nf = 11
            wg = A.bf16(8 * nf * 128).rearrange("p (k n) -> p k n", k=8)
            wu = A.bf16(8 * nf * 128).rearrange("p (k n) -> p k n", k=8)
            wo = A.bf16(nf * D).rearrange("p (j n) -> p j n", j=nf)
            aT = [A.bf16(nf * 512).rearrange("p (j n) -> p j n", j=nf) for _ in range(2)]
            xt = [A.f32(D) for _ in range(2)]
            t1 = [A.f32(D) for _ in range(2)]
            sg = [A.f32(512) for _ in range(2)]
            nr = 2 if need_ctx else 1
            G5 = [A.f32(D) for _ in range(nr)]
            for r in range(nr):
                load_bc(G5[r], l, r, 5, ("G5", r))
            g3 = gates[:].rearrange("p (t e) -> p t e", e=8)
            tiles = [(tt * 512, 512) for tt in range(8)] + ([(S_LAT, 256)] if need_ctx else [])
            S.op("pool", lambda e: e.memset(tiny[:, 0:1], 0.0), reads=[("hT", t) for t in range(ntok // 128)], writes=["hTall"])

            def fchunk(tok0, n, asl, j):
                u = j % 2
                S.begin_group()
                for k in range(8):
                    S.op("pe", lambda e, k=k: e.matmul(ps[u][:, 0:n], wg[:, k, j * 128:(j + 1) * 128], hT[:, k, tok0:tok0 + n],
                                                      start=(k == 0), stop=(k == 7)),
                         reads=["wg", "hTall"], writes=[PK(u)])
                for k in range(8):
                    S.op("pe", lambda e, k=k: e.matmul(ps[2 + u][:, 0:n], wu[:, k, j * 128:(j + 1) * 128], hT[:, k, tok0:tok0 + n],
                                                      start=(k == 0), stop=(k == 7)),
                         reads=["wu", "hTall"], writes=[PK(2 + u)])
                S.end_group()
                S.op("act", lambda e: e.activation(sg[u][:, 0:n], ps[u][:, 0:n], AF.Silu), reads=[PK(u)], writes=[("sg", u)])
                S.op("dve", lambda e: e.tensor_tensor(aT[asl][:, j, 0:n], sg[u][:, 0:n], ps[2 + u][:, 0:n], ALU.mult),
                     reads=[("sg", u), PK(2 + u)], writes=[("aT", asl, j)])

            def ytile(tok0, asl, sub, eidx):
                t = tok0 // 128 + sub
                r = 0 if t < 32 else 1
                xsl = t % 2
                S.dma("sp", ("xt", xsl), lambda e: e.dma_start(out=xt[xsl], in_=xs_d[t * 128:(t + 1) * 128, :]),
                      reads=[("xs", t)], writes=[("xt", xsl)])
                S.begin_group()
                for half in range(2):
                    b = 4 + 2 * xsl + half
                    for j in range(nf):
                        S.op("pe", lambda e, b=b, j=j, half=half: e.matmul(
                            ps[b][:, :], aT[asl][:, j, sub * 128:(sub + 1) * 128], wo[:, j, half * 512:(half + 1) * 512],
                            start=(j == 0), stop=(j == nf - 1)),
                            reads=[("aT", asl, j), "wo"], writes=[PK(b)])
                    if half == 1:
                        S.end_group()
                    sc = g3[:, t, eidx:eidx + 1] if eidx is not None else 1.0
                    rd = [PK(b), ("G5", r)] + ([("gates", t)] if eidx is not None else [])
                    S.op("dve", lambda e, b=b, half=half, sc=sc: e.scalar_tensor_tensor(
                        t1[xsl][:, half * 512:(half + 1) * 512], ps[b][:, :], sc, G5[r][:, half * 512:(half + 1) * 512], ALU.mult, ALU.mult),
                        reads=rd, writes=[("t1", xsl)])
                S.op("dve", lambda e: e.tensor_tensor(t1[xsl], t1[xsl], xt[xsl], ALU.add),
                     reads=[("t1", xsl), ("xt", xsl)], writes=[("t1", xsl)])
                S.dma("sp", ("xst", xsl), lambda e: e.dma_start(out=xs_d[t * 128:(t + 1) * 128, :], in_=t1[xsl]),
                      reads=[("t1", xsl)], writes=[("xs", t)])

            def load_unit(gd, ud, od):
                S.dma("pool", "wg", lambda e: e.dma_start(out=wg, in_=gd.rearrange("(k p) n -> p k n", p=128)), writes=["wg"])
                S.dma("pool", "wu", lambda e: e.dma_start(out=wu, in_=ud.rearrange("(k p) n -> p k n", p=128)), writes=["wu"])
                S.dma("pool", "wo", lambda e: e.dma_start(out=wo, in_=od.rearrange("(j p) n -> p j n", p=128)), writes=["wo"])
            ti = 0
            for (gd, ud, od, eidx) in units:
                load_unit(gd, ud, od)
                for (tok0, n) in tiles:
                    asl = ti % 2
                    ti += 1
                    for j in range(nf):
                        fchunk(tok0, n, asl, j)
                    for sub in range(n // 128):
                        ytile(tok0, asl, sub, eidx)

        def phase_final():
            A.reset(0)
            gf = A.f32(D)
            xt = [A.f32(D) for _ in range(3)]
            sqj = [A.bf16(D) for _ in range(2)]
            ot = [A.f32(D) for _ in range(2)]
            S.dma("sp", "gf", lambda e: e.dma_start(out=gf, in_=g_final_d[0].partition_broadcast(128)), writes=["gf"])
            S.op("dve", lambda e: e.memset(ssq[:], 0.0), writes=[("F", "ssq", t) for t in range(32)])

            def fload(t):
                s3 = t % 3
                S.dma("sp", ("xt", s3), lambda e: e.dma_start(out=xt[s3], in_=xs_d[t * 128:(t + 1) * 128, :]),
                      reads=[("xs", t)], writes=[("xt", s3)])

            def ftile(t):
                s3 = t % 3
                sl = t % 2
                S.op("act", lambda e: e.activation(sqj[sl], xt[s3], AF.Square, accum_out=ssq[:, t:t + 1]),
                     reads=[("xt", s3)], writes=[("sqj", sl), ("F", "ssq", t)])
                S.op("act", lambda e: e.activation(rstd[:, t:t + 1], ssq[:, t:t + 1], AF.Ln, bias=cvec[:, 0:1], scale=1.0 / D),
                     reads=[("F", "ssq", t)], writes=[("F", "rstd", t)])
                S.op("act", lambda e: e.activation(rstd[:, t:t + 1], rstd[:, t:t + 1], AF.Exp, scale=-0.5),
                     reads=[("F", "rstd", t)], writes=[("F", "rstd", t)])
                S.op("dve", lambda e: e.scalar_tensor_tensor(ot[sl], xt[s3], rstd[:, t:t + 1], gf, ALU.mult, ALU.mult),
                     reads=[("xt", s3), ("F", "rstd", t), "gf"], writes=[("ot", sl)])
                S.dma("sp", ("ost", sl), lambda e: e.dma_start(out=out_d[t * 128:(t + 1) * 128, :], in_=ot[sl]),
                      reads=[("ot", sl)], writes=[("out", t)])
            fload(0)
            fload(1)
            for t in range(32):
                if t + 2 < 32:
                    fload(t + 2)
                ftile(t)
            S.final_waits += [("ost", 0), ("ost", 1)]

        for l in range(n_layers):
            need_ctx = l < 1
            moe = (l % 2 == 1)
            w_inx_d_l[0] = w_inx_d[l]
            phase_A(l)
            S.barrier()
            if debug == "hT" and l == 0:
                S.dma("sp", "dbg", lambda e: e.dma_start(out=dbg_d, in_=A.t[:, 0:H_BYTES // 4].bitcast(BF16)), writes=["dbg"])
                S.final_waits += ["dbg"]
                break
            phase_gqa(l, need_ctx)
            S.barrier()
            phase_diff(l, need_ctx)
            S.barrier()
            phase_mla(l, need_ctx)
            S.barrier()
            phase_C(l, need_ctx, moe)
            S.barrier()
            phase_D(l, need_ctx, moe)
            S.barrier()
        phase_final()
        S.run()
    return nc


_CACHE = {}


def _host_inputs(inputs):
    f = lambda a: np.ascontiguousarray(np.asarray(a, dtype=np.float32))
    cols = _winx_cols()
    w_in = f(inputs["w_in"])
    shared = {
        "w_mod": f(inputs["w_mod"]), "b_mod": f(inputs["b_mod"]),
        "g_attn": f(inputs["g_attn"]), "g_ffn": f(inputs["g_ffn"]),
        "g_final": f(inputs["g_final"]).reshape(1, D),
        "w_inx": np.ascontiguousarray(w_in[:, :, cols]),
        "w_out": f(inputs["w_out"]),
        "ffn_w_in": f(inputs["ffn_w_in"]), "ffn_w_out": f(inputs["ffn_w_out"]),
        "moe_router": f(inputs["moe_router"]), "moe_w_in": f(inputs["moe_w_in"]), "moe_w_out": f(inputs["moe_w_out"]),
    }
    p = np.arange(128)
    sp = np.zeros((128, 16), np.float32)
    gq, gk = f(inputs["gqa_gq"]), f(inputs["gqa_gk"])
    gs, gcq, gckv = f(inputs["diff_gsub"]), f(inputs["mla_gcq"]), f(inputs["mla_gckv"])
    for l in range(2):
        sp[:, l * 8 + 0] = gq[l][p % 64]
        sp[:, l * 8 + 1] = gq[l][(p % 64) ^ 1]
        sp[:, l * 8 + 2] = gk[l][p % 64]
        sp[:, l * 8 + 3] = gk[l][(p % 64) ^ 1]
        sp[:, l * 8 + 4] = gs[l][p % 64]
        sp[:, l * 8 + 5] = gcq[l][0:128]
        sp[:, l * 8 + 6] = gcq[l][128 + (p % 64)]
        sp[:, l * 8 + 7] = gckv[l]
    shared["smallp"] = sp
    shared["lamv"] = np.ascontiguousarray(np.concatenate(
        [f(inputs["diff_lq1"]), f(inputs["diff_lk1"]), f(inputs["diff_lq2"]), f(inputs["diff_lk2"])], axis=1))
    wuq = f(inputs["mla_wuq"])
    swc = np.arange(384)
    hh, rr = swc // 96, swc % 96
    swc = np.where(rr >= 64, hh * 96 + 64 + ((rr - 64) ^ 1), swc)
    shared["w_uq"] = wuq
    shared["w_uqs"] = np.ascontiguousarray(wuq[:, :, swc])
    wukv = f(inputs["mla_wukv"])
    kc = np.concatenate([np.arange(h * 128, h * 128 + 64) for h in range(4)])
    vc = np.concatenate([np.arange(h * 128 + 64, h * 128 + 128) for h in range(4)])
    shared["w_ukvk"] = np.ascontiguousarray(wukv[:, :, kc])
    shared["w_ukvv"] = np.ascontiguousarray(wukv[:, :, vc])
    ac, as_, bc, bs = _rope_tables()
    shared.update({"ropeA_c": ac, "ropeA_s": as_, "ropeB_c": bc, "ropeB_s": bs})
    cst = np.zeros((128, 384), np.float32)
    cst[:, 0:128] = np.eye(128, dtype=np.float32)
    cst[:, 128:256] = 1.0
    cst[0:64, 256:320] = 1.0
    cst[64:128, 320:384] = 1.0
    shared["cst"] = cst
    x = f(inputs["x"])
    ctx = f(inputs["ctx"])
    c = f(inputs["c"])
    c_ctx = f(inputs["c_ctx"])
    maps = []
    for b in range(x.shape[0]):
        m = dict(shared)
        m["x"] = x[b]
        m["ctx"] = ctx[b]
        m["cc"] = np.ascontiguousarray(np.stack([c[b], c_ctx], axis=0))
        maps.append(m)
    return maps


def kernel(**inputs):
    maps = _host_inputs(inputs)
    if "nc" not in _CACHE:
        _CACHE["nc"] = build()
    nc = _CACHE["nc"]
    res = run_bass_kernel_spmd(nc, maps, core_ids=list(range(len(maps))))
    return np.stack([np.asarray(r["out"]).astype(np.float32) for r in res.results], axis=0)
```
